# Optimizing a Trainium2 kernel written in Bass

```python
import math
import numpy as np
import jax
import jax.numpy as jnp
from jax import lax

D_MODEL = 1024
BATCH = 16
SEQ = 2048
DEPTH = 4

N_MIXERS = 3
HEAD_DIM = 64
N_HEADS = D_MODEL // HEAD_DIM
D_FF = 4 * D_MODEL
ROPE_THETA = 10000.0
NORM_EPS = 1e-6
NEG_INF = -1e30
NSA_KV_GROUPS = 4
NSA_CMP_LEN = 32
NSA_CMP_STRIDE = 16
NSA_CMP_HIDDEN = 4 * HEAD_DIM
NSA_SEL_LEN = 64
NSA_TOP_N = 16
NSA_WINDOW = 512
NSA_SEL_QBLOCK = 32
NSA_FORCE_BONUS = 1000.0
FOX_QBLOCK = 128
SWA_KV_HEADS = 2
SWA_WINDOW = 128
BAND_QBLOCK = 128
N_LAYERS_A = len(range(0, DEPTH, N_MIXERS))
N_LAYERS_B = len(range(1, DEPTH, N_MIXERS))
N_LAYERS_C = len(range(2, DEPTH, N_MIXERS))

kernel_name = 'hybrid_nsa_fox_swa_sink_decoder'


def rms_norm(x, g):
    xf = x.astype(jnp.float32)
    y = xf * lax.rsqrt(jnp.mean(xf * xf, axis=-1, keepdims=True) + NORM_EPS)
    return (y * g.astype(jnp.float32)).astype(x.dtype)


def rope_tables(seq):
    inv = ROPE_THETA ** (-jnp.arange(0, HEAD_DIM, 2, dtype=jnp.float32) / HEAD_DIM)
    ang = jnp.arange(seq, dtype=jnp.float32)[:, None] * inv[None, :]
    return jnp.cos(ang), jnp.sin(ang)


def apply_rope(x, cos, sin):
    xf = x.astype(jnp.float32)
    x1, x2 = jnp.split(xf, 2, axis=-1)
    c = cos[None, :, None, :]
    s = sin[None, :, None, :]
    return jnp.concatenate([x1 * c - x2 * s, x2 * c + x1 * s], axis=-1).astype(x.dtype)


def banded_attention(q, k, v, window, sinks=None):
    B, T, H, dh = q.shape
    G = k.shape[2]
    rep = H // G
    blk = BAND_QBLOCK
    nb = T // blk
    span = blk + window
    scale = dh ** -0.5
    kp = jnp.pad(k, ((0, 0), (window, 0), (0, 0), (0, 0)))
    vp = jnp.pad(v, ((0, 0), (window, 0), (0, 0), (0, 0)))
    qc = q.reshape(B, nb, blk, G, rep, dh).transpose(1, 0, 2, 3, 4, 5)
    offs_q = jnp.arange(blk)
    offs_k = jnp.arange(span) - window

    def one(args):
        i, qi = args
        start = i * blk
        kc = lax.dynamic_slice_in_dim(kp, start, span, axis=1)
        vc = lax.dynamic_slice_in_dim(vp, start, span, axis=1)
        qpos = start + offs_q
        kpos = start + offs_k
        dist = qpos[:, None] - kpos[None, :]
        valid = (kpos[None, :] >= 0) & (dist >= 0) & (dist < window)
        s = jnp.einsum('bqgrd,bkgd->bgrqk', qi, kc).astype(jnp.float32) * scale
        s = jnp.where(valid, s, NEG_INF)
        if sinks is None:
            p = jax.nn.softmax(s, axis=-1)
        else:
            sink = jnp.broadcast_to(sinks.astype(jnp.float32).reshape(G, rep)[None, :, :, None, None],
                                    (B, G, rep, blk, 1))
            p = jax.nn.softmax(jnp.concatenate([s, sink], axis=-1), axis=-1)[..., :-1]
        return jnp.einsum('bgrqk,bkgd->bqgrd', p.astype(vc.dtype), vc)

    out = lax.map(one, (jnp.arange(nb), qc))
    return out.transpose(1, 0, 2, 3, 4, 5).reshape(B, T, H, dh)


def nsa_compress(tok, pos, w1, w2):
    B, T, G, dh = tok.shape
    nc = (T - NSA_CMP_LEN) // NSA_CMP_STRIDE + 1
    idx = np.arange(nc)[:, None] * NSA_CMP_STRIDE + np.arange(NSA_CMP_LEN)[None, :]
    win = tok[:, idx] + pos[:, None, :].astype(tok.dtype)
    win = win.transpose(0, 1, 3, 2, 4).reshape(B, nc, G, NSA_CMP_LEN * dh)
    return jax.nn.silu(win @ w1) @ w2


def nsa_selected_attention(qg, k, v, sel):
    B, T, G, rep, dh = qg.shape
    ns = T // NSA_SEL_LEN
    n = sel.shape[-1]
    qb = NSA_SEL_QBLOCK
    nq = T // qb
    scale = dh ** -0.5
    kb = k.reshape(B, ns, NSA_SEL_LEN, G, dh).transpose(0, 3, 1, 2, 4)
    vb = v.reshape(B, ns, NSA_SEL_LEN, G, dh).transpose(0, 3, 1, 2, 4)
    qc = qg.reshape(B, nq, qb, G, rep, dh).transpose(1, 0, 2, 3, 4, 5)
    sc = sel.reshape(B, G, nq, qb, n).transpose(2, 0, 1, 3, 4)
    gather = jax.vmap(jax.vmap(lambda tab, ix: tab[ix]))
    offs = jnp.arange(NSA_SEL_LEN)

    def one(args):
        i, qi, si = args
        gk = gather(kb, si).reshape(B, G, qb, n * NSA_SEL_LEN, dh)
        gv = gather(vb, si).reshape(B, G, qb, n * NSA_SEL_LEN, dh)
        kpos = (si[..., None] * NSA_SEL_LEN + offs).reshape(B, G, qb, n * NSA_SEL_LEN)
        qpos = i * qb + jnp.arange(qb)
        valid = kpos <= qpos[None, None, :, None]
        s = jnp.einsum('bqgrd,bgqkd->bgrqk', qi, gk).astype(jnp.float32) * scale
        s = jnp.where(valid[:, :, None], s, NEG_INF)
        p = jax.nn.softmax(s, axis=-1)
        return jnp.einsum('bgrqk,bgqkd->bqgrd', p.astype(gv.dtype), gv)

    out = lax.map(one, (jnp.arange(nq), qc, sc))
    return out.transpose(1, 0, 2, 3, 4, 5).reshape(B, T, G * rep, dh)


def nsa_mixer(h, w_in, cmp_pos, cmp_w1, cmp_w2, w_out, cos, sin):
    B, T, _ = h.shape
    H, G, dh = N_HEADS, NSA_KV_GROUPS, HEAD_DIM
    rep = H // G
    kvw = G * dh
    scale = dh ** -0.5
    proj = h @ w_in
    sizes = [H * dh, kvw, kvw, kvw, kvw, kvw, kvw, 3 * H]
    cuts = np.cumsum(sizes)[:-1].tolist()
    q, kc_t, vc_t, ks, vs, kw, vw, gate = jnp.split(proj, cuts, axis=-1)
    q = apply_rope(q.reshape(B, T, H, dh), cos, sin)
    kc_t = apply_rope(kc_t.reshape(B, T, G, dh), cos, sin)
    ks = apply_rope(ks.reshape(B, T, G, dh), cos, sin)
    kw = apply_rope(kw.reshape(B, T, G, dh), cos, sin)
    vc_t = vc_t.reshape(B, T, G, dh)
    vs = vs.reshape(B, T, G, dh)
    vw = vw.reshape(B, T, G, dh)
    qg = q.reshape(B, T, G, rep, dh)
    tpos = jnp.arange(T)

    kcmp = nsa_compress(kc_t, cmp_pos[0], cmp_w1[0], cmp_w2[0])
    vcmp = nsa_compress(vc_t, cmp_pos[1], cmp_w1[1], cmp_w2[1])
    nc = kcmp.shape[1]
    s = jnp.einsum('btgrd,bcgd->bgrtc', qg, kcmp).astype(jnp.float32) * scale
    cend = jnp.arange(nc) * NSA_CMP_STRIDE + NSA_CMP_LEN - 1
    cvalid = cend[None, :] <= tpos[:, None]
    p_cmp = jax.nn.softmax(jnp.where(cvalid, s, NEG_INF), axis=-1)
    p_cmp = jnp.where((tpos >= NSA_CMP_LEN - 1)[:, None], p_cmp, 0.0)
    o_cmp = jnp.einsum('bgrtc,bcgd->btgrd', p_cmp.astype(vcmp.dtype), vcmp).reshape(B, T, H, dh)

    ns = T // NSA_SEL_LEN
    c_lo = np.arange(nc)[:, None] * NSA_CMP_STRIDE
    s_lo = np.arange(ns)[None, :] * NSA_SEL_LEN
    ov = np.clip(np.minimum(c_lo + NSA_CMP_LEN, s_lo + NSA_SEL_LEN) - np.maximum(c_lo, s_lo), 0, None)
    overlap = jnp.asarray(ov.astype(np.float32) / NSA_CMP_LEN)
    imp = jnp.einsum('bgrtc,cn->bgtn', p_cmp, overlap)
    tblk = tpos // NSA_SEL_LEN
    j = jnp.arange(ns)
    forced = (j[None, :] == 0) | (j[None, :] == tblk[:, None]) | (j[None, :] == tblk[:, None] - 1)
    bvalid = j[None, :] <= tblk[:, None]
    score = jnp.where(bvalid, imp + jnp.where(forced, NSA_FORCE_BONUS, 0.0), -1.0)
    n_top = min(NSA_TOP_N, ns)
    _, sel = lax.top_k(score, n_top)

    o_slc = nsa_selected_attention(qg, ks, vs, sel)
    o_win = banded_attention(q, kw, vw, NSA_WINDOW)

    g = jax.nn.sigmoid(gate.astype(jnp.float32)).reshape(B, T, H, 3)
    o = g[..., 0:1] * o_cmp + g[..., 1:2] * o_slc + g[..., 2:3] * o_win
    return o.astype(h.dtype).reshape(B, T, H * dh) @ w_out


def fox_attention(q, k, v, logf):
    B, T, H, dh = q.shape
    blk = FOX_QBLOCK
    nb = T // blk
    scale = dh ** -0.5
    c = jnp.cumsum(logf, axis=1).transpose(0, 2, 1)
    outs = []
    for i in range(nb):
        lo, hi = i * blk, (i + 1) * blk
        s = jnp.einsum('bqhd,bkhd->bhqk', q[:, lo:hi], k[:, :hi]).astype(jnp.float32) * scale
        s = s + c[:, :, lo:hi, None] - c[:, :, None, :hi]
        causal = jnp.arange(lo, hi)[:, None] >= jnp.arange(hi)[None, :]
        p = jax.nn.softmax(jnp.where(causal, s, NEG_INF), axis=-1)
        outs.append(jnp.einsum('bhqk,bkhd->bqhd', p.astype(v.dtype), v[:, :hi]))
    return jnp.concatenate(outs, axis=1)


def fox_mixer(h, w_in, b_f, w_out):
    B, T, _ = h.shape
    H, dh = N_HEADS, HEAD_DIM
    proj = h @ w_in
    q, k, v, fl = jnp.split(proj, [H * dh, 2 * H * dh, 3 * H * dh], axis=-1)
    logf = jax.nn.log_sigmoid(fl.astype(jnp.float32) + b_f.astype(jnp.float32))
    o = fox_attention(q.reshape(B, T, H, dh), k.reshape(B, T, H, dh), v.reshape(B, T, H, dh), logf)
    return o.reshape(B, T, H * dh) @ w_out


def swa_mixer(h, w_in, b_in, sinks, w_out, b_out, cos, sin):
    B, T, _ = h.shape
    H, G, dh = N_HEADS, SWA_KV_HEADS, HEAD_DIM
    proj = h @ w_in + b_in
    q, k, v = jnp.split(proj, [H * dh, H * dh + G * dh], axis=-1)
    q = apply_rope(q.reshape(B, T, H, dh), cos, sin)
    k = apply_rope(k.reshape(B, T, G, dh), cos, sin)
    o = banded_attention(q, k, v.reshape(B, T, G, dh), SWA_WINDOW, sinks)
    return o.reshape(B, T, H * dh) @ w_out + b_out


def squared_relu_mlp(h, w_up, w_down):
    return jnp.square(jax.nn.relu(h @ w_up)) @ w_down


def setup_inputs(seed: int = 0) -> dict:
    key = jax.random.key(seed)
    ks = jax.random.split(key, 20)
    D, H, dh = D_MODEL, N_HEADS, HEAD_DIM
    f32 = jnp.float32
    nsa_in_w = H * dh + 6 * NSA_KV_GROUPS * dh + 3 * H
    fox_in_w = 3 * H * dh + H
    swa_in_w = H * dh + 2 * SWA_KV_HEADS * dh
    cmp_in = NSA_CMP_LEN * dh
    nrm = lambda k, shape, fan: jax.random.normal(k, shape, f32) * (fan ** -0.5)
    return {
        'x': jax.random.normal(ks[0], (BATCH, SEQ, D), f32),
        'norm_g': 1.0 + 0.02 * jax.random.normal(ks[1], (DEPTH, 4, D), f32),
        'mlp_w_up': nrm(ks[2], (DEPTH, D, D_FF), D),
        'mlp_w_down': nrm(ks[3], (DEPTH, D_FF, D), D_FF),
        'nsa_w_in': nrm(ks[4], (N_LAYERS_A, D, nsa_in_w), D),
        'nsa_cmp_pos': 0.2 * jax.random.normal(ks[5], (N_LAYERS_A, 2, NSA_CMP_LEN, dh), f32),
        'nsa_cmp_w1': nrm(ks[6], (N_LAYERS_A, 2, cmp_in, NSA_CMP_HIDDEN), cmp_in),
        'nsa_cmp_w2': nrm(ks[7], (N_LAYERS_A, 2, NSA_CMP_HIDDEN, dh), NSA_CMP_HIDDEN),
        'nsa_w_out': nrm(ks[8], (N_LAYERS_A, H * dh, D), H * dh),
        'fox_w_in': nrm(ks[9], (N_LAYERS_B, D, fox_in_w), D),
        'fox_b_f': jax.random.uniform(ks[10], (N_LAYERS_B, H), f32, 1.0, 5.0),
        'fox_w_out': nrm(ks[11], (N_LAYERS_B, H * dh, D), H * dh),
        'swa_w_in': nrm(ks[12], (N_LAYERS_C, D, swa_in_w), D),
        'swa_b_in': 0.02 * jax.random.normal(ks[13], (N_LAYERS_C, swa_in_w), f32),
        'swa_sinks': jax.random.normal(ks[14], (N_LAYERS_C, H), f32),
        'swa_w_out': nrm(ks[15], (N_LAYERS_C, H * dh, D), H * dh),
        'swa_b_out': 0.02 * jax.random.normal(ks[16], (N_LAYERS_C, D), f32),
    }


def reference(x, norm_g, mlp_w_up, mlp_w_down, nsa_w_in, nsa_cmp_pos, nsa_cmp_w1, nsa_cmp_w2, nsa_w_out,
              fox_w_in, fox_b_f, fox_w_out, swa_w_in, swa_b_in, swa_sinks, swa_w_out, swa_b_out):
    T = x.shape[1]
    cos, sin = rope_tables(T)
    for i in range(DEPTH):
        kind = i % N_MIXERS
        slot = i // N_MIXERS
        h = rms_norm(x, norm_g[i, 0])
        if kind == 0:
            m = nsa_mixer(h, nsa_w_in[slot], nsa_cmp_pos[slot], nsa_cmp_w1[slot], nsa_cmp_w2[slot],
                          nsa_w_out[slot], cos, sin)
        elif kind == 1:
            m = fox_mixer(h, fox_w_in[slot], fox_b_f[slot], fox_w_out[slot])
        else:
            m = swa_mixer(h, swa_w_in[slot], swa_b_in[slot], swa_sinks[slot], swa_w_out[slot],
                          swa_b_out[slot], cos, sin)
        x = x + rms_norm(m, norm_g[i, 1])
        h = rms_norm(x, norm_g[i, 2])
        x = x + rms_norm(squared_relu_mlp(h, mlp_w_up[i], mlp_w_down[i]), norm_g[i, 3])
    return x
```

```python
import numpy as np
from contextlib import ExitStack
import concourse.bass as bass
import concourse.mybir as mybir
from concourse.bass_utils import run_bass_kernel_spmd

F32 = mybir.dt.float32
BF16 = mybir.dt.bfloat16
AF = mybir.ActivationFunctionType
ALU = mybir.AluOpType
AX = mybir.AxisListType

NCORES = 8
D = 1024
T = 2048
SEQ_PER_CORE = 2
NTOK = T * SEQ_PER_CORE
DFF = 4096
H = 16
DH = 64
EPS = 1e-6
NEG = -30000.0


_UNIQ = [0]


def uniq(name):
    _UNIQ[0] += 1
    return '%s_%d' % (name, _UNIQ[0])


class Chan:
    def __init__(self, sem):
        self.sem = sem
        self.count = 0


class Prog:
    def __init__(self, nc, es):
        self.nc = nc
        self.es = es
        self.names = ('pe', 'act', 'dve', 'pool', 'sp')
        self.ins = {e: [] for e in self.names}
        self.res = {}
        self.known = {e: {} for e in self.names}
        self.chans = {}
        self.epoch = 0
        self.KS = 4
        self.RS = 4096
        self.esem = {e: [self.es.enter_context(self.nc.semaphore('s_%s_%d' % (e, k))) for k in range(self.KS)]
                     for e in self.names}

    def chan(self, name):
        c = self.chans.get(name)
        if c is None:
            c = Chan(self.es.enter_context(self.nc.semaphore('c_' + name)))
            self.chans[name] = c
        return c

    def _collect(self, eng, reads, writes):
        need = {}

        def add(src, idx):
            if src[0] == 'd':
                idx = self.chans[src[1]].count
            if need.get(src, -1) < idx:
                need[src] = idx

        for k in reads:
            st = self.res.get(k)
            if st is not None and st[0] is not None:
                add(*st[0])
        for k in writes:
            st = self.res.get(k)
            if st is not None:
                if st[0] is not None:
                    add(*st[0])
                for src, idx in st[1].items():
                    add(src, idx)
        waits = []
        kn = self.known[eng]
        for src, idx in need.items():
            if eng == 'pe' and src == ('e', 'pe'):
                continue
            if kn.get(src, -1) >= idx:
                continue
            kn[src] = idx
            waits.append((src, idx))
            if src[0] == 'e':
                self.ins[src[1]][idx]['sig'] = True
        return waits

    def _update(self, ev, reads, writes):
        for k in reads:
            st = self.res.get(k)
            if st is None:
                st = [None, {}]
                self.res[k] = st
            st[1][ev[0]] = ev[1]
        for k in writes:
            self.res[k] = [ev, {}]

    def op(self, eng, method, kw, reads=(), writes=()):
        waits = self._collect(eng, reads, writes)
        idx = len(self.ins[eng])
        self.ins[eng].append(dict(m=method, kw=kw, waits=waits, sig=False, dma=None, ep=self.epoch))
        self._update((('e', eng), idx), reads, writes)

    def dma(self, q, ch, kw, reads=(), writes=()):
        c = self.chan(ch)
        waits = self._collect(q, reads, writes)
        c.count += 1
        self.ins[q].append(dict(m='dma_start', kw=kw, waits=waits, sig=False, dma=c.sem, ep=self.epoch))
        self._update((('d', ch), c.count), reads, writes)

    def barrier(self):
        last = {}
        for e in self.names:
            for i in range(len(self.ins[e]) - 1, -1, -1):
                it = self.ins[e][i]
                if it['ep'] != self.epoch:
                    break
                if it['dma'] is None and it['m'] != 'wait_only':
                    last[e] = i
                    it['sig'] = True
                    break
        for e in self.names:
            waits = []
            for y, i in last.items():
                if e == 'pe' and y == 'pe':
                    continue
                waits.append((('e', y), i))
            for name, c in self.chans.items():
                if c.count > 0:
                    waits.append((('d', name), c.count))
            self.ins[e].append(dict(m='wait_only', kw=None, waits=waits, sig=False, dma=None, ep=self.epoch))
        self.res = {}
        self.known = {e: {} for e in self.names}
        self.epoch += 1

    def emit(self):
        cnt = {}
        for e in self.names:
            n = 0
            arr = []
            for it in self.ins[e]:
                if it['sig']:
                    b = n // self.RS
                    arr.append((b % self.KS, (b // self.KS) * self.RS + (n % self.RS) + 1))
                    n += 1
                else:
                    arr.append(None)
            cnt[e] = arr
        stats = {e: (len(self.ins[e]), sum(len(it['waits']) for it in self.ins[e])) for e in self.names}
        self.stats = stats

        def mk(e):
            def body(engobj):
                for idx_, it in enumerate(self.ins[e]):
                    for (src, idx) in it['waits']:
                        if src[0] == 'e':
                            y = src[1]
                            k, v = cnt[y][idx]
                            engobj.wait_ge(self.esem[y][k], v)
                        else:
                            engobj.wait_ge(self.chans[src[1]].sem, 16 * idx)
                    if it['m'] == 'wait_only':
                        continue
                    r = getattr(engobj, it['m'])(**it['kw'])
                    if it['dma'] is not None:
                        r.then_inc(it['dma'], 16)
                    elif it['sig']:
                        r.then_inc(self.esem[e][cnt[e][idx_][0]], 1)
            return body

        with self.nc.Block() as block:
            block.tensor(mk('pe'))
            block.scalar(mk('act'))
            block.vector(mk('dve'))
            block.gpsimd(mk('pool'))
            block.sync(mk('sp'))


class Ctx:
    pass


def declare_inputs(nc):
    t = {}

    def inp(name, shape):
        t[name] = nc.dram_tensor(name, list(shape), F32, kind="ExternalInput").ap()

    inp('x', (NTOK, D))
    inp('norm_g', (4, 4, D))
    inp('mlp_w_up', (4, D, DFF))
    inp('mlp_w_down', (4, DFF, D))
    inp('nsa_w_in', (2, D, 2608))
    inp('nsa_cmp_pos', (2, 2, 32, 64))
    inp('nsa_cmp_w1', (2, 2, 2048, 256))
    inp('nsa_cmp_w2', (2, 2, 256, 64))
    inp('nsa_w_out', (2, D, D))
    inp('fox_w_in', (1, D, 3088))
    inp('fox_b_f', (1, 16))
    inp('fox_w_out', (1, D, D))
    inp('swa_w_in', (1, D, 1280))
    inp('swa_b_in', (1, 1280))
    inp('swa_sinks', (1, 16))
    inp('swa_w_out', (1, D, D))
    inp('swa_b_out', (1, D))
    t['y'] = nc.dram_tensor('y', [NTOK, D], F32, kind="ExternalOutput").ap()
    return t


def load_w_cast(P, ch, out_ap, in_ap, writes):
    P.dma('pool', ch, dict(out=out_ap, in_=in_ap, max_dma_last_dim=4096), reads=(), writes=writes)


def rstd_from_ss(P, C, ss, rstd, n, key_ss, key_rstd):
    P.op('act', 'activation', dict(out=rstd, in_=ss, func=AF.Ln, scale=1.0 / D, bias=C.eps[:, 0:1]),
         reads=[key_ss], writes=[key_rstd])
    P.op('act', 'activation', dict(out=rstd, in_=rstd, func=AF.Exp, scale=-0.5),
         reads=[key_rstd], writes=[key_rstd])


def mlp_phase(P, C, L, ysrc):
    nc = C.nc
    tn = C.t
    TT = 256
    NS = TT // 128
    ntiles = NTOK // TT
    with ExitStack() as es:
        sb = lambda name, shape, dt: es.enter_context(nc.sbuf_tensor(uniq(name), shape, dt))
        wup = sb('wup', [128, 8, DFF], BF16)
        wdn = sb('wdn', [128, 32, D], BF16)
        g3 = sb('g3', [128, D], F32)
        g4 = sb('g4', [128, D], F32)
        xts = [sb('xt%d' % i, [128, NS, D], F32) for i in range(2)]
        hb = [sb('hb%d' % i, [128, D], BF16) for i in range(2)]
        hT = [sb('hT%d' % i, [128, 8, TT], BF16) for i in range(2)]
        aT = sb('aT', [128, 32, TT], BF16)
        rl = [sb('rl%d' % i, [128, TT], F32) for i in range(2)]
        junk = sb('junk', [128, D], BF16)
        tmp = [sb('tmp%d' % i, [128, D], F32) for i in range(2)]
        ss = [sb('ss%d' % i, [128, 4], F32) for i in range(2)]
        rs = [sb('rs%d' % i, [128, 4], F32) for i in range(2)]
        ss4 = [sb('ss4%d' % i, [128, 4], F32) for i in range(2)]
        rs4 = [sb('rs4%d' % i, [128, 4], F32) for i in range(2)]

        for k in range(8):
            for hf in range(2):
                load_w_cast(P, 'wA', wup[:, k, hf * 2048:(hf + 1) * 2048],
                            tn['mlp_w_up'][L, k * 128:(k + 1) * 128, hf * 2048:(hf + 1) * 2048],
                            writes=[('wup', k)])
        for c0 in range(0, 32, 4):
            load_w_cast(P, 'wB', wdn[:, c0:c0 + 4, :],
                        tn['mlp_w_down'][L, c0 * 128:(c0 + 4) * 128, :].rearrange('(c p) n -> p c n', p=128),
                        writes=[('wdn', c0 // 4)])
        P.dma('sp', 'g', dict(out=g3[:], in_=tn['norm_g'][L, 2, :].partition_broadcast(128)), writes=[('g3',)])
        P.dma('sp', 'g', dict(out=g4[:], in_=tn['norm_g'][L, 3, :].partition_broadcast(128)), writes=[('g4',)])

        def load_x(ti):
            sl = ti % 2
            P.dma('sp', 'x%d' % sl,
                  dict(out=xts[sl][:], in_=ysrc[ti * TT:(ti + 1) * TT, :].rearrange('(s p) d -> p s d', p=128)),
                  reads=[('y', ti)], writes=[('xt', sl)])

        load_x(0)
        for ti in range(ntiles):
            sl = ti % 2
            xt = xts[sl]
            if ti + 1 < ntiles:
                load_x(ti + 1)
            for s in range(NS):
                P.op('act', 'activation', dict(out=junk[:], in_=xt[:, s, :], func=AF.Square,
                                               accum_out=ss[sl][:, s:s + 1]),
                     reads=[('xt', sl)], writes=[('junk',), ('ss', sl)])
            rstd_from_ss(P, C, ss[sl][:, 0:NS], rs[sl][:, 0:NS], NS, ('ss', sl), ('rs', sl))
            for s in range(NS):
                hs = (ti * NS + s) % 2
                P.op('dve', 'scalar_tensor_tensor',
                     dict(out=hb[hs][:], in0=xt[:, s, :], scalar=rs[sl][:, s:s + 1], in1=g3[:],
                          op0=ALU.mult, op1=ALU.mult),
                     reads=[('xt', sl), ('rs', sl), ('g3',)], writes=[('hb', hs)])
                pb = C.ps_bf[hs]
                for c in range(8):
                    P.op('pe', 'transpose', dict(out=pb[:, c, :], in_=hb[hs][:, c * 128:(c + 1) * 128],
                                                 identity=C.ident[:]),
                         reads=[('hb', hs), ('ident',)], writes=[('ps', hs)])
                P.op('act', 'copy', dict(out=hT[sl][:, :, s * 128:(s + 1) * 128], in_=pb[:, :, :]),
                     reads=[('ps', hs)], writes=[('hT', sl)])
            for fc in range(32):
                bk = 2 + (fc % 2)
                for k in range(8):
                    P.op('pe', 'matmul', dict(out=C.ps[bk][:, 0:TT], lhsT=wup[:, k, fc * 128:(fc + 1) * 128],
                                              rhs=hT[sl][:, k, :], start=(k == 0), stop=(k == 7)),
                         reads=[('wup', k), ('hT', sl)], writes=[('ps', bk)])
                r = rl[fc % 2]
                P.op('act', 'activation', dict(out=r[:], in_=C.ps[bk][:, 0:TT], func=AF.Relu),
                     reads=[('ps', bk)], writes=[('rl', fc % 2)])
                P.op('dve', 'tensor_tensor', dict(out=aT[:, fc, :], in0=r[:], in1=r[:], op=ALU.mult),
                     reads=[('rl', fc % 2)], writes=[('aT', fc)])
            for s in range(NS):
                for dh in range(2):
                    bk = 4 + dh
                    for fc in range(32):
                        P.op('pe', 'matmul',
                             dict(out=C.ps[bk][:, :], lhsT=aT[:, fc, s * 128:(s + 1) * 128],
                                  rhs=wdn[:, fc, dh * 512:(dh + 1) * 512], start=(fc == 0), stop=(fc == 31)),
                             reads=[('aT', fc), ('wdn', fc // 4)], writes=[('ps', bk)])
                    P.op('act', 'activation', dict(out=junk[:, 0:512], in_=C.ps[bk][:, :], func=AF.Square,
                                                   accum_out=ss4[sl][:, 2 * s + dh:2 * s + dh + 1]),
                         reads=[('ps', bk)], writes=[('junk',), ('ss4', sl, s, dh)])
                P.op('dve', 'tensor_tensor', dict(out=ss4[sl][:, 2 * s:2 * s + 1], in0=ss4[sl][:, 2 * s:2 * s + 1],
                                                  in1=ss4[sl][:, 2 * s + 1:2 * s + 2], op=ALU.add),
                     reads=[('ss4', sl, s, 0), ('ss4', sl, s, 1)], writes=[('ss4', sl, s, 0)])
                rstd_from_ss(P, C, ss4[sl][:, 2 * s:2 * s + 1], rs4[sl][:, s:s + 1], 1,
                             ('ss4', sl, s, 0), ('rs4', sl, s))
                tm = tmp[s % 2]
                for dh in range(2):
                    bk = 4 + dh
                    P.op('dve', 'scalar_tensor_tensor',
                         dict(out=tm[:, dh * 512:(dh + 1) * 512], in0=C.ps[bk][:, :], scalar=rs4[sl][:, s:s + 1],
                              in1=g4[:, dh * 512:(dh + 1) * 512], op0=ALU.mult, op1=ALU.mult),
                         reads=[('ps', bk), ('rs4', sl, s), ('g4',)], writes=[('tmp', s % 2, dh)])
                P.op('pool', 'tensor_tensor', dict(out=xt[:, s, :], in0=xt[:, s, :], in1=tm[:], op=ALU.add),
                     reads=[('xt', sl), ('tmp', s % 2, 0), ('tmp', s % 2, 1)], writes=[('xt', sl)])
            P.dma('sp', 'yo%d' % sl,
                  dict(out=C.t['y'][ti * TT:(ti + 1) * TT, :].rearrange('(s p) d -> p s d', p=128), in_=xt[:]),
                  reads=[('xt', sl)], writes=[('y', ti)])
        P.barrier()


CONST_SHAPES = {
    'c_rope': (2, 128, T),
    'c_maskC': (128, 128),
    'c_maskW': (128, 128),
    'c_cmaskT': (128, T),
    'c_cmask': (128, 8, 128),
    'c_eexp': (32, T),
    'c_bonus': (128, 8, 32),
    'c_gsel': (12, 768),
}


def host_consts():
    c = {}
    inv = (np.float32(10000.0) ** (-np.arange(0, DH, 2, dtype=np.float32) / np.float32(DH))).astype(np.float32)
    ang = (np.arange(T, dtype=np.float32)[:, None] * inv[None, :]).astype(np.float32)
    cos = np.cos(ang).astype(np.float32).T
    sin = np.sin(ang).astype(np.float32).T
    c['c_rope'] = np.stack([np.tile(cos, (4, 1)), np.tile(sin, (4, 1))]).astype(np.float32)
    s = np.arange(128)[:, None]
    t = np.arange(128)[None, :]
    c['c_maskC'] = np.where(t >= s, 0.0, NEG).astype(np.float32)
    c['c_maskW'] = np.where(t < s, 0.0, NEG).astype(np.float32)
    cc = np.arange(128)[:, None]
    tt = np.arange(T)[None, :]
    c['c_cmaskT'] = np.where(16 * cc + 31 <= tt, 0.0, NEG).astype(np.float32)
    tq = 1024 + np.arange(8)[None, :, None] * 128 + np.arange(128)[:, None, None]
    c['c_cmask'] = np.where(16 * np.arange(128)[None, None, :] + 31 <= tq, 0.0, NEG).astype(np.float32)
    c['c_eexp'] = (np.arange(T)[None, :] // 64 == np.arange(32)[:, None]).astype(np.float32)
    tblk = tq // 64
    n = np.arange(32)[None, None, :]
    forced = (n == 0) | (n == tblk) | (n == tblk - 1)
    c['c_bonus'] = np.where(n <= tblk, np.where(forced, 1000.0, 0.0), -1.0).astype(np.float32)
    c['c_gsel'] = (np.arange(768)[None, :] // 64 == np.arange(12)[:, None]).astype(np.float32)
    return c


class Rot:
    def __init__(self, n):
        self.n = n
        self.i = 0

    def next(self):
        v = self.i % self.n
        self.i += 1
        return v


def attn_phase(P, C, L, ysrc):
    nc = C.nc
    tn = C.t
    kind = L % 3
    slot = L // 3
    if kind == 0:
        win, wout_d = tn['nsa_w_in'][slot], tn['nsa_w_out'][slot]
    elif kind == 1:
        win, wout_d = tn['fox_w_in'][slot], tn['fox_w_out'][slot]
    else:
        win, wout_d = tn['swa_w_in'][slot], tn['swa_w_out'][slot]
    ydst = tn['y']

    with ExitStack() as es:
        sb = lambda name, shape, dt: es.enter_context(nc.sbuf_tensor(uniq(name), shape, dt))
        hT = sb('a_hT', [128, 8, T], BF16)
        OT = sb('a_OT', [128, 8, T], BF16)
        maskC = sb('a_maskC', [128, 128], BF16)
        maskW = sb('a_maskW', [128, 128], BF16)
        ones_row = sb('a_ones', [1, 512], BF16)
        wch = [sb('a_wch%d' % i, [128, 8, 128], BF16) for i in range(2)]
        wrot = [sb('a_wrot%d' % i, [128, 8, 128], BF16) for i in range(2)]
        bch = [sb('a_bch%d' % i, [1, 128], BF16) for i in range(2)]
        brot = [sb('a_brot%d' % i, [1, 128], BF16) for i in range(2)]
        rtmp = [sb('a_rtmp%d' % i, [128, 512], F32) for i in range(4)]
        pT = [sb('a_pT%d' % i, [128, 512], BF16) for i in range(3)]
        fden = [sb('a_fden%d' % i, [128, 512], F32) for i in range(2)]
        if kind != 1:
            ropeC = sb('a_ropeC', [128, T], F32)
            ropeS = sb('a_ropeS', [128, T], F32)
            P.dma('sp', 'cst', dict(out=ropeC[:], in_=tn['c_rope'][0]), writes=[('ropeC',)])
            P.dma('sp', 'cst', dict(out=ropeS[:], in_=tn['c_rope'][1]), writes=[('ropeS',)])
        load_w_cast(P, 'cstp', maskC[:], tn['c_maskC'], [('maskC',)])
        load_w_cast(P, 'cstp', maskW[:], tn['c_maskW'], [('maskW',)])
        P.op('pool', 'memset', dict(ap=ones_row[:], constant=1.0), writes=[('ones_row',)])
        A = Ctx()
        A.jobrot = Rot(2)
        A.psA = Rot(2)
        A.rt = Rot(2)
        A.ptr = Rot(3)
        A.psS = Rot(3)
        A.psO = Rot(2)
        A.fd = Rot(2)

        def load_chunk(segs, bias_d):
            bi = A.jobrot.next()
            off = 0
            for (c0, n) in segs:
                load_w_cast(P, 'wc%d' % bi, wch[bi][:, :, off:off + n],
                            win[:, c0:c0 + n].rearrange('(k p) n -> p k n', p=128), writes=[('wch', bi)])
                if bias_d is not None:
                    load_w_cast(P, 'wc%d' % bi, bch[bi][0:1, off:off + n], bias_d(c0, n).unsqueeze(0),
                                writes=[('bch', bi)])
                off += n
            return bi, off

        def make_rot(bi, rows, has_bias):
            nb = rows // 64
            v = wch[bi][:, :, 0:rows].rearrange('p k (b two r) -> p k b two r', two=2, r=32)
            w = wrot[bi][:, :, 0:rows].rearrange('p k (b two r) -> p k b two r', two=2, r=32)
            for b in range(nb):
                P.op('pool', 'tensor_scalar', dict(out=w[:, :, b, 0, :], in0=v[:, :, b, 1, :], scalar1=-1.0,
                                                   scalar2=None, op0=ALU.mult),
                     reads=[('wch', bi)], writes=[('wrot', bi, b, 0)])
                P.op('pool', 'tensor_copy', dict(out=w[:, :, b, 1, :], in_=v[:, :, b, 0, :]),
                     reads=[('wch', bi)], writes=[('wrot', bi, b, 1)])
            if has_bias:
                v = bch[bi][0:1, 0:rows].rearrange('p (b two r) -> p b two r', two=2, r=32)
                w = brot[bi][0:1, 0:rows].rearrange('p (b two r) -> p b two r', two=2, r=32)
                P.op('pool', 'tensor_scalar', dict(out=w[:, :, 0, :], in0=v[:, :, 1, :], scalar1=-1.0,
                                                   scalar2=None, op0=ALU.mult),
                     reads=[('bch', bi)], writes=[('brot', bi, 0)])
                P.op('pool', 'tensor_copy', dict(out=w[:, :, 1, :], in_=v[:, :, 0, :]),
                     reads=[('bch', bi)], writes=[('brot', bi, 1)])

        def proj_fm(segs, rope, evac, bias_d=None):
            bi, rows = load_chunk(segs, bias_d)
            if rope:
                make_rot(bi, rows, bias_d is not None)
            rotkeys = [('wrot', bi, b, x) for b in range(rows // 64) for x in range(2)]
            for tq in range(4):
                bkA = 2 + A.psA.next()
                for k in range(8):
                    P.op('pe', 'matmul', dict(out=C.ps[bkA][0:rows, :], lhsT=wch[bi][:, k, 0:rows],
                                              rhs=hT[:, k, tq * 512:(tq + 1) * 512], start=(k == 0),
                                              stop=(k == 7 and bias_d is None)),
                         reads=[('wch', bi), ('hT', tq)], writes=[('ps', bkA)])
                if bias_d is not None:
                    P.op('pe', 'matmul', dict(out=C.ps[bkA][0:rows, :], lhsT=bch[bi][0:1, 0:rows],
                                              rhs=ones_row[0:1, :], start=False, stop=True),
                         reads=[('bch', bi), ('ones_row',)], writes=[('ps', bkA)])
                if not rope:
                    evac(tq, C.ps[bkA][0:rows, :], ('ps', bkA))
                    continue
                bkB = bkA + 2
                for k in range(8):
                    P.op('pe', 'matmul', dict(out=C.ps[bkB][0:rows, :], lhsT=wrot[bi][:, k, 0:rows],
                                              rhs=hT[:, k, tq * 512:(tq + 1) * 512], start=(k == 0),
                                              stop=(k == 7 and bias_d is None)),
                         reads=rotkeys + [('hT', tq)], writes=[('ps', bkB)])
                if bias_d is not None:
                    P.op('pe', 'matmul', dict(out=C.ps[bkB][0:rows, :], lhsT=brot[bi][0:1, 0:rows],
                                              rhs=ones_row[0:1, :], start=False, stop=True),
                         reads=[('brot', bi, 0), ('brot', bi, 1), ('ones_row',)], writes=[('ps', bkB)])
                ri = A.rt.next()
                t1, t2 = rtmp[2 * ri], rtmp[2 * ri + 1]
                P.op('dve', 'tensor_tensor', dict(out=t1[0:rows, :], in0=C.ps[bkA][0:rows, :],
                                                  in1=ropeC[0:rows, tq * 512:(tq + 1) * 512], op=ALU.mult),
                     reads=[('ps', bkA), ('ropeC',)], writes=[('rtmp', 2 * ri)])
                P.op('dve', 'tensor_tensor', dict(out=t2[0:rows, :], in0=C.ps[bkB][0:rows, :],
                                                  in1=ropeS[0:rows, tq * 512:(tq + 1) * 512], op=ALU.mult),
                     reads=[('ps', bkB), ('ropeS',)], writes=[('rtmp', 2 * ri + 1)])
                evac(tq, (t1[0:rows, :], t2[0:rows, :]), [('rtmp', 2 * ri), ('rtmp', 2 * ri + 1)])

        def evac_to(dst_fn, wkey_fn, rope):
            def f(tq, src, skeys):
                if rope:
                    P.op('pool', 'tensor_tensor', dict(out=dst_fn(tq), in0=src[0], in1=src[1], op=ALU.add),
                         reads=skeys, writes=[wkey_fn(tq)])
                else:
                    P.op('act', 'copy', dict(out=dst_fn(tq), in_=src), reads=[skeys], writes=[wkey_fn(tq)])
            return f

        def proj_tm(c0, ncol, out_fn, vkey, wv, bias_d=None):
            load_w_cast(P, 'wv', wv[:, :, 0:ncol], win[:, c0:c0 + ncol].rearrange('(k p) n -> p k n', p=128),
                        writes=[('wv',)])
            if bias_d is not None:
                load_w_cast(P, 'wv', bch[0][0:1, 0:ncol], bias_d(c0, ncol).unsqueeze(0), writes=[('bch', 0)])
            for k4 in range(4):
                bk = 6 + (k4 % 2)
                for j in range(4):
                    kt = k4 * 4 + j
                    for k in range(8):
                        P.op('pe', 'matmul', dict(out=C.ps[bk][:, j * 64:(j + 1) * 64],
                                                  lhsT=hT[:, k, kt * 128:(kt + 1) * 128], rhs=wv[:, k, 0:ncol],
                                                  start=(k == 0), stop=(k == 7 and bias_d is None),
                                                  skip_group_check=True),
                             reads=[('wv',), ('hT', kt // 4)], writes=[('ps', bk)])
                    if bias_d is not None:
                        P.op('pe', 'matmul', dict(out=C.ps[bk][:, j * 64:(j + 1) * 64], lhsT=ones_row[0:1, 0:128],
                                                  rhs=bch[0][0:1, 0:ncol], start=False, stop=True,
                                                  skip_group_check=True),
                             reads=[('bch', 0), ('ones_row',)], writes=[('ps', bk)])
                P.op('act', 'copy', dict(out=out_fn(k4),
                                         in_=C.ps[bk][:, 0:256].rearrange('p (j d) -> p j d', d=64)),
                     reads=[('ps', bk)], writes=[(vkey, k4)])

        def st_tile(qsrc, ksrc, kt, q0, ca, cb, masks, extra=None):
            bk = A.psS.next()
            rhs, rkeys = qsrc(q0 + ca, q0 + cb)
            lhs, lkeys = ksrc(kt)
            last = (not masks) and (extra is None)
            P.op('pe', 'matmul', dict(out=C.ps[bk][:, ca:cb], lhsT=lhs, rhs=rhs, start=True, stop=last,
                                      skip_group_check=True),
                 reads=rkeys + lkeys, writes=[('ps', bk)])
            if extra is not None:
                elhs, erhs, ekeys = extra(kt, q0 + ca, q0 + cb)
                P.op('pe', 'matmul', dict(out=C.ps[bk][:, ca:cb], lhsT=elhs, rhs=erhs, start=False,
                                          stop=(not masks), skip_group_check=True),
                     reads=ekeys, writes=[('ps', bk)])
            for mi, (mt, mkey, bc) in enumerate(masks):
                P.op('pe', 'matmul', dict(out=C.ps[bk][:, bc:bc + 128], lhsT=C.ident[:], rhs=mt, start=False,
                                          stop=(mi == len(masks) - 1), skip_group_check=True),
                     reads=[('ident',), mkey], writes=[('ps', bk)])
            return bk

        def exp_pv(bk, ca, cb, vlhs, vkeys, bo, first, lastpv):
            pi = A.ptr.next()
            P.op('act', 'activation', dict(out=pT[pi][:, ca:cb], in_=C.ps[bk][:, ca:cb], func=AF.Exp, scale=0.125),
                 reads=[('ps', bk)], writes=[('pT', pi)])
            P.op('pe', 'matmul', dict(out=C.ps[bo][:, ca:cb], lhsT=vlhs, rhs=pT[pi][:, ca:cb], start=first,
                                      stop=lastpv, skip_group_check=True),
                 reads=[('pT', pi)] + vkeys, writes=[('ps', bo)])

        def band_tiles(qi, window_tiles, causal_only):
            out = []
            if causal_only:
                js = list(range(-4 * qi, 4))
            else:
                js = [j for j in range(-window_tiles, 4) if 4 * qi + j >= 0]
            js.sort(key=lambda j: (0 if j <= 0 and (causal_only or j + window_tiles >= 3) else 1, j))
            for j in js:
                kt = 4 * qi + j
                ca = max(0, 128 * j)
                masks = []
                if j >= 0:
                    masks.append((maskC[:], ('maskC',), 128 * j))
                if causal_only:
                    cb = 512
                else:
                    cb = min(512, 128 * (j + window_tiles) + 128)
                    jb = j + window_tiles
                    if 0 <= jb <= 3:
                        masks.append((maskW[:], ('maskW',), 128 * jb))
                out.append((kt, ca, cb, masks))
            return out

        for sq in range(SEQ_PER_CORE):
            tb = sq * T
            with ExitStack() as es1:
                sb1 = lambda name, shape, dt: es1.enter_context(nc.sbuf_tensor(uniq(name), shape, dt))
                xts = [sb1('n_xt%d' % i, [128, 2, D], F32) for i in range(2)]
                hb = [sb1('n_hb%d' % i, [128, D], BF16) for i in range(2)]
                junk = sb1('n_junk', [128, D], BF16)
                ss = [sb1('n_ss%d' % i, [128, 2], F32) for i in range(2)]
                rs = [sb1('n_rs%d' % i, [128, 2], F32) for i in range(2)]
                g1 = sb1('a_g1', [128, D], F32)
                P.dma('sp', 'g', dict(out=g1[:], in_=tn['norm_g'][L, 0, :].partition_broadcast(128)), writes=[('g1',)])

                def load_x(ti):
                    sl = ti % 2
                    P.dma('sp', 'x%d' % sl,
                          dict(out=xts[sl][:], in_=ysrc[tb + ti * 256:tb + (ti + 1) * 256, :]
                               .rearrange('(s p) d -> p s d', p=128)),
                          reads=[('y', sq, ti // 2)], writes=[('xt', sl)])
                load_x(0)
                for ti in range(8):
                    sl = ti % 2
                    if ti + 1 < 8:
                        load_x(ti + 1)
                    for s in range(2):
                        P.op('act', 'activation', dict(out=junk[:], in_=xts[sl][:, s, :], func=AF.Square,
                                                       accum_out=ss[sl][:, s:s + 1]),
                             reads=[('xt', sl)], writes=[('junk',), ('ss', sl)])
                    rstd_from_ss(P, C, ss[sl][:, 0:2], rs[sl][:, 0:2], 2, ('ss', sl), ('rs', sl))
                    for s in range(2):
                        hs = (ti * 2 + s) % 2
                        P.op('dve', 'scalar_tensor_tensor',
                             dict(out=hb[hs][:], in0=xts[sl][:, s, :], scalar=rs[sl][:, s:s + 1], in1=g1[:],
                                  op0=ALU.mult, op1=ALU.mult),
                             reads=[('xt', sl), ('rs', sl), ('g1',)], writes=[('hb', hs)])
                        pb = C.ps_bf[hs]
                        for c in range(8):
                            P.op('pe', 'transpose', dict(out=pb[:, c, :], in_=hb[hs][:, c * 128:(c + 1) * 128],
                                                         identity=C.ident[:]),
                                 reads=[('hb', hs), ('ident',)], writes=[('ps', hs)])
                        col = ti * 256 + s * 128
                        P.op('act', 'copy', dict(out=hT[:, :, col:col + 128], in_=pb[:, :, :]),
                             reads=[('ps', hs)], writes=[('hT', col // 512)])
                P.barrier()

            if kind == 2:
                swa_seq(P, C, A, locals())
            elif kind == 1:
                fox_seq(P, C, A, locals())
            else:
                nsa_seq(P, C, A, locals())

            with ExitStack() as es3:
                sb3 = lambda name, shape, dt: es3.enter_context(nc.sbuf_tensor(uniq(name), shape, dt))
                xt = [sb3('c_xt%d' % i, [128, 2, D], F32) for i in range(2)]
                tmp = [sb3('c_tmp%d' % i, [128, D], F32) for i in range(2)]
                junk = sb3('c_junk', [128, 512], BF16)
                ss2 = sb3('c_ss', [128, 64], F32)
                rs2 = sb3('c_rs', [128, 32], F32)
                bo = sb3('c_bo', [1, D], BF16)
                wo = sb3('a_wo', [128, 8, D], BF16)
                g2 = sb3('a_g2', [128, D], F32)
                for c0 in range(0, 8, 4):
                    load_w_cast(P, 'wA', wo[:, c0:c0 + 4, :],
                                wout_d[c0 * 128:(c0 + 4) * 128, :].rearrange('(c p) n -> p c n', p=128),
                                writes=[('wo', c0 // 4)])
                P.dma('sp', 'g', dict(out=g2[:], in_=tn['norm_g'][L, 1, :].partition_broadcast(128)), writes=[('g2',)])
                if kind == 2:
                    load_w_cast(P, 'wv', bo[0:1, :], tn['swa_b_out'][slot].unsqueeze(0), writes=[('bo',)])
                for ti in range(8):
                    sl = ti % 2
                    P.dma('sp', 'x%d' % sl,
                          dict(out=xt[sl][:], in_=ysrc[tb + ti * 256:tb + (ti + 1) * 256, :]
                               .rearrange('(s p) d -> p s d', p=128)),
                          reads=[('y', sq, ti // 2)], writes=[('cxt', sl)])
                    for s in range(2):
                        kt = ti * 2 + s
                        for dh in range(2):
                            bk = 2 + dh
                            for c in range(8):
                                P.op('pe', 'matmul',
                                     dict(out=C.ps[bk][:, :], lhsT=OT[:, c, kt * 128:(kt + 1) * 128],
                                          rhs=wo[:, c, dh * 512:(dh + 1) * 512], start=(c == 0),
                                          stop=(c == 7 and kind != 2)),
                                     reads=[('OT', c, kt // 4), ('wo', c // 4)], writes=[('ps', bk)])
                            if kind == 2:
                                P.op('pe', 'matmul',
                                     dict(out=C.ps[bk][:, :], lhsT=ones_row[0:1, 0:128],
                                          rhs=bo[0:1, dh * 512:(dh + 1) * 512], start=False, stop=True),
                                     reads=[('bo',), ('ones_row',)], writes=[('ps', bk)])
                            P.op('act', 'activation', dict(out=junk[:, :], in_=C.ps[bk][:, :], func=AF.Square,
                                                           accum_out=ss2[:, 2 * kt + dh:2 * kt + dh + 1]),
                                 reads=[('ps', bk)], writes=[('cjunk',), ('css', kt, dh)])
                        P.op('dve', 'tensor_tensor', dict(out=ss2[:, 2 * kt:2 * kt + 1], in0=ss2[:, 2 * kt:2 * kt + 1],
                                                          in1=ss2[:, 2 * kt + 1:2 * kt + 2], op=ALU.add),
                             reads=[('css', kt, 0), ('css', kt, 1)], writes=[('css', kt, 0)])
                        rstd_from_ss(P, C, ss2[:, 2 * kt:2 * kt + 1], rs2[:, kt:kt + 1], 1, ('css', kt, 0), ('crs', kt))
                        tm = tmp[s % 2]
                        for dh in range(2):
                            bk = 2 + dh
                            P.op('dve', 'scalar_tensor_tensor',
                                 dict(out=tm[:, dh * 512:(dh + 1) * 512], in0=C.ps[bk][:, :], scalar=rs2[:, kt:kt + 1],
                                      in1=g2[:, dh * 512:(dh + 1) * 512], op0=ALU.mult, op1=ALU.mult),
                                 reads=[('ps', bk), ('crs', kt), ('g2',)], writes=[('ctmp', s % 2, dh)])
                        P.op('pool', 'tensor_tensor', dict(out=xt[sl][:, s, :], in0=xt[sl][:, s, :], in1=tm[:],
                                                           op=ALU.add),
                             reads=[('cxt', sl), ('ctmp', s % 2, 0), ('ctmp', s % 2, 1)], writes=[('cxt', sl)])
                    P.dma('sp', 'yo%d' % sl,
                          dict(out=ydst[tb + ti * 256:tb + (ti + 1) * 256, :].rearrange('(s p) d -> p s d', p=128),
                               in_=xt[sl][:]),
                          reads=[('cxt', sl)], writes=[('y', sq, ti // 2)])
                P.barrier()


def run_jobs(jobs, LA=2):
    banks = {}
    n = len(jobs)
    for i in range(n + LA):
        if i < n:
            if jobs[i].get('pre'):
                jobs[i]['pre']()
            banks[i] = jobs[i]['st']()
        k = i - LA
        if k >= 0:
            jobs[k]['ep'](banks.pop(k))
            if jobs[k].get('fin'):
                jobs[k]['fin']()


class NS:
    def __init__(self, d):
        self.__dict__.update(d)


def finish_simple(P, C, A, E, bo, rows, chunk, q0, addk):
    fd = E.fden[A.fd.next()]
    fkey = ('fden', (A.fd.i - 1) % 2)
    if addk is not None:
        P.op('dve', 'tensor_scalar', dict(out=fd[0:64, :], in0=C.ps[bo][64:128, :], scalar1=addk, scalar2=None,
                                          op0=ALU.add),
             reads=[('ps', bo), ('esk',)], writes=[fkey])
        P.op('dve', 'reciprocal', dict(out=fd[0:64, :], in_=fd[0:64, :]), reads=[fkey], writes=[fkey])
    else:
        P.op('dve', 'reciprocal', dict(out=fd[0:64, :], in_=C.ps[bo][64:128, :]), reads=[('ps', bo)], writes=[fkey])
    P.op('dve', 'tensor_tensor', dict(out=E.OT[rows, chunk, q0:q0 + 512], in0=C.ps[bo][0:64, :], in1=fd[0:64, :],
                                      op=ALU.mult),
         reads=[('ps', bo), fkey], writes=[('OT', chunk, q0 // 512)])


def swa_seq(P, C, A, Ed):
    E = NS(Ed)
    nc, tn = C.nc, C.t
    b_in = tn['swa_b_in'][E.slot]
    bias_fn = lambda c0, n: b_in[c0:c0 + n]
    for g in range(2):
        with ExitStack() as es2:
            sb = lambda name, shape, dt: es2.enter_context(nc.sbuf_tensor(uniq(name), shape, dt))
            qT = sb('s_qT', [128, 4, T], BF16)
            kd = sb('s_kd', [128, T], BF16)
            Vt = sb('s_V', [128, 16, 2, 64], BF16)
            wv = sb('s_wv', [128, 8, 64], BF16)
            esk = sb('s_esk', [128, 16], F32)
            P.op('pool', 'memset', dict(ap=Vt[:, :, 1, :], constant=1.0), writes=[('Vones',)])
            P.dma('sp', 'g', dict(out=esk[:], in_=tn['swa_sinks'][E.slot].partition_broadcast(128)), writes=[('esk',)])
            P.op('act', 'activation', dict(out=esk[:], in_=esk[:], func=AF.Exp), reads=[('esk',)], writes=[('esk',)])
            for j in range(4):
                E.proj_fm([(g * 512 + j * 128, 128)], True,
                          E.evac_to(lambda tq, j=j: qT[:, j, tq * 512:(tq + 1) * 512],
                                    lambda tq, j=j: ('qT', j, tq), True), bias_fn)
            E.proj_fm([(1024 + g * 64, 64)] * 2, True,
                      E.evac_to(lambda tq: kd[:, tq * 512:(tq + 1) * 512], lambda tq: ('kd', tq), True), bias_fn)
            E.proj_tm(1152 + g * 64, 64, lambda k4: Vt[:, k4 * 4:(k4 + 1) * 4, 0, :], 'Vt', wv, bias_fn)
            jobs = []
            for r in range(8):
                h = 8 * g + r
                j, half = r // 2, r % 2
                rows = slice(64 * half, 64 * half + 64)
                qsrc = lambda c0, c1, rows=rows, j=j: (qT[rows, j, c0:c1], [('qT', j, c0 // 512)])
                ksrc = lambda kt, rows=rows: (kd[rows, kt * 128:(kt + 1) * 128], [('kd', kt // 4)])
                for qi in range(4):
                    tiles = E.band_tiles(qi, 1, False)
                    bo = 3 + A.psO.next()
                    for n_, (kt, ca, cb, masks) in enumerate(tiles):
                        job = dict(
                            st=lambda qsrc=qsrc, ksrc=ksrc, kt=kt, qi=qi, ca=ca, cb=cb, masks=masks:
                            E.st_tile(qsrc, ksrc, kt, qi * 512, ca, cb, masks),
                            ep=lambda bk, kt=kt, ca=ca, cb=cb, bo=bo, f=(n_ == 0), l=(n_ == len(tiles) - 1):
                            E.exp_pv(bk, ca, cb, Vt[:, kt, :, :].rearrange('p a d -> p (a d)'),
                                     [('Vt', kt // 4), ('Vones',)], bo, f, l))
                        if n_ == len(tiles) - 1:
                            job['fin'] = (lambda bo=bo, rows=rows, j=j, qi=qi, h=h:
                                          finish_simple(P, C, A, E, bo, rows, 4 * g + j, qi * 512, esk[64:128, h:h + 1]))
                        jobs.append(job)
            run_jobs(jobs)
            P.barrier()


def fox_seq(P, C, A, Ed):
    E = NS(Ed)
    nc, tn = C.nc, C.t
    b_f = tn['fox_b_f'][E.slot]
    scrQ, scrK = C.scrQ, C.scrK
    with ExitStack() as es2:
        sb = lambda name, shape, dt: es2.enter_context(nc.sbuf_tensor(uniq(name), shape, dt))
        spt = sb('f_spt', [16, T], F32)
        cs = sb('f_cs', [16, T], F32)
        ones16 = sb('f_ones', [16, T], F32)
        res_ = sb('f_res', [16, T], F32)
        parts = [sb('f_a%d' % i, [16, T], BF16) for i in range(3)]
        nparts = [sb('f_n%d' % i, [16, T], BF16) for i in range(3)]
        P.op('pool', 'memset', dict(ap=ones16[:], constant=1.0), writes=[('ones16',)])

        def evac_fl(tq, src, skey):
            sl = spt[0:16, tq * 512:(tq + 1) * 512]
            P.op('act', 'activation', dict(out=sl, in_=src, func=AF.Exp, scale=-1.0), reads=[skey],
                 writes=[('spt', tq)])
            P.op('act', 'activation', dict(out=sl, in_=sl, func=AF.Ln, bias=C.one[0:16, 0:1], scale=1.0),
                 reads=[('spt', tq)], writes=[('spt', tq)])
        E.proj_fm([(3072, 16)], False, evac_fl, lambda c0, n: b_f[0:16])
        P.op('dve', 'tensor_tensor_scan', dict(out=cs[:], data0=ones16[:], data1=spt[:], initial=0.0,
                                               op0=ALU.mult, op1=ALU.add),
             reads=[('ones16',)] + [('spt', tq) for tq in range(4)], writes=[('cs',)])
        P.op('dve', 'tensor_scalar', dict(out=cs[:], in0=cs[:], scalar1=8.0, scalar2=None, op0=ALU.mult),
             reads=[('cs',)], writes=[('cs',)])
        cur = cs
        ckey = ('cs',)
        for i in range(3):
            P.op('dve', 'tensor_copy', dict(out=parts[i][:], in_=cur[:]), reads=[ckey], writes=[('part', i)])
            P.op('dve', 'tensor_scalar', dict(out=nparts[i][:], in0=parts[i][:], scalar1=-1.0, scalar2=None,
                                              op0=ALU.mult), reads=[('part', i)], writes=[('npart', i)])
            if i < 2:
                P.op('dve', 'tensor_tensor', dict(out=res_[:], in0=cur[:], in1=parts[i][:], op=ALU.subtract),
                     reads=[ckey, ('part', i)], writes=[('res',)])
                cur, ckey = res_, ('res',)
            P.dma('sp', 'scr', dict(out=scrQ[i], in_=nparts[i][:]), reads=[('npart', i)], writes=[('scrQ', i)])
            P.dma('sp', 'scr', dict(out=scrK[i], in_=parts[i][:]), reads=[('part', i)], writes=[('scrK', i)])
        P.barrier()
    for gp in range(4):
        with ExitStack() as es2:
            sb = lambda name, shape, dt: es2.enter_context(nc.sbuf_tensor(uniq(name), shape, dt))
            qT = sb('f_qT', [128, 2, T], BF16)
            kT = sb('f_kT', [128, 2, T], BF16)
            Vt = sb('f_V', [128, 16, 4, 2, 64], BF16)
            wv = sb('f_wv', [128, 8, 64], BF16)
            cq = sb('f_cq', [6, 4, T], BF16)
            ck = sb('f_ck', [6, 4, T], BF16)
            P.op('pool', 'memset', dict(ap=Vt[:, :, :, 1, :], constant=1.0), writes=[('Vones',)])
            P.op('pool', 'memset', dict(ap=cq[:], constant=1.0), writes=[('cq',)])
            P.op('pool', 'memset', dict(ap=ck[:], constant=1.0), writes=[('ck',)])
            P.dma('sp', 'scr2', dict(out=cq[0:3, :, :], in_=scrQ[:, 4 * gp:4 * gp + 4, :]),
                  reads=[('scrQ', i) for i in range(3)] + [('cq',)], writes=[('cq',)])
            P.dma('sp', 'scr2', dict(out=ck[3:6, :, :], in_=scrK[:, 4 * gp:4 * gp + 4, :]),
                  reads=[('scrK', i) for i in range(3)] + [('ck',)], writes=[('ck',)])
            for j in range(2):
                E.proj_fm([(gp * 256 + j * 128, 128)], False,
                          E.evac_to(lambda tq, j=j: qT[:, j, tq * 512:(tq + 1) * 512],
                                    lambda tq, j=j: ('qT', j, tq), False))
                E.proj_fm([(1024 + gp * 256 + j * 128, 128)], False,
                          E.evac_to(lambda tq, j=j: kT[:, j, tq * 512:(tq + 1) * 512],
                                    lambda tq, j=j: ('kT', j, tq), False))
            for r in range(4):
                E.proj_tm(2048 + (4 * gp + r) * 64, 64, lambda k4, r=r: Vt[:, k4 * 4:(k4 + 1) * 4, r, 0, :],
                          ('Vt', r), wv)
            jobs = []
            for r in range(4):
                j, half = r // 2, r % 2
                rows = slice(64 * half, 64 * half + 64)
                qsrc = lambda c0, c1, rows=rows, j=j: (qT[rows, j, c0:c1], [('qT', j, c0 // 512)])
                ksrc = lambda kt, rows=rows, j=j: (kT[rows, j, kt * 128:(kt + 1) * 128], [('kT', j, kt // 4)])
                extra = lambda kt, c0, c1, r=r: (ck[0:6, r, kt * 128:(kt + 1) * 128], cq[0:6, r, c0:c1], [('cq',), ('ck',)])
                for qi in range(4):
                    tiles = E.band_tiles(qi, None, True)
                    bo = 3 + A.psO.next()
                    for n_, (kt, ca, cb, masks) in enumerate(tiles):
                        job = dict(
                            st=lambda qsrc=qsrc, ksrc=ksrc, extra=extra, kt=kt, qi=qi, ca=ca, cb=cb, masks=masks:
                            E.st_tile(qsrc, ksrc, kt, qi * 512, ca, cb, masks, extra),
                            ep=lambda bk, kt=kt, ca=ca, cb=cb, bo=bo, r=r, f=(n_ == 0), l=(n_ == len(tiles) - 1):
                            E.exp_pv(bk, ca, cb, Vt[:, kt, r, :, :].rearrange('p a d -> p (a d)'),
                                     [(('Vt', r), kt // 4), ('Vones',)], bo, f, l))
                        if n_ == len(tiles) - 1:
                            job['fin'] = (lambda bo=bo, rows=rows, j=j, qi=qi:
                                          finish_simple(P, C, A, E, bo, rows, 2 * gp + j, qi * 512, None))
                        jobs.append(job)
            run_jobs(jobs)
            P.barrier()


def nsa_seq(P, C, A, Ed):
    E = NS(Ed)
    nc, tn = C.nc, C.t
    slot = E.slot
    with ExitStack() as esL:
        sbL = lambda name, shape, dt: esL.enter_context(nc.sbuf_tensor(uniq(name), shape, dt))
        w1 = [sbL('n_w1%d' % i, [128, 16, 256], BF16) for i in range(2)]
        w2k = sbL('n_w2k', [128, 2, 128], BF16)
        w2v = sbL('n_w2v', [128, 2, 64], BF16)
        posf = sbL('n_posf', [128, 2, 16], F32)
        posS = sbL('n_posS', [128, 2, 16, 2], BF16)
        biasT = sbL('n_biasT', [128, 2, 2], F32)
        cmaskT = sbL('n_cmaskT', [128, T], BF16)
        cmask = sbL('n_cmask', [128, 8, 128], BF16)
        eexp = sbL('n_eexp', [32, T], BF16)
        bonus = sbL('n_bonus', [128, 8, 32], F32)
        gsel = sbL('n_gsel', [12, 768], BF16)
        load_w_cast(P, 'cstp', cmaskT[:], tn['c_cmaskT'], [('cmaskT',)])
        load_w_cast(P, 'cstp', cmask[:], tn['c_cmask'], [('cmask',)])
        load_w_cast(P, 'cstp', eexp[:], tn['c_eexp'], [('eexp',)])
        load_w_cast(P, 'cstp', gsel[:], tn['c_gsel'], [('gsel',)])
        P.dma('sp', 'cst', dict(out=bonus[:], in_=tn['c_bonus']), writes=[('bonus',)])
        for kv in range(2):
            for c0 in range(0, 16, 8):
                load_w_cast(P, 'wA', w1[kv][:, c0:c0 + 8, :],
                            tn['nsa_cmp_w1'][slot, kv, c0 * 128:(c0 + 8) * 128, :]
                            .rearrange('(c p) n -> p c n', p=128), [('w1', kv)])
            pr = tn['nsa_cmp_pos'][slot, kv].rearrange('(c two) d -> two d c', two=2)
            for two in range(2):
                P.dma('sp', 'cst', dict(out=posf[64 * two:64 * two + 64, kv, :], in_=pr[two],
                                        allow_slow_non_contiguous=True), writes=[('posf', kv, two)])
            for x in range(2):
                P.op('pool', 'tensor_copy', dict(out=posS[:, kv, :, x], in_=posf[:, kv, :]),
                     reads=[('posf', kv, 0), ('posf', kv, 1)], writes=[('posS', kv, x)])
        w2d = tn['nsa_cmp_w2'][slot]
        for dup in range(2):
            load_w_cast(P, 'wA', w2k[:, :, 64 * dup:64 * dup + 64], w2d[0].rearrange('(c p) n -> p c n', p=128),
                        [('w2k', dup)])
        load_w_cast(P, 'wA', w2v[:, :, :], w2d[1].rearrange('(c p) n -> p c n', p=128), [('w2v',)])
        for kv in range(2):
            for hc in range(2):
                for c2 in range(16):
                    P.op('pe', 'matmul', dict(out=C.ps[7][:, 0:2], lhsT=w1[kv][:, c2, hc * 128:(hc + 1) * 128],
                                              rhs=posS[:, kv, c2, :], start=(c2 == 0), stop=(c2 == 15)),
                         reads=[('w1', kv), ('posS', kv, 0), ('posS', kv, 1)], writes=[('ps', 7)])
                P.op('act', 'copy', dict(out=biasT[:, kv, hc:hc + 1], in_=C.ps[7][:, 0:1]),
                     reads=[('ps', 7)], writes=[('biasT', kv, hc)])
        P.barrier()

        for g in range(4):
            with ExitStack() as es2:
                sb = lambda name, shape, dt: es2.enter_context(nc.sbuf_tensor(uniq(name), shape, dt))
                qT = sb('n_qT', [128, 2, T], BF16)
                ksT = sb('n_ksT', [128, T], BF16)
                kwT = sb('n_kwT', [128, T], BF16)
                cS = [sb('n_cS%d' % i, [128, T], BF16) for i in range(2)]
                Vs = sb('n_Vs', [128, 16, 2, 64], BF16)
                Vw = sb('n_Vw', [128, 16, 2, 64], BF16)
                wv = sb('n_wv', [128, 8, 64], BF16)
                gTh = sb('n_gTh', [12, T], BF16)
                kcmpT = sb('n_kcmpT', [128, 128], BF16)
                Vc = sb('n_Vc', [128, 2, 64], BF16)
                hidT = [sb('n_hid%d' % i, [128, 2, 128], BF16) for i in range(2)]
                negselT = sb('n_negselT', [32, T], BF16)
                Pn = sb('n_Pn', [128, 4, 128], F32)
                Ps8 = sb('n_Ps8', [128, 8, 128], F32)
                imp8 = sb('n_imp8', [128, 8, 32], F32)
                sc2 = sb('n_sc2', [128, 32], F32)
                m1 = sb('n_m1', [128, 8], F32)
                m2 = sb('n_m2', [128, 8, 8], F32)
                den4 = sb('n_den4', [128, 8, 4], F32)
                negsel = sb('n_negsel', [128, 8, 32], BF16)
                acc = [sb('n_acc%d' % i, [64, 512], F32) for i in range(2)]
                ctmp = [sb('n_ctmp%d' % i, [64, 512], F32) for i in range(2)]
                P.op('pool', 'memset', dict(ap=Vs[:, :, 1, :], constant=1.0), writes=[('Vsones',)])
                P.op('pool', 'memset', dict(ap=Vw[:, :, 1, :], constant=1.0), writes=[('Vwones',)])
                P.op('pool', 'memset', dict(ap=Vc[:, 1, :], constant=1.0), writes=[('Vc1',)])
                P.op('pool', 'memset', dict(ap=Vc[:, 0, :], constant=0.0), writes=[('Vc0',)])
                P.op('pool', 'memset', dict(ap=kcmpT[:], constant=0.0), writes=[('kcmpT',)])
                for i in range(2):
                    P.op('pool', 'memset', dict(ap=hidT[i][:], constant=0.0), writes=[('hidT', i)])

                for j in range(2):
                    E.proj_fm([(g * 256 + j * 128, 128)], True,
                              E.evac_to(lambda tq, j=j: qT[:, j, tq * 512:(tq + 1) * 512],
                                        lambda tq, j=j: ('qT', j, tq), True))
                E.proj_fm([(1536 + g * 64, 64)] * 2, True,
                          E.evac_to(lambda tq: ksT[:, tq * 512:(tq + 1) * 512], lambda tq: ('ksT', tq), True))
                E.proj_fm([(2048 + g * 64, 64)] * 2, True,
                          E.evac_to(lambda tq: kwT[:, tq * 512:(tq + 1) * 512], lambda tq: ('kwT', tq), True))

                def evac_shift(i, rope):
                    def f(tq, src, skeys):
                        lo, hi = tq * 512, (tq + 1) * 512
                        if rope:
                            a, b = src
                            P.op('pool', 'tensor_tensor', dict(out=cS[i][0:64, lo:hi], in0=a[0:64, :], in1=b[0:64, :],
                                                               op=ALU.add), reads=skeys, writes=[('cS', i, tq, 0)])
                            if tq == 0:
                                P.op('pool', 'tensor_tensor', dict(out=cS[i][64:128, 0:511], in0=a[64:128, 1:512],
                                                                   in1=b[64:128, 1:512], op=ALU.add),
                                     reads=skeys, writes=[('cS', i, tq, 1)])
                            else:
                                P.op('pool', 'tensor_tensor', dict(out=cS[i][64:128, lo - 1:hi - 1], in0=a[64:128, :],
                                                                   in1=b[64:128, :], op=ALU.add),
                                     reads=skeys, writes=[('cS', i, tq, 1)])
                        else:
                            P.op('act', 'copy', dict(out=cS[i][0:64, lo:hi], in_=src[0:64, :]), reads=[skeys],
                                 writes=[('cS', i, tq, 0)])
                            if tq == 0:
                                P.op('act', 'copy', dict(out=cS[i][64:128, 0:511], in_=src[64:128, 1:512]),
                                     reads=[skeys], writes=[('cS', i, tq, 1)])
                            else:
                                P.op('act', 'copy', dict(out=cS[i][64:128, lo - 1:hi - 1], in_=src[64:128, :]),
                                     reads=[skeys], writes=[('cS', i, tq, 1)])
                    return f
                E.proj_fm([(1024 + g * 64, 64)] * 2, True, evac_shift(0, True))
                E.proj_fm([(1280 + g * 64, 64)] * 2, False, evac_shift(1, False))

                def evac_gate(tq, src, skey):
                    ri = A.rt.next()
                    gf = E.rtmp[2 * ri]
                    gk = ('rtmp', 2 * ri)
                    P.op('act', 'activation', dict(out=gf[0:12, :], in_=src, func=AF.Sigmoid), reads=[skey], writes=[gk])
                    P.op('dve', 'tensor_copy', dict(out=gTh[0:12, tq * 512:(tq + 1) * 512], in_=gf[0:12, :]),
                         reads=[gk], writes=[('gTh', tq)])
                E.proj_fm([(2560 + 12 * g, 12)], False, evac_gate)
                E.proj_tm(1792 + g * 64, 64, lambda k4: Vs[:, k4 * 4:(k4 + 1) * 4, 0, :], 'Vs', wv)
                E.proj_tm(2304 + g * 64, 64, lambda k4: Vw[:, k4 * 4:(k4 + 1) * 4, 0, :], 'Vw', wv)

                cSkeys = lambda i: [('cS', i, tq, x) for tq in range(4) for x in range(2)]
                for kv in range(2):
                    for hc in range(2):
                        for c2 in range(16):
                            P.op('pe', 'matmul',
                                 dict(out=C.ps[7][:, hc * 128:hc * 128 + 127], lhsT=w1[kv][:, c2, hc * 128:(hc + 1) * 128],
                                      rhs=cS[kv][:, 2 * c2:2 * c2 + 2017:16], start=(c2 == 0), stop=(c2 == 15),
                                      skip_group_check=True),
                                 reads=[('w1', kv)] + cSkeys(kv), writes=[('ps', 7)])
                        P.op('act', 'activation',
                             dict(out=hidT[kv][:, hc, 0:127], in_=C.ps[7][:, hc * 128:hc * 128 + 127], func=AF.Silu,
                                  bias=biasT[:, kv, hc:hc + 1], scale=1.0),
                             reads=[('ps', 7), ('biasT', kv, hc)], writes=[('hidT', kv)])
                for hc in range(2):
                    P.op('pe', 'matmul', dict(out=C.ps[6][:, 0:127], lhsT=w2k[:, hc, :], rhs=hidT[0][:, hc, 0:127],
                                              start=(hc == 0), stop=(hc == 1)),
                         reads=[('w2k', 0), ('w2k', 1), ('hidT', 0)], writes=[('ps', 6)])
                P.op('act', 'copy', dict(out=kcmpT[:, 0:127], in_=C.ps[6][:, 0:127]), reads=[('ps', 6)],
                     writes=[('kcmpT',)])
                for hc in range(2):
                    P.op('pe', 'matmul', dict(out=C.ps[6][0:127, 256:320], lhsT=hidT[1][:, hc, 0:127], rhs=w2v[:, hc, :],
                                              start=(hc == 0), stop=(hc == 1), skip_group_check=True),
                         reads=[('w2v',), ('hidT', 1)], writes=[('ps', 6)])
                P.op('act', 'copy', dict(out=Vc[0:127, 0, :], in_=C.ps[6][0:127, 256:320]), reads=[('ps', 6), ('Vc0',)],
                     writes=[('Vc0',)])

                for tt in range(8):
                    t0 = 1024 + tt * 128
                    for r in range(4):
                        j, half = r // 2, r % 2
                        rows = slice(64 * half, 64 * half + 64)
                        P.op('pe', 'matmul', dict(out=C.ps[5][:, r * 128:(r + 1) * 128], lhsT=qT[rows, j, t0:t0 + 128],
                                                  rhs=kcmpT[rows, :], start=True, stop=False, skip_group_check=True),
                             reads=[('qT', j, t0 // 512), ('kcmpT',)], writes=[('ps', 5)])
                        P.op('pe', 'matmul', dict(out=C.ps[5][:, r * 128:(r + 1) * 128], lhsT=C.ident[:],
                                                  rhs=cmask[:, tt, :], start=False, stop=True, skip_group_check=True),
                             reads=[('ident',), ('cmask',)], writes=[('ps', 5)])
                    for r in range(4):
                        P.op('act', 'activation', dict(out=Pn[:, r, :], in_=C.ps[5][:, r * 128:(r + 1) * 128],
                                                       func=AF.Exp, scale=0.125, accum_out=den4[:, tt, r:r + 1]),
                             reads=[('ps', 5)], writes=[('Pn', r), ('den4', tt, r)])
                    dk = [('den4', tt, r) for r in range(4)]
                    P.op('dve', 'tensor_scalar', dict(out=den4[:, tt, :], in0=den4[:, tt, :], scalar1=1e-30, scalar2=None,
                                                      op0=ALU.max), reads=dk, writes=[('den4', tt)])
                    P.op('dve', 'reciprocal', dict(out=den4[:, tt, :], in_=den4[:, tt, :]), reads=[('den4', tt)],
                         writes=[('den4', tt)])
                    P.op('dve', 'tensor_scalar', dict(out=Ps8[:, tt, :], in0=Pn[:, 0, :], scalar1=den4[:, tt, 0:1],
                                                      scalar2=None, op0=ALU.mult),
                         reads=[('Pn', 0), ('den4', tt)], writes=[('Ps8', tt)])
                    for r in range(1, 4):
                        P.op('dve', 'scalar_tensor_tensor',
                             dict(out=Ps8[:, tt, :], in0=Pn[:, r, :], scalar=den4[:, tt, r:r + 1], in1=Ps8[:, tt, :],
                                  op0=ALU.mult, op1=ALU.add),
                             reads=[('Pn', r), ('den4', tt), ('Ps8', tt)], writes=[('Ps8', tt)])
                pk = [('Ps8', tt) for tt in range(8)]
                Pv = Ps8[:].rearrange('p t (n i) -> p t n i', i=4)
                P.op('dve', 'tensor_tensor', dict(out=imp8[:], in0=Pv[:, :, :, 0], in1=Pv[:, :, :, 1], op=ALU.add),
                     reads=pk, writes=[('imp8',)])
                P.op('dve', 'tensor_tensor', dict(out=imp8[:], in0=imp8[:], in1=Pv[:, :, :, 2], op=ALU.add),
                     reads=pk + [('imp8',)], writes=[('imp8',)])
                P.op('dve', 'scalar_tensor_tensor', dict(out=imp8[:], in0=Pv[:, :, :, 3], scalar=0.5, in1=imp8[:],
                                                         op0=ALU.mult, op1=ALU.add),
                     reads=pk + [('imp8',)], writes=[('imp8',)])
                P.op('dve', 'scalar_tensor_tensor', dict(out=imp8[:, :, 1:32], in0=Pv[:, :, 0:31, 3], scalar=0.5,
                                                         in1=imp8[:, :, 1:32], op0=ALU.mult, op1=ALU.add),
                     reads=pk + [('imp8',)], writes=[('imp8',)])
                P.op('dve', 'tensor_tensor', dict(out=imp8[:], in0=imp8[:], in1=bonus[:], op=ALU.add),
                     reads=[('imp8',), ('bonus',)], writes=[('imp8',)])
                for tt in range(8):
                    P.op('dve', 'max', dict(out=m1[:], in_=imp8[:, tt, :]), reads=[('imp8',)], writes=[('m1',)])
                    P.op('dve', 'match_replace', dict(out=sc2[:], in_to_replace=m1[:], in_values=imp8[:, tt, :],
                                                      imm_value=-1e30), reads=[('m1',), ('imp8',)], writes=[('sc2',)])
                    P.op('dve', 'max', dict(out=m2[:, tt, :], in_=sc2[:]), reads=[('sc2',)], writes=[('m2', tt)])
                    P.op('dve', 'tensor_scalar', dict(out=negsel[:, tt, :], in0=imp8[:, tt, :], scalar1=m2[:, tt, 7:8],
                                                      scalar2=NEG, op0=ALU.is_lt, op1=ALU.mult),
                         reads=[('imp8',), ('m2', tt)], writes=[('negsel', tt)])
                    P.op('pe', 'transpose', dict(out=C.ps_bf[6][0:32, tt, :], in_=negsel[:, tt, :], identity=C.ident[:]),
                         reads=[('negsel', tt), ('ident',)], writes=[('ps', 6)])
                P.op('act', 'copy', dict(out=negselT[0:32, 1024:2048],
                                         in_=C.ps_bf[6][0:32, :, :].rearrange('p a b -> p (a b)')),
                     reads=[('ps', 6)], writes=[('negselT',)])

                jobs = []
                first_gate = [True]

                def gate_pre(r, qi, par):
                    q0 = qi * 512
                    gk = [('gTh', qi), ('gsel',)]
                    b01 = 5 if par == 0 else 7
                    P.op('pe', 'matmul', dict(out=C.ps[b01][:, :], lhsT=gsel[0:12, (3 * r) * 64:(3 * r + 2) * 64],
                                              rhs=gTh[0:12, q0:q0 + 512], start=True, stop=True),
                         reads=gk, writes=[('ps', b01)])
                    wk = [('ps6h', par)]
                    if first_gate[0]:
                        wk = [('ps', 6), ('ps6h', 0), ('ps6h', 1)]
                        first_gate[0] = False
                    P.op('pe', 'matmul', dict(out=C.ps[6][64 * par:64 * par + 64, :],
                                              lhsT=gsel[0:12, (3 * r + 2) * 64:(3 * r + 3) * 64],
                                              rhs=gTh[0:12, q0:q0 + 512], start=True, stop=True,
                                              skip_group_check=True),
                         reads=gk, writes=wk)

                def combine(b, bo, r, qi, par, rows, j):
                    q0 = qi * 512
                    b01 = 5 if par == 0 else 7
                    gap = [C.ps[b01][0:64, :], C.ps[b01][64:128, :], C.ps[6][64 * par:64 * par + 64, :]][b]
                    gkey = [('ps', b01), ('ps', b01), ('ps6h', par)][b]
                    ai = par
                    fi = A.fd.next()
                    fd = E.fden[fi]
                    fk = ('fden', fi)
                    if b == 0:
                        P.op('dve', 'tensor_scalar', dict(out=fd[0:64, :], in0=C.ps[bo][64:128, :], scalar1=1e-30,
                                                          scalar2=None, op0=ALU.max),
                             reads=[('ps', bo)], writes=[fk])
                        P.op('dve', 'reciprocal', dict(out=fd[0:64, :], in_=fd[0:64, :]), reads=[fk], writes=[fk])
                    else:
                        P.op('dve', 'reciprocal', dict(out=fd[0:64, :], in_=C.ps[bo][64:128, :]),
                             reads=[('ps', bo)], writes=[fk])
                    P.op('dve', 'tensor_tensor', dict(out=fd[0:64, :], in0=gap, in1=fd[0:64, :], op=ALU.mult),
                         reads=[gkey, fk], writes=[fk])
                    if b == 0:
                        P.op('dve', 'tensor_tensor', dict(out=acc[ai][:], in0=C.ps[bo][0:64, :], in1=fd[0:64, :],
                                                          op=ALU.mult),
                             reads=[('ps', bo), fk], writes=[('acc', ai)])
                    else:
                        ci = b - 1
                        P.op('dve', 'tensor_tensor', dict(out=ctmp[ci][:], in0=C.ps[bo][0:64, :], in1=fd[0:64, :],
                                                          op=ALU.mult),
                             reads=[('ps', bo), fk], writes=[('ctmp', ci)])
                        if b == 1:
                            P.op('pool', 'tensor_tensor', dict(out=acc[ai][:], in0=acc[ai][:], in1=ctmp[ci][:],
                                                               op=ALU.add),
                                 reads=[('acc', ai), ('ctmp', ci)], writes=[('acc', ai)])
                        else:
                            P.op('pool', 'tensor_tensor',
                                 dict(out=E.OT[rows, 2 * g + j, q0:q0 + 512], in0=acc[ai][:], in1=ctmp[ci][:],
                                      op=ALU.add),
                                 reads=[('acc', ai), ('ctmp', ci)], writes=[('OT', 2 * g + j, qi)])

                for r in range(4):
                    j, half = r // 2, r % 2
                    rows = slice(64 * half, 64 * half + 64)
                    qsrc = lambda c0, c1, rows=rows, j=j: (qT[rows, j, c0:c1], [('qT', j, c0 // 512)])
                    for qi in range(4):
                        q0 = qi * 512
                        par = (r * 4 + qi) % 2
                        for b in range(3):
                            bo = 3 + A.psO.next()
                            if b == 0:
                                tiles = [(0, 0, 512, [])]
                                ksrc = lambda kt, rows=rows: (kcmpT[rows, :], [('kcmpT',)])
                                extra = lambda kt, c0, c1: (C.ident[:], cmaskT[:, c0:c1], [('ident',), ('cmaskT',)])
                                vfn = lambda kt: (Vc[:, :, :].rearrange('p a d -> p (a d)'), [('Vc0',), ('Vc1',)])
                            elif b == 1:
                                tiles = E.band_tiles(qi, None, True)
                                extra = None
                                if qi >= 2:
                                    extra = lambda kt, c0, c1: (eexp[0:32, kt * 128:(kt + 1) * 128], negselT[0:32, c0:c1],
                                                                [('eexp',), ('negselT',)])
                                ksrc = lambda kt, rows=rows: (ksT[rows, kt * 128:(kt + 1) * 128], [('ksT', kt // 4)])
                                vfn = lambda kt: (Vs[:, kt, :, :].rearrange('p a d -> p (a d)'),
                                                  [('Vs', kt // 4), ('Vsones',)])
                            else:
                                tiles = E.band_tiles(qi, 4, False)
                                extra = None
                                ksrc = lambda kt, rows=rows: (kwT[rows, kt * 128:(kt + 1) * 128], [('kwT', kt // 4)])
                                vfn = lambda kt: (Vw[:, kt, :, :].rearrange('p a d -> p (a d)'),
                                                  [('Vw', kt // 4), ('Vwones',)])
                            for n_, (kt, ca, cb, masks) in enumerate(tiles):
                                job = dict(
                                    st=lambda qsrc=qsrc, ksrc=ksrc, extra=extra, kt=kt, q0=q0, ca=ca, cb=cb, masks=masks:
                                    E.st_tile(qsrc, ksrc, kt, q0, ca, cb, masks, extra),
                                    ep=lambda bk, vfn=vfn, kt=kt, ca=ca, cb=cb, bo=bo, f=(n_ == 0), l=(n_ == len(tiles) - 1):
                                    E.exp_pv(bk, ca, cb, vfn(kt)[0], vfn(kt)[1], bo, f, l))
                                if b == 0 and n_ == 0:
                                    job['pre'] = lambda r=r, qi=qi, par=par: gate_pre(r, qi, par)
                                if n_ == len(tiles) - 1:
                                    job['fin'] = (lambda b=b, bo=bo, r=r, qi=qi, par=par, rows=rows, j=j:
                                                  combine(b, bo, r, qi, par, rows, j))
                                jobs.append(job)
                run_jobs(jobs)
                P.barrier()


def build_program(layers=(0, 1, 2, 3), do_attn=True, do_mlp=True):
    nc = bass.Bass("TRN2", target_bir_lowering=False)
    C = Ctx()
    C.nc = nc
    C.t = declare_inputs(nc)
    for name, shp in CONST_SHAPES.items():
        C.t[name] = nc.dram_tensor(name, list(shp), F32, kind="ExternalInput").ap()
    C.scrQ = nc.dram_tensor('scrQ', [3, 16, T], BF16, kind="Internal").ap()
    C.scrK = nc.dram_tensor('scrK', [3, 16, T], BF16, kind="Internal").ap()
    with ExitStack() as es:
        P = Prog(nc, es)
        C.P = P
        C.ps = [es.enter_context(nc.psum_tensor('ps%d' % i, [128, 512], F32)) for i in range(8)]
        C.ps_bf = [C.ps[i][:].bitcast(BF16).rearrange('p (c t) -> p c t', c=8) for i in range(8)]
        C.ident = es.enter_context(nc.sbuf_tensor('ident', [128, 128], BF16))
        C.identf = es.enter_context(nc.sbuf_tensor('identf', [128, 128], F32))
        C.eps = es.enter_context(nc.sbuf_tensor('eps', [128, 1], F32))
        C.one = es.enter_context(nc.sbuf_tensor('one', [128, 1], F32))
        P.op('pool', 'memset', dict(ap=C.identf[:], constant=0.0), writes=[('identf',)])
        P.op('pool', 'affine_select',
             dict(out=C.identf[:], in_=C.identf[:], pattern=[[-1, 128]], compare_op=ALU.not_equal,
                  fill=1.0, base=0, channel_multiplier=1),
             reads=[('identf',)], writes=[('identf',)])
        P.op('pool', 'tensor_copy', dict(out=C.ident[:], in_=C.identf[:]), reads=[('identf',)], writes=[('ident',)])
        P.op('pool', 'memset', dict(ap=C.eps[:], constant=EPS), writes=[('eps',)])
        P.op('pool', 'memset', dict(ap=C.one[:], constant=1.0), writes=[('one',)])
        P.barrier()
        src = C.t['x']
        for L in layers:
            if do_attn:
                attn_phase(P, C, L, src)
                src = C.t['y']
            if do_mlp:
                mlp_phase(P, C, L, src)
                src = C.t['y']
        P.barrier()
        P.emit()
    return nc, P


_CONSTS = None


def kernel(**inputs):
    global _CONSTS
    if _CONSTS is None:
        _CONSTS = host_consts()
    nc, P = build_program()
    x = np.ascontiguousarray(np.asarray(inputs['x'], dtype=np.float32))
    shared = {k: np.ascontiguousarray(np.asarray(v, dtype=np.float32)) for k, v in inputs.items() if k != 'x'}
    shared.update(_CONSTS)
    in_maps = []
    for c in range(NCORES):
        m = dict(shared)
        m['x'] = x[c * SEQ_PER_CORE:(c + 1) * SEQ_PER_CORE].reshape(NTOK, D)
        in_maps.append(m)
    res = run_bass_kernel_spmd(nc, in_maps, core_ids=list(range(NCORES)))
    out = np.stack([np.asarray(r['y']).reshape(SEQ_PER_CORE, T, D) for r in res.results], axis=0)
    return out.reshape(NCORES * SEQ_PER_CORE, T, D).astype(np.float32)
```

```python
import numpy as np
from contextlib import ExitStack
import concourse.bass as bass
import concourse.mybir as mybir
from concourse.bass_utils import run_bass_kernel_spmd

F32 = mybir.dt.float32
BF16 = mybir.dt.bfloat16
AF = mybir.ActivationFunctionType
ALU = mybir.AluOpType
AX = mybir.AxisListType

NCORES = 8
D = 1024
T = 2048
SEQ_PER_CORE = 2
NTOK = T * SEQ_PER_CORE
DFF = 4096
H = 16
DH = 64
EPS = 1e-6
NEG = -30000.0


_UNIQ = [0]


def uniq(name):
    _UNIQ[0] += 1
    return '%s_%d' % (name, _UNIQ[0])


class Chan:
    def __init__(self, sem):
        self.sem = sem
        self.count = 0


class Prog:
    def __init__(self, nc, es):
        self.nc = nc
        self.es = es
        self.names = ('pe', 'act', 'dve', 'pool', 'sp')
        self.ins = {e: [] for e in self.names}
        self.res = {}
        self.known = {e: {} for e in self.names}
        self.chans = {}
        self.epoch = 0
        self.KS = 4
        self.RS = 4096
        self.esem = {e: [self.es.enter_context(self.nc.semaphore('s_%s_%d' % (e, k))) for k in range(self.KS)]
                     for e in self.names}

    def chan(self, name):
        c = self.chans.get(name)
        if c is None:
            c = Chan(self.es.enter_context(self.nc.semaphore('c_' + name)))
            self.chans[name] = c
        return c

    def _collect(self, eng, reads, writes):
        need = {}

        def add(src, idx):
            if src[0] == 'd':
                idx = self.chans[src[1]].count
            if need.get(src, -1) < idx:
                need[src] = idx

        for k in reads:
            st = self.res.get(k)
            if st is not None and st[0] is not None:
                add(*st[0])
        for k in writes:
            st = self.res.get(k)
            if st is not None:
                if st[0] is not None:
                    add(*st[0])
                for src, idx in st[1].items():
                    add(src, idx)
        waits = []
        kn = self.known[eng]
        for src, idx in need.items():
            if eng == 'pe' and src == ('e', 'pe'):
                continue
            if kn.get(src, -1) >= idx:
                continue
            kn[src] = idx
            waits.append((src, idx))
            if src[0] == 'e':
                self.ins[src[1]][idx]['sig'] = True
        return waits

    def _update(self, ev, reads, writes):
        for k in reads:
            st = self.res.get(k)
            if st is None:
                st = [None, {}]
                self.res[k] = st
            st[1][ev[0]] = ev[1]
        for k in writes:
            self.res[k] = [ev, {}]

    def op(self, eng, method, kw, reads=(), writes=()):
        waits = self._collect(eng, reads, writes)
        idx = len(self.ins[eng])
        self.ins[eng].append(dict(m=method, kw=kw, waits=waits, sig=False, dma=None, ep=self.epoch))
        self._update((('e', eng), idx), reads, writes)

    def dma(self, q, ch, kw, reads=(), writes=()):
        c = self.chan(ch)
        waits = self._collect(q, reads, writes)
        c.count += 1
        self.ins[q].append(dict(m='dma_start', kw=kw, waits=waits, sig=False, dma=c.sem, ep=self.epoch))
        self._update((('d', ch), c.count), reads, writes)

    def barrier(self):
        last = {}
        for e in self.names:
            for i in range(len(self.ins[e]) - 1, -1, -1):
                it = self.ins[e][i]
                if it['ep'] != self.epoch:
                    break
                if it['dma'] is None and it['m'] != 'wait_only':
                    last[e] = i
                    it['sig'] = True
                    break
        for e in self.names:
            waits = []
            for y, i in last.items():
                if e == 'pe' and y == 'pe':
                    continue
                waits.append((('e', y), i))
            for name, c in self.chans.items():
                if c.count > 0:
                    waits.append((('d', name), c.count))
            self.ins[e].append(dict(m='wait_only', kw=None, waits=waits, sig=False, dma=None, ep=self.epoch))
        self.res = {}
        self.known = {e: {} for e in self.names}
        self.epoch += 1

    def emit(self):
        cnt = {}
        for e in self.names:
            n = 0
            arr = []
            for it in self.ins[e]:
                if it['sig']:
                    b = n // self.RS
                    arr.append((b % self.KS, (b // self.KS) * self.RS + (n % self.RS) + 1))
                    n += 1
                else:
                    arr.append(None)
            cnt[e] = arr
        stats = {e: (len(self.ins[e]), sum(len(it['waits']) for it in self.ins[e])) for e in self.names}
        self.stats = stats

        def mk(e):
            def body(engobj):
                for idx_, it in enumerate(self.ins[e]):
                    for (src, idx) in it['waits']:
                        if src[0] == 'e':
                            y = src[1]
                            k, v = cnt[y][idx]
                            engobj.wait_ge(self.esem[y][k], v)
                        else:
                            engobj.wait_ge(self.chans[src[1]].sem, 16 * idx)
                    if it['m'] == 'wait_only':
                        continue
                    r = getattr(engobj, it['m'])(**it['kw'])
                    if it['dma'] is not None:
                        r.then_inc(it['dma'], 16)
                    elif it['sig']:
                        r.then_inc(self.esem[e][cnt[e][idx_][0]], 1)
            return body

        with self.nc.Block() as block:
            block.tensor(mk('pe'))
            block.scalar(mk('act'))
            block.vector(mk('dve'))
            block.gpsimd(mk('pool'))
            block.sync(mk('sp'))


class Ctx:
    pass


def declare_inputs(nc):
    t = {}

    def inp(name, shape):
        t[name] = nc.dram_tensor(name, list(shape), F32, kind="ExternalInput").ap()

    inp('x', (NTOK, D))
    inp('norm_g', (4, 4, D))
    inp('mlp_w_up', (4, D, DFF))
    inp('mlp_w_down', (4, DFF, D))
    inp('nsa_w_in', (2, D, 2608))
    inp('nsa_cmp_pos', (2, 2, 32, 64))
    inp('nsa_cmp_w1', (2, 2, 2048, 256))
    inp('nsa_cmp_w2', (2, 2, 256, 64))
    inp('nsa_w_out', (2, D, D))
    inp('fox_w_in', (1, D, 3088))
    inp('fox_b_f', (1, 16))
    inp('fox_w_out', (1, D, D))
    inp('swa_w_in', (1, D, 1280))
    inp('swa_b_in', (1, 1280))
    inp('swa_sinks', (1, 16))
    inp('swa_w_out', (1, D, D))
    inp('swa_b_out', (1, D))
    t['y'] = nc.dram_tensor('y', [NTOK, D], F32, kind="ExternalOutput").ap()
    return t


def load_w_cast(P, ch, out_ap, in_ap, writes):
    P.dma('pool', ch, dict(out=out_ap, in_=in_ap, max_dma_last_dim=4096), reads=(), writes=writes)


def rstd_from_ss(P, C, ss, rstd, n, key_ss, key_rstd):
    P.op('act', 'activation', dict(out=rstd, in_=ss, func=AF.Ln, scale=1.0 / D, bias=C.eps[:, 0:1]),
         reads=[key_ss], writes=[key_rstd])
    P.op('act', 'activation', dict(out=rstd, in_=rstd, func=AF.Exp, scale=-0.5),
         reads=[key_rstd], writes=[key_rstd])


def mlp_phase(P, C, L, ysrc):
    nc = C.nc
    tn = C.t
    TT = 256
    NS = TT // 128
    ntiles = NTOK // TT
    with ExitStack() as es:
        sb = lambda name, shape, dt: es.enter_context(nc.sbuf_tensor(uniq(name), shape, dt))
        wup = sb('wup', [128, 8, DFF], BF16)
        wdn = sb('wdn', [128, 32, D], BF16)
        g3 = sb('g3', [128, D], F32)
        g4 = sb('g4', [128, D], F32)
        xts = [sb('xt%d' % i, [128, NS, D], F32) for i in range(2)]
        hb = [sb('hb%d' % i, [128, D], BF16) for i in range(2)]
        hT = [sb('hT%d' % i, [128, 8, TT], BF16) for i in range(2)]
        aT = sb('aT', [128, 32, TT], BF16)
        rl = [sb('rl%d' % i, [128, TT], F32) for i in range(2)]
        junk = sb('junk', [128, D], BF16)
        tmp = [sb('tmp%d' % i, [128, D], F32) for i in range(2)]
        ss = [sb('ss%d' % i, [128, 4], F32) for i in range(2)]
        rs = [sb('rs%d' % i, [128, 4], F32) for i in range(2)]
        ss4 = [sb('ss4%d' % i, [128, 4], F32) for i in range(2)]
        rs4 = [sb('rs4%d' % i, [128, 4], F32) for i in range(2)]

        for k in range(8):
            for hf in range(2):
                load_w_cast(P, 'wA', wup[:, k, hf * 2048:(hf + 1) * 2048],
                            tn['mlp_w_up'][L, k * 128:(k + 1) * 128, hf * 2048:(hf + 1) * 2048],
                            writes=[('wup', k)])
        for c0 in range(0, 32, 4):
            load_w_cast(P, 'wB', wdn[:, c0:c0 + 4, :],
                        tn['mlp_w_down'][L, c0 * 128:(c0 + 4) * 128, :].rearrange('(c p) n -> p c n', p=128),
                        writes=[('wdn', c0 // 4)])
        P.dma('sp', 'g', dict(out=g3[:], in_=tn['norm_g'][L, 2, :].partition_broadcast(128)), writes=[('g3',)])
        P.dma('sp', 'g', dict(out=g4[:], in_=tn['norm_g'][L, 3, :].partition_broadcast(128)), writes=[('g4',)])

        def load_x(ti):
            sl = ti % 2
            P.dma('sp', 'x%d' % sl,
                  dict(out=xts[sl][:], in_=ysrc[ti * TT:(ti + 1) * TT, :].rearrange('(s p) d -> p s d', p=128)),
                  reads=[('y', ti)], writes=[('xt', sl)])

        load_x(0)
        for ti in range(ntiles):
            sl = ti % 2
            xt = xts[sl]
            if ti + 1 < ntiles:
                load_x(ti + 1)
            for s in range(NS):
                P.op('act', 'activation', dict(out=junk[:], in_=xt[:, s, :], func=AF.Square,
                                               accum_out=ss[sl][:, s:s + 1]),
                     reads=[('xt', sl)], writes=[('junk',), ('ss', sl)])
            rstd_from_ss(P, C, ss[sl][:, 0:NS], rs[sl][:, 0:NS], NS, ('ss', sl), ('rs', sl))
            for s in range(NS):
                hs = (ti * NS + s) % 2
                P.op('dve', 'scalar_tensor_tensor',
                     dict(out=hb[hs][:], in0=xt[:, s, :], scalar=rs[sl][:, s:s + 1], in1=g3[:],
                          op0=ALU.mult, op1=ALU.mult),
                     reads=[('xt', sl), ('rs', sl), ('g3',)], writes=[('hb', hs)])
                pb = C.ps_bf[hs]
                for c in range(8):
                    P.op('pe', 'transpose', dict(out=pb[:, c, :], in_=hb[hs][:, c * 128:(c + 1) * 128],
                                                 identity=C.ident[:]),
                         reads=[('hb', hs), ('ident',)], writes=[('ps', hs)])
                P.op('act', 'copy', dict(out=hT[sl][:, :, s * 128:(s + 1) * 128], in_=pb[:, :, :]),
                     reads=[('ps', hs)], writes=[('hT', sl)])
            for fc in range(32):
                bk = 2 + (fc % 2)
                for k in range(8):
                    P.op('pe', 'matmul', dict(out=C.ps[bk][:, 0:TT], lhsT=wup[:, k, fc * 128:(fc + 1) * 128],
                                              rhs=hT[sl][:, k, :], start=(k == 0), stop=(k == 7)),
                         reads=[('wup', k), ('hT', sl)], writes=[('ps', bk)])
                r = rl[fc % 2]
                P.op('act', 'activation', dict(out=r[:], in_=C.ps[bk][:, 0:TT], func=AF.Relu),
                     reads=[('ps', bk)], writes=[('rl', fc % 2)])
                P.op('dve', 'tensor_tensor', dict(out=aT[:, fc, :], in0=r[:], in1=r[:], op=ALU.mult),
                     reads=[('rl', fc % 2)], writes=[('aT', fc)])
            for s in range(NS):
                for dh in range(2):
                    bk = 4 + dh
                    for fc in range(32):
                        P.op('pe', 'matmul',
                             dict(out=C.ps[bk][:, :], lhsT=aT[:, fc, s * 128:(s + 1) * 128],
                                  rhs=wdn[:, fc, dh * 512:(dh + 1) * 512], start=(fc == 0), stop=(fc == 31)),
                             reads=[('aT', fc), ('wdn', fc // 4)], writes=[('ps', bk)])
                    P.op('act', 'activation', dict(out=junk[:, 0:512], in_=C.ps[bk][:, :], func=AF.Square,
                                                   accum_out=ss4[sl][:, 2 * s + dh:2 * s + dh + 1]),
                         reads=[('ps', bk)], writes=[('junk',), ('ss4', sl, s, dh)])
                P.op('dve', 'tensor_tensor', dict(out=ss4[sl][:, 2 * s:2 * s + 1], in0=ss4[sl][:, 2 * s:2 * s + 1],
                                                  in1=ss4[sl][:, 2 * s + 1:2 * s + 2], op=ALU.add),
                     reads=[('ss4', sl, s, 0), ('ss4', sl, s, 1)], writes=[('ss4', sl, s, 0)])
                rstd_from_ss(P, C, ss4[sl][:, 2 * s:2 * s + 1], rs4[sl][:, s:s + 1], 1,
                             ('ss4', sl, s, 0), ('rs4', sl, s))
                tm = tmp[s % 2]
                for dh in range(2):
                    bk = 4 + dh
                    P.op('dve', 'scalar_tensor_tensor',
                         dict(out=tm[:, dh * 512:(dh + 1) * 512], in0=C.ps[bk][:, :], scalar=rs4[sl][:, s:s + 1],
                              in1=g4[:, dh * 512:(dh + 1) * 512], op0=ALU.mult, op1=ALU.mult),
                         reads=[('ps', bk), ('rs4', sl, s), ('g4',)], writes=[('tmp', s % 2, dh)])
                P.op('pool', 'tensor_tensor', dict(out=xt[:, s, :], in0=xt[:, s, :], in1=tm[:], op=ALU.add),
                     reads=[('xt', sl), ('tmp', s % 2, 0), ('tmp', s % 2, 1)], writes=[('xt', sl)])
            P.dma('sp', 'yo%d' % sl,
                  dict(out=C.t['y'][ti * TT:(ti + 1) * TT, :].rearrange('(s p) d -> p s d', p=128), in_=xt[:]),
                  reads=[('xt', sl)], writes=[('y', ti)])
        P.barrier()


CONST_SHAPES = {
    'c_rope': (2, 128, T),
    'c_maskC': (128, 128),
    'c_maskW': (128, 128),
    'c_cmaskT': (128, T),
    'c_cmask': (128, 8, 128),
    'c_eexp': (32, T),
    'c_bonus': (128, 8, 32),
    'c_gsel': (12, 768),
}


def host_consts():
    c = {}
    inv = (np.float32(10000.0) ** (-np.arange(0, DH, 2, dtype=np.float32) / np.float32(DH))).astype(np.float32)
    ang = (np.arange(T, dtype=np.float32)[:, None] * inv[None, :]).astype(np.float32)
    cos = np.cos(ang).astype(np.float32).T
    sin = np.sin(ang).astype(np.float32).T
    c['c_rope'] = np.stack([np.tile(cos, (4, 1)), np.tile(sin, (4, 1))]).astype(np.float32)
    s = np.arange(128)[:, None]
    t = np.arange(128)[None, :]
    c['c_maskC'] = np.where(t >= s, 0.0, NEG).astype(np.float32)
    c['c_maskW'] = np.where(t < s, 0.0, NEG).astype(np.float32)
    cc = np.arange(128)[:, None]
    tt = np.arange(T)[None, :]
    c['c_cmaskT'] = np.where(16 * cc + 31 <= tt, 0.0, NEG).astype(np.float32)
    c['c_cmaskT'][127, :] = -332.0
    tq = 1024 + np.arange(8)[None, :, None] * 128 + np.arange(128)[:, None, None]
    c['c_cmask'] = np.where(16 * np.arange(128)[None, None, :] + 31 <= tq, 0.0, NEG).astype(np.float32)
    c['c_eexp'] = (np.arange(T)[None, :] // 64 == np.arange(32)[:, None]).astype(np.float32)
    tblk = tq // 64
    n = np.arange(32)[None, None, :]
    forced = (n == 0) | (n == tblk) | (n == tblk - 1)
    c['c_bonus'] = np.where(n <= tblk, np.where(forced, 1000.0, 0.0), -1.0).astype(np.float32)
    c['c_gsel'] = (np.arange(768)[None, :] // 64 == np.arange(12)[:, None]).astype(np.float32)
    return c


class Rot:
    def __init__(self, n):
        self.n = n
        self.i = 0

    def next(self):
        v = self.i % self.n
        self.i += 1
        return v


def attn_phase(P, C, L, ysrc):
    nc = C.nc
    tn = C.t
    kind = L % 3
    slot = L // 3
    if kind == 0:
        win, wout_d = tn['nsa_w_in'][slot], tn['nsa_w_out'][slot]
    elif kind == 1:
        win, wout_d = tn['fox_w_in'][slot], tn['fox_w_out'][slot]
    else:
        win, wout_d = tn['swa_w_in'][slot], tn['swa_w_out'][slot]
    ydst = tn['y']

    with ExitStack() as es:
        sb = lambda name, shape, dt: es.enter_context(nc.sbuf_tensor(uniq(name), shape, dt))
        hT = sb('a_hT', [128, 8, T], BF16)
        OT = sb('a_OT', [128, 8, T], BF16)
        maskC = sb('a_maskC', [128, 128], BF16)
        maskW = sb('a_maskW', [128, 128], BF16)
        ones_row = sb('a_ones', [1, 512], BF16)
        wch = [sb('a_wch%d' % i, [128, 8, 128], BF16) for i in range(2)]
        wrot = [sb('a_wrot%d' % i, [128, 8, 128], BF16) for i in range(2)]
        bch = [sb('a_bch%d' % i, [1, 128], BF16) for i in range(2)]
        brot = [sb('a_brot%d' % i, [1, 128], BF16) for i in range(2)]
        rtmp = [sb('a_rtmp%d' % i, [128, 512], F32) for i in range(4)]
        pT = [sb('a_pT%d' % i, [128, 512], BF16) for i in range(3)]
        fden = [sb('a_fden%d' % i, [128, 512], F32) for i in range(2)]
        if kind != 1:
            ropeC = sb('a_ropeC', [128, T], F32)
            ropeS = sb('a_ropeS', [128, T], F32)
            P.dma('sp', 'cst', dict(out=ropeC[:], in_=tn['c_rope'][0]), writes=[('ropeC',)])
            P.dma('sp', 'cst', dict(out=ropeS[:], in_=tn['c_rope'][1]), writes=[('ropeS',)])
        load_w_cast(P, 'cstp', maskC[:], tn['c_maskC'], [('maskC',)])
        load_w_cast(P, 'cstp', maskW[:], tn['c_maskW'], [('maskW',)])
        P.op('pool', 'memset', dict(ap=ones_row[:], constant=1.0), writes=[('ones_row',)])
        A = Ctx()
        A.jobrot = Rot(2)
        A.psA = Rot(2)
        A.rt = Rot(2)
        A.ptr = Rot(3)
        A.psS = Rot(3)
        A.psO = Rot(2)
        A.fd = Rot(2)

        def load_chunk(segs, bias_d):
            bi = A.jobrot.next()
            off = 0
            for (c0, n) in segs:
                load_w_cast(P, 'wc%d' % bi, wch[bi][:, :, off:off + n],
                            win[:, c0:c0 + n].rearrange('(k p) n -> p k n', p=128), writes=[('wch', bi)])
                if bias_d is not None:
                    load_w_cast(P, 'wc%d' % bi, bch[bi][0:1, off:off + n], bias_d(c0, n).unsqueeze(0),
                                writes=[('bch', bi)])
                off += n
            return bi, off

        def make_rot(bi, rows, has_bias):
            nb = rows // 64
            v = wch[bi][:, :, 0:rows].rearrange('p k (b two r) -> p k b two r', two=2, r=32)
            w = wrot[bi][:, :, 0:rows].rearrange('p k (b two r) -> p k b two r', two=2, r=32)
            for b in range(nb):
                P.op('pool', 'tensor_scalar', dict(out=w[:, :, b, 0, :], in0=v[:, :, b, 1, :], scalar1=-1.0,
                                                   scalar2=None, op0=ALU.mult),
                     reads=[('wch', bi)], writes=[('wrot', bi, b, 0)])
                P.op('pool', 'tensor_copy', dict(out=w[:, :, b, 1, :], in_=v[:, :, b, 0, :]),
                     reads=[('wch', bi)], writes=[('wrot', bi, b, 1)])
            if has_bias:
                v = bch[bi][0:1, 0:rows].rearrange('p (b two r) -> p b two r', two=2, r=32)
                w = brot[bi][0:1, 0:rows].rearrange('p (b two r) -> p b two r', two=2, r=32)
                P.op('pool', 'tensor_scalar', dict(out=w[:, :, 0, :], in0=v[:, :, 1, :], scalar1=-1.0,
                                                   scalar2=None, op0=ALU.mult),
                     reads=[('bch', bi)], writes=[('brot', bi, 0)])
                P.op('pool', 'tensor_copy', dict(out=w[:, :, 1, :], in_=v[:, :, 0, :]),
                     reads=[('bch', bi)], writes=[('brot', bi, 1)])

        def proj_fm(segs, rope, evac, bias_d=None):
            bi, rows = load_chunk(segs, bias_d)
            if rope:
                make_rot(bi, rows, bias_d is not None)
            rotkeys = [('wrot', bi, b, x) for b in range(rows // 64) for x in range(2)]
            for tq in range(4):
                bkA = 2 + A.psA.next()
                for k in range(8):
                    P.op('pe', 'matmul', dict(out=C.ps[bkA][0:rows, :], lhsT=wch[bi][:, k, 0:rows],
                                              rhs=hT[:, k, tq * 512:(tq + 1) * 512], start=(k == 0),
                                              stop=(k == 7 and bias_d is None)),
                         reads=[('wch', bi), ('hT', tq)], writes=[('ps', bkA)])
                if bias_d is not None:
                    P.op('pe', 'matmul', dict(out=C.ps[bkA][0:rows, :], lhsT=bch[bi][0:1, 0:rows],
                                              rhs=ones_row[0:1, :], start=False, stop=True),
                         reads=[('bch', bi), ('ones_row',)], writes=[('ps', bkA)])
                if not rope:
                    evac(tq, C.ps[bkA][0:rows, :], ('ps', bkA))
                    continue
                bkB = bkA + 2
                for k in range(8):
                    P.op('pe', 'matmul', dict(out=C.ps[bkB][0:rows, :], lhsT=wrot[bi][:, k, 0:rows],
                                              rhs=hT[:, k, tq * 512:(tq + 1) * 512], start=(k == 0),
                                              stop=(k == 7 and bias_d is None)),
                         reads=rotkeys + [('hT', tq)], writes=[('ps', bkB)])
                if bias_d is not None:
                    P.op('pe', 'matmul', dict(out=C.ps[bkB][0:rows, :], lhsT=brot[bi][0:1, 0:rows],
                                              rhs=ones_row[0:1, :], start=False, stop=True),
                         reads=[('brot', bi, 0), ('brot', bi, 1), ('ones_row',)], writes=[('ps', bkB)])
                ri = A.rt.next()
                t1, t2 = rtmp[2 * ri], rtmp[2 * ri + 1]
                P.op('dve', 'tensor_tensor', dict(out=t1[0:rows, :], in0=C.ps[bkA][0:rows, :],
                                                  in1=ropeC[0:rows, tq * 512:(tq + 1) * 512], op=ALU.mult),
                     reads=[('ps', bkA), ('ropeC',)], writes=[('rtmp', 2 * ri)])
                P.op('dve', 'tensor_tensor', dict(out=t2[0:rows, :], in0=C.ps[bkB][0:rows, :],
                                                  in1=ropeS[0:rows, tq * 512:(tq + 1) * 512], op=ALU.mult),
                     reads=[('ps', bkB), ('ropeS',)], writes=[('rtmp', 2 * ri + 1)])
                evac(tq, (t1[0:rows, :], t2[0:rows, :]), [('rtmp', 2 * ri), ('rtmp', 2 * ri + 1)])

        def evac_to(dst_fn, wkey_fn, rope):
            def f(tq, src, skeys):
                if rope:
                    P.op('pool', 'tensor_tensor', dict(out=dst_fn(tq), in0=src[0], in1=src[1], op=ALU.add),
                         reads=skeys, writes=[wkey_fn(tq)])
                else:
                    P.op('act', 'copy', dict(out=dst_fn(tq), in_=src), reads=[skeys], writes=[wkey_fn(tq)])
            return f

        def evac_halves(lo_fn, hi_fn, wkey_fn, rope):
            def f(tq, src, skeys):
                for hf, fn in ((0, lo_fn), (1, hi_fn)):
                    rs_ = slice(64 * hf, 64 * hf + 64)
                    if rope:
                        P.op('pool', 'tensor_tensor', dict(out=fn(tq)[rs_, :], in0=src[0][rs_, :], in1=src[1][rs_, :],
                                                           op=ALU.add), reads=skeys, writes=[wkey_fn(tq, hf)])
                    else:
                        P.op('act', 'copy', dict(out=fn(tq)[rs_, :], in_=src[rs_, :]), reads=[skeys],
                             writes=[wkey_fn(tq, hf)])
            return f

        def proj_tm(c0, ncol, out_fn, vkey, wv, bias_d=None):
            load_w_cast(P, 'wv', wv[:, :, 0:ncol], win[:, c0:c0 + ncol].rearrange('(k p) n -> p k n', p=128),
                        writes=[('wv',)])
            if bias_d is not None:
                load_w_cast(P, 'wv', bch[0][0:1, 0:ncol], bias_d(c0, ncol).unsqueeze(0), writes=[('bch', 0)])
            for k4 in range(4):
                bk = 6 + (k4 % 2)
                for j in range(4):
                    kt = k4 * 4 + j
                    for k in range(8):
                        P.op('pe', 'matmul', dict(out=C.ps[bk][:, j * 64:(j + 1) * 64],
                                                  lhsT=hT[:, k, kt * 128:(kt + 1) * 128], rhs=wv[:, k, 0:ncol],
                                                  start=(k == 0), stop=(k == 7 and bias_d is None),
                                                  skip_group_check=True),
                             reads=[('wv',), ('hT', kt // 4)], writes=[('ps', bk)])
                    if bias_d is not None:
                        P.op('pe', 'matmul', dict(out=C.ps[bk][:, j * 64:(j + 1) * 64], lhsT=ones_row[0:1, 0:128],
                                                  rhs=bch[0][0:1, 0:ncol], start=False, stop=True,
                                                  skip_group_check=True),
                             reads=[('bch', 0), ('ones_row',)], writes=[('ps', bk)])
                P.op('act', 'copy', dict(out=out_fn(k4),
                                         in_=C.ps[bk][:, 0:256].rearrange('p (j d) -> p j d', d=64)),
                     reads=[('ps', bk)], writes=[(vkey, k4)])

        def st_tile(qsrc, ksrc, kt, q0, ca, cb, masks, extra=None):
            bk = A.psS.next()
            rhs, rkeys = qsrc(q0 + ca, q0 + cb)
            lhs, lkeys = ksrc(kt)
            last = (not masks) and (extra is None)
            P.op('pe', 'matmul', dict(out=C.ps[bk][:, ca:cb], lhsT=lhs, rhs=rhs, start=True, stop=last,
                                      skip_group_check=True),
                 reads=rkeys + lkeys, writes=[('ps', bk)])
            if extra is not None:
                elhs, erhs, ekeys = extra(kt, q0 + ca, q0 + cb)
                P.op('pe', 'matmul', dict(out=C.ps[bk][:, ca:cb], lhsT=elhs, rhs=erhs, start=False,
                                          stop=(not masks), skip_group_check=True),
                     reads=ekeys, writes=[('ps', bk)])
            for mi, (mt, mkey, bc) in enumerate(masks):
                P.op('pe', 'matmul', dict(out=C.ps[bk][:, bc:bc + 128], lhsT=C.ident[:], rhs=mt, start=False,
                                          stop=(mi == len(masks) - 1), skip_group_check=True),
                     reads=[('ident',), mkey], writes=[('ps', bk)])
            return bk

        def exp_pv(bk, ca, cb, vlhs, vkeys, bo, first, lastpv):
            pi = A.ptr.next()
            P.op('act', 'activation', dict(out=pT[pi][:, ca:cb], in_=C.ps[bk][:, ca:cb], func=AF.Exp, scale=0.125),
                 reads=[('ps', bk)], writes=[('pT', pi)])
            P.op('pe', 'matmul', dict(out=C.ps[bo][:, ca:cb], lhsT=vlhs, rhs=pT[pi][:, ca:cb], start=first,
                                      stop=lastpv, skip_group_check=True),
                 reads=[('pT', pi)] + vkeys, writes=[('ps', bo)])

        def band_tiles(qi, window_tiles, causal_only):
            out = []
            if causal_only:
                js = list(range(-4 * qi, 4))
            else:
                js = [j for j in range(-window_tiles, 4) if 4 * qi + j >= 0]
            js.sort(key=lambda j: (0 if j <= 0 and (causal_only or j + window_tiles >= 3) else 1, j))
            for j in js:
                kt = 4 * qi + j
                ca = max(0, 128 * j)
                masks = []
                if j >= 0:
                    masks.append((maskC[:], ('maskC',), 128 * j))
                if causal_only:
                    cb = 512
                else:
                    cb = min(512, 128 * (j + window_tiles) + 128)
                    jb = j + window_tiles
                    if 0 <= jb <= 3:
                        masks.append((maskW[:], ('maskW',), 128 * jb))
                out.append((kt, ca, cb, masks))
            return out

        for sq in range(SEQ_PER_CORE):
            tb = sq * T
            with ExitStack() as es1:
                sb1 = lambda name, shape, dt: es1.enter_context(nc.sbuf_tensor(uniq(name), shape, dt))
                xts = [sb1('n_xt%d' % i, [128, 2, D], F32) for i in range(2)]
                hb = [sb1('n_hb%d' % i, [128, D], BF16) for i in range(2)]
                junk = sb1('n_junk', [128, D], BF16)
                ss = [sb1('n_ss%d' % i, [128, 2], F32) for i in range(2)]
                rs = [sb1('n_rs%d' % i, [128, 2], F32) for i in range(2)]
                g1 = sb1('a_g1', [128, D], F32)
                P.dma('sp', 'g', dict(out=g1[:], in_=tn['norm_g'][L, 0, :].partition_broadcast(128)), writes=[('g1',)])

                def load_x(ti):
                    sl = ti % 2
                    P.dma('sp', 'x%d' % sl,
                          dict(out=xts[sl][:], in_=ysrc[tb + ti * 256:tb + (ti + 1) * 256, :]
                               .rearrange('(s p) d -> p s d', p=128)),
                          reads=[('y', sq, ti // 2)], writes=[('xt', sl)])
                load_x(0)
                for ti in range(8):
                    sl = ti % 2
                    if ti + 1 < 8:
                        load_x(ti + 1)
                    for s in range(2):
                        P.op('act', 'activation', dict(out=junk[:], in_=xts[sl][:, s, :], func=AF.Square,
                                                       accum_out=ss[sl][:, s:s + 1]),
                             reads=[('xt', sl)], writes=[('junk',), ('ss', sl)])
                    rstd_from_ss(P, C, ss[sl][:, 0:2], rs[sl][:, 0:2], 2, ('ss', sl), ('rs', sl))
                    for s in range(2):
                        hs = (ti * 2 + s) % 2
                        P.op('dve', 'scalar_tensor_tensor',
                             dict(out=hb[hs][:], in0=xts[sl][:, s, :], scalar=rs[sl][:, s:s + 1], in1=g1[:],
                                  op0=ALU.mult, op1=ALU.mult),
                             reads=[('xt', sl), ('rs', sl), ('g1',)], writes=[('hb', hs)])
                        pb = C.ps_bf[hs]
                        for c in range(8):
                            P.op('pe', 'transpose', dict(out=pb[:, c, :], in_=hb[hs][:, c * 128:(c + 1) * 128],
                                                         identity=C.ident[:]),
                                 reads=[('hb', hs), ('ident',)], writes=[('ps', hs)])
                        col = ti * 256 + s * 128
                        P.op('act', 'copy', dict(out=hT[:, :, col:col + 128], in_=pb[:, :, :]),
                             reads=[('ps', hs)], writes=[('hT', col // 512)])
                P.barrier()

            if kind == 2:
                swa_seq(P, C, A, locals())
            elif kind == 1:
                fox_seq(P, C, A, locals())
            else:
                nsa_seq(P, C, A, locals())

            with ExitStack() as es3:
                sb3 = lambda name, shape, dt: es3.enter_context(nc.sbuf_tensor(uniq(name), shape, dt))
                xt = [sb3('c_xt%d' % i, [128, 2, D], F32) for i in range(2)]
                tmp = [sb3('c_tmp%d' % i, [128, D], F32) for i in range(2)]
                junk = sb3('c_junk', [128, 512], BF16)
                ss2 = sb3('c_ss', [128, 64], F32)
                rs2 = sb3('c_rs', [128, 32], F32)
                bo = sb3('c_bo', [1, D], BF16)
                wo = sb3('a_wo', [128, 8, D], BF16)
                g2 = sb3('a_g2', [128, D], F32)
                for c0 in range(0, 8, 4):
                    load_w_cast(P, 'wA', wo[:, c0:c0 + 4, :],
                                wout_d[c0 * 128:(c0 + 4) * 128, :].rearrange('(c p) n -> p c n', p=128),
                                writes=[('wo', c0 // 4)])
                P.dma('sp', 'g', dict(out=g2[:], in_=tn['norm_g'][L, 1, :].partition_broadcast(128)), writes=[('g2',)])
                if kind == 2:
                    load_w_cast(P, 'wv', bo[0:1, :], tn['swa_b_out'][slot].unsqueeze(0), writes=[('bo',)])
                for ti in range(8):
                    sl = ti % 2
                    P.dma('sp', 'x%d' % sl,
                          dict(out=xt[sl][:], in_=ysrc[tb + ti * 256:tb + (ti + 1) * 256, :]
                               .rearrange('(s p) d -> p s d', p=128)),
                          reads=[('y', sq, ti // 2)], writes=[('cxt', sl)])
                    for s in range(2):
                        kt = ti * 2 + s
                        for dh in range(2):
                            bk = 2 + dh
                            for c in range(8):
                                P.op('pe', 'matmul',
                                     dict(out=C.ps[bk][:, :], lhsT=OT[:, c, kt * 128:(kt + 1) * 128],
                                          rhs=wo[:, c, dh * 512:(dh + 1) * 512], start=(c == 0),
                                          stop=(c == 7 and kind != 2)),
                                     reads=[('OT', c, kt // 4), ('wo', c // 4)], writes=[('ps', bk)])
                            if kind == 2:
                                P.op('pe', 'matmul',
                                     dict(out=C.ps[bk][:, :], lhsT=ones_row[0:1, 0:128],
                                          rhs=bo[0:1, dh * 512:(dh + 1) * 512], start=False, stop=True),
                                     reads=[('bo',), ('ones_row',)], writes=[('ps', bk)])
                            P.op('act', 'activation', dict(out=junk[:, :], in_=C.ps[bk][:, :], func=AF.Square,
                                                           accum_out=ss2[:, 2 * kt + dh:2 * kt + dh + 1]),
                                 reads=[('ps', bk)], writes=[('cjunk',), ('css', kt, dh)])
                        P.op('dve', 'tensor_tensor', dict(out=ss2[:, 2 * kt:2 * kt + 1], in0=ss2[:, 2 * kt:2 * kt + 1],
                                                          in1=ss2[:, 2 * kt + 1:2 * kt + 2], op=ALU.add),
                             reads=[('css', kt, 0), ('css', kt, 1)], writes=[('css', kt, 0)])
                        rstd_from_ss(P, C, ss2[:, 2 * kt:2 * kt + 1], rs2[:, kt:kt + 1], 1, ('css', kt, 0), ('crs', kt))
                        tm = tmp[s % 2]
                        for dh in range(2):
                            bk = 2 + dh
                            P.op('dve', 'scalar_tensor_tensor',
                                 dict(out=tm[:, dh * 512:(dh + 1) * 512], in0=C.ps[bk][:, :], scalar=rs2[:, kt:kt + 1],
                                      in1=g2[:, dh * 512:(dh + 1) * 512], op0=ALU.mult, op1=ALU.mult),
                                 reads=[('ps', bk), ('crs', kt), ('g2',)], writes=[('ctmp', s % 2, dh)])
                        P.op('pool', 'tensor_tensor', dict(out=xt[sl][:, s, :], in0=xt[sl][:, s, :], in1=tm[:],
                                                           op=ALU.add),
                             reads=[('cxt', sl), ('ctmp', s % 2, 0), ('ctmp', s % 2, 1)], writes=[('cxt', sl)])
                    P.dma('sp', 'yo%d' % sl,
                          dict(out=ydst[tb + ti * 256:tb + (ti + 1) * 256, :].rearrange('(s p) d -> p s d', p=128),
                               in_=xt[sl][:]),
                          reads=[('cxt', sl)], writes=[('y', sq, ti // 2)])
                P.barrier()


def run_jobs(jobs, LA=2):
    banks = {}
    n = len(jobs)
    for i in range(n + LA + 1):
        if i < n:
            if jobs[i].get('pre'):
                jobs[i]['pre']()
            banks[i] = jobs[i]['st']()
        k = i - LA
        if 0 <= k < n:
            jobs[k]['ep'](banks.pop(k))
        if 0 <= k - 1 < n and jobs[k - 1].get('fin'):
            jobs[k - 1]['fin']()


class NS:
    def __init__(self, d):
        self.__dict__.update(d)


def finish_simple(P, C, A, E, bo, rows, chunk, q0, addk):
    fi = A.fd.next()
    fd = E.fden[fi]
    fkey = ('fden', fi)
    if addk is not None:
        P.op('act', 'activation', dict(out=fd[64:128, :], in_=C.ps[bo][64:128, :], func=AF.Ln, bias=addk, scale=1.0),
             reads=[('ps', bo), ('esk',)], writes=[fkey])
    else:
        P.op('act', 'activation', dict(out=fd[64:128, :], in_=C.ps[bo][64:128, :], func=AF.Ln),
             reads=[('ps', bo)], writes=[fkey])
    P.op('act', 'activation', dict(out=fd[64:128, :], in_=fd[64:128, :], func=AF.Exp, scale=-1.0), reads=[fkey],
         writes=[fkey])
    P.op('dve', 'tensor_tensor', dict(out=E.OT[rows, chunk, q0:q0 + 512], in0=C.ps[bo][0:64, :], in1=fd[64:128, :],
                                      op=ALU.mult),
         reads=[('ps', bo), fkey], writes=[('OT', chunk, q0 // 512)])


def swa_seq(P, C, A, Ed):
    E = NS(Ed)
    nc, tn = C.nc, C.t
    b_in = tn['swa_b_in'][E.slot]
    bias_fn = lambda c0, n: b_in[c0:c0 + n]
    for g in range(2):
        with ExitStack() as es2:
            sb = lambda name, shape, dt: es2.enter_context(nc.sbuf_tensor(uniq(name), shape, dt))
            qT = sb('s_qT', [128, 4, T], BF16)
            kdl = sb('s_kdl', [128, T], BF16)
            kdh = sb('s_kdh', [128, T], BF16)
            Vt = sb('s_V', [128, 16, 2, 64], BF16)
            wv = sb('s_wv', [128, 8, 64], BF16)
            esk = sb('s_esk', [128, 16], F32)
            P.op('pool', 'memset', dict(ap=Vt[:, :, 1, :], constant=1.0), writes=[('Vones',)])
            P.op('pool', 'memset', dict(ap=kdl[64:128, :], constant=0.0), writes=[('kdz', 0)])
            P.op('pool', 'memset', dict(ap=kdh[0:64, :], constant=0.0), writes=[('kdz', 1)])
            P.dma('sp', 'g', dict(out=esk[:], in_=tn['swa_sinks'][E.slot].partition_broadcast(128)), writes=[('esk',)])
            P.op('act', 'activation', dict(out=esk[:], in_=esk[:], func=AF.Exp), reads=[('esk',)], writes=[('esk',)])
            for j in range(4):
                E.proj_fm([(g * 512 + j * 128, 128)], True,
                          E.evac_to(lambda tq, j=j: qT[:, j, tq * 512:(tq + 1) * 512],
                                    lambda tq, j=j: ('qT', j, tq), True), bias_fn)
            E.proj_fm([(1024 + g * 64, 64)] * 2, True,
                      E.evac_halves(lambda tq: kdl[:, tq * 512:(tq + 1) * 512], lambda tq: kdh[:, tq * 512:(tq + 1) * 512],
                                    lambda tq, hf: ('kd', hf, tq), True), bias_fn)
            E.proj_tm(1152 + g * 64, 64, lambda k4: Vt[:, k4 * 4:(k4 + 1) * 4, 0, :], 'Vt', wv, bias_fn)
            jobs = []
            for r in range(8):
                h = 8 * g + r
                j, half = r // 2, r % 2
                rows = slice(64 * half, 64 * half + 64)
                qsrc = lambda c0, c1, j=j: (qT[:, j, c0:c1], [('qT', j, c0 // 512)])
                ksrc = lambda kt, half=half: ((kdh if half else kdl)[:, kt * 128:(kt + 1) * 128], [('kd', half, kt // 4), ('kdz', half)])
                for qi in range(4):
                    tiles = E.band_tiles(qi, 1, False)
                    bo = 3 + A.psO.next()
                    for n_, (kt, ca, cb, masks) in enumerate(tiles):
                        job = dict(
                            st=lambda qsrc=qsrc, ksrc=ksrc, kt=kt, qi=qi, ca=ca, cb=cb, masks=masks:
                            E.st_tile(qsrc, ksrc, kt, qi * 512, ca, cb, masks),
                            ep=lambda bk, kt=kt, ca=ca, cb=cb, bo=bo, f=(n_ == 0), l=(n_ == len(tiles) - 1):
                            E.exp_pv(bk, ca, cb, Vt[:, kt, :, :].rearrange('p a d -> p (a d)'),
                                     [('Vt', kt // 4), ('Vones',)], bo, f, l))
                        if n_ == len(tiles) - 1:
                            job['fin'] = (lambda bo=bo, rows=rows, j=j, qi=qi, h=h:
                                          finish_simple(P, C, A, E, bo, rows, 4 * g + j, qi * 512, esk[64:128, h:h + 1]))
                        jobs.append(job)
            run_jobs(jobs)
            P.barrier()


def fox_seq(P, C, A, Ed):
    E = NS(Ed)
    nc, tn = C.nc, C.t
    b_f = tn['fox_b_f'][E.slot]
    scrQ, scrK = C.scrQ, C.scrK
    with ExitStack() as es2:
        sb = lambda name, shape, dt: es2.enter_context(nc.sbuf_tensor(uniq(name), shape, dt))
        spt = sb('f_spt', [16, T], F32)
        cs = sb('f_cs', [16, T], F32)
        ones16 = sb('f_ones', [16, T], F32)
        res_ = sb('f_res', [16, T], F32)
        parts = [sb('f_a%d' % i, [16, T], BF16) for i in range(3)]
        nparts = [sb('f_n%d' % i, [16, T], BF16) for i in range(3)]
        P.op('pool', 'memset', dict(ap=ones16[:], constant=1.0), writes=[('ones16',)])
        onesb = sb('f_onesb', [16, T], BF16)
        P.op('pool', 'memset', dict(ap=onesb[:], constant=1.0), writes=[('onesb',)])

        def evac_fl(tq, src, skey):
            sl = spt[0:16, tq * 512:(tq + 1) * 512]
            P.op('act', 'activation', dict(out=sl, in_=src, func=AF.Exp, scale=-1.0), reads=[skey],
                 writes=[('spt', tq)])
            P.op('act', 'activation', dict(out=sl, in_=sl, func=AF.Ln, bias=C.one[0:16, 0:1], scale=1.0),
                 reads=[('spt', tq)], writes=[('spt', tq)])
        E.proj_fm([(3072, 16)], False, evac_fl, lambda c0, n: b_f[0:16])
        P.op('dve', 'tensor_tensor_scan', dict(out=cs[:], data0=ones16[:], data1=spt[:], initial=0.0,
                                               op0=ALU.mult, op1=ALU.add),
             reads=[('ones16',)] + [('spt', tq) for tq in range(4)], writes=[('cs',)])
        P.op('dve', 'tensor_scalar', dict(out=cs[:], in0=cs[:], scalar1=8.0, scalar2=None, op0=ALU.mult),
             reads=[('cs',)], writes=[('cs',)])
        cur = cs
        ckey = ('cs',)
        for i in range(3):
            P.op('dve', 'tensor_copy', dict(out=parts[i][:], in_=cur[:]), reads=[ckey], writes=[('part', i)])
            P.op('dve', 'tensor_scalar', dict(out=nparts[i][:], in0=parts[i][:], scalar1=-1.0, scalar2=None,
                                              op0=ALU.mult), reads=[('part', i)], writes=[('npart', i)])
            if i < 2:
                P.op('dve', 'tensor_tensor', dict(out=res_[:], in0=cur[:], in1=parts[i][:], op=ALU.subtract),
                     reads=[ckey, ('part', i)], writes=[('res',)])
                cur, ckey = res_, ('res',)
            P.dma('sp', 'scr', dict(out=scrQ[i], in_=nparts[i][:]), reads=[('npart', i)], writes=[('scrQ', i)])
            P.dma('sp', 'scr', dict(out=scrK[3 + i], in_=parts[i][:]), reads=[('part', i)], writes=[('scrK', 3 + i)])
            P.dma('sp', 'scr', dict(out=scrQ[3 + i], in_=onesb[:]), reads=[('onesb',)], writes=[('scrQ', 3 + i)])
            P.dma('sp', 'scr', dict(out=scrK[i], in_=onesb[:]), reads=[('onesb',)], writes=[('scrK', i)])
        P.barrier()
    for gp in range(4):
        with ExitStack() as es2:
            sb = lambda name, shape, dt: es2.enter_context(nc.sbuf_tensor(uniq(name), shape, dt))
            qT = sb('f_qT', [128, 2, T], BF16)
            kTl = sb('f_kTl', [128, 2, T], BF16)
            kTh = sb('f_kTh', [128, 2, T], BF16)
            Vt = sb('f_V', [128, 16, 4, 2, 64], BF16)
            wv = sb('f_wv', [128, 8, 64], BF16)
            cq = sb('f_cq', [128, 4, T], BF16)
            ck = sb('f_ck', [128, 4, T], BF16)
            P.op('pool', 'memset', dict(ap=Vt[:, :, :, 1, :], constant=1.0), writes=[('Vones',)])
            P.op('pool', 'memset', dict(ap=cq[:], constant=0.0), writes=[('cq',)])
            P.op('pool', 'memset', dict(ap=ck[:], constant=0.0), writes=[('ck',)])
            P.op('pool', 'memset', dict(ap=kTl[64:128, :, :], constant=0.0), writes=[('kTz', 0)])
            P.op('pool', 'memset', dict(ap=kTh[0:64, :, :], constant=0.0), writes=[('kTz', 1)])
            P.dma('sp', 'scr2', dict(out=cq[0:6, :, :], in_=scrQ[:, 4 * gp:4 * gp + 4, :]),
                  reads=[('scrQ', i) for i in range(6)] + [('cq',)], writes=[('cq',)])
            P.dma('sp', 'scr2', dict(out=ck[0:6, :, :], in_=scrK[:, 4 * gp:4 * gp + 4, :]),
                  reads=[('scrK', i) for i in range(6)] + [('ck',)], writes=[('ck',)])
            for j in range(2):
                E.proj_fm([(gp * 256 + j * 128, 128)], False,
                          E.evac_to(lambda tq, j=j: qT[:, j, tq * 512:(tq + 1) * 512],
                                    lambda tq, j=j: ('qT', j, tq), False))
                E.proj_fm([(1024 + gp * 256 + j * 128, 128)], False,
                          E.evac_halves(lambda tq, j=j: kTl[:, j, tq * 512:(tq + 1) * 512],
                                        lambda tq, j=j: kTh[:, j, tq * 512:(tq + 1) * 512],
                                        lambda tq, hf, j=j: ('kT', j, hf, tq), False))
            for r in range(4):
                E.proj_tm(2048 + (4 * gp + r) * 64, 64, lambda k4, r=r: Vt[:, k4 * 4:(k4 + 1) * 4, r, 0, :],
                          ('Vt', r), wv)
            jobs = []
            for r in range(4):
                j, half = r // 2, r % 2
                rows = slice(64 * half, 64 * half + 64)
                qsrc = lambda c0, c1, j=j: (qT[:, j, c0:c1], [('qT', j, c0 // 512)])
                ksrc = lambda kt, half=half, j=j: ((kTh if half else kTl)[:, j, kt * 128:(kt + 1) * 128], [('kT', j, half, kt // 4), ('kTz', half)])
                extra = lambda kt, c0, c1, r=r: (ck[:, r, kt * 128:(kt + 1) * 128], cq[:, r, c0:c1], [('cq',), ('ck',)])
                for qi in range(4):
                    tiles = E.band_tiles(qi, None, True)
                    bo = 3 + A.psO.next()
                    for n_, (kt, ca, cb, masks) in enumerate(tiles):
                        job = dict(
                            st=lambda qsrc=qsrc, ksrc=ksrc, extra=extra, kt=kt, qi=qi, ca=ca, cb=cb, masks=masks:
                            E.st_tile(qsrc, ksrc, kt, qi * 512, ca, cb, masks, extra),
                            ep=lambda bk, kt=kt, ca=ca, cb=cb, bo=bo, r=r, f=(n_ == 0), l=(n_ == len(tiles) - 1):
                            E.exp_pv(bk, ca, cb, Vt[:, kt, r, :, :].rearrange('p a d -> p (a d)'),
                                     [(('Vt', r), kt // 4), ('Vones',)], bo, f, l))
                        if n_ == len(tiles) - 1:
                            job['fin'] = (lambda bo=bo, rows=rows, j=j, qi=qi:
                                          finish_simple(P, C, A, E, bo, rows, 2 * gp + j, qi * 512, None))
                        jobs.append(job)
            run_jobs(jobs)
            P.barrier()


def nsa_seq(P, C, A, Ed):
    E = NS(Ed)
    nc, tn = C.nc, C.t
    slot = E.slot
    with ExitStack() as esL:
        sbL = lambda name, shape, dt: esL.enter_context(nc.sbuf_tensor(uniq(name), shape, dt))
        w1 = [sbL('n_w1%d' % i, [128, 16, 256], BF16) for i in range(2)]
        w2k = sbL('n_w2k', [128, 2, 128], BF16)
        w2v = sbL('n_w2v', [128, 2, 64], BF16)
        posf = sbL('n_posf', [128, 2, 16], F32)
        posS = sbL('n_posS', [128, 2, 16, 2], BF16)
        biasT = sbL('n_biasT', [128, 2, 2], F32)
        cmaskT = sbL('n_cmaskT', [128, T], BF16)
        cmask = sbL('n_cmask', [128, 8, 128], BF16)
        eexp = sbL('n_eexp', [128, T], BF16)
        bonus = sbL('n_bonus', [128, 8, 32], F32)
        gsel = sbL('n_gsel', [12, 768], BF16)
        load_w_cast(P, 'cstp', cmaskT[:], tn['c_cmaskT'], [('cmaskT',)])
        load_w_cast(P, 'cstp', cmask[:], tn['c_cmask'], [('cmask',)])
        P.op('pool', 'memset', dict(ap=eexp[:], constant=0.0), writes=[('eexp',)])
        load_w_cast(P, 'cstp', eexp[0:32, :], tn['c_eexp'], [('eexp',)])
        load_w_cast(P, 'cstp', gsel[:], tn['c_gsel'], [('gsel',)])
        P.dma('sp', 'cst', dict(out=bonus[:], in_=tn['c_bonus']), writes=[('bonus',)])
        for kv in range(2):
            for c0 in range(0, 16, 8):
                load_w_cast(P, 'wA', w1[kv][:, c0:c0 + 8, :],
                            tn['nsa_cmp_w1'][slot, kv, c0 * 128:(c0 + 8) * 128, :]
                            .rearrange('(c p) n -> p c n', p=128), [('w1', kv)])
            pr = tn['nsa_cmp_pos'][slot, kv].rearrange('(c two) d -> two d c', two=2)
            for two in range(2):
                P.dma('sp', 'cst', dict(out=posf[64 * two:64 * two + 64, kv, :], in_=pr[two],
                                        allow_slow_non_contiguous=True), writes=[('posf', kv, two)])
            for x in range(2):
                P.op('pool', 'tensor_copy', dict(out=posS[:, kv, :, x], in_=posf[:, kv, :]),
                     reads=[('posf', kv, 0), ('posf', kv, 1)], writes=[('posS', kv, x)])
        w2d = tn['nsa_cmp_w2'][slot]
        for dup in range(2):
            load_w_cast(P, 'wA', w2k[:, :, 64 * dup:64 * dup + 64], w2d[0].rearrange('(c p) n -> p c n', p=128),
                        [('w2k', dup)])
        load_w_cast(P, 'wA', w2v[:, :, :], w2d[1].rearrange('(c p) n -> p c n', p=128), [('w2v',)])
        for kv in range(2):
            for hc in range(2):
                for c2 in range(16):
                    P.op('pe', 'matmul', dict(out=C.ps[7][:, 0:2], lhsT=w1[kv][:, c2, hc * 128:(hc + 1) * 128],
                                              rhs=posS[:, kv, c2, :], start=(c2 == 0), stop=(c2 == 15)),
                         reads=[('w1', kv), ('posS', kv, 0), ('posS', kv, 1)], writes=[('ps', 7)])
                P.op('act', 'copy', dict(out=biasT[:, kv, hc:hc + 1], in_=C.ps[7][:, 0:1]),
                     reads=[('ps', 7)], writes=[('biasT', kv, hc)])
        P.barrier()

        for g in range(4):
            with ExitStack() as es2:
                sb = lambda name, shape, dt: es2.enter_context(nc.sbuf_tensor(uniq(name), shape, dt))
                qT = sb('n_qT', [128, 2, T], BF16)
                ksT = [sb('n_ksT%d' % i, [128, T], BF16) for i in range(2)]
                kwT = [sb('n_kwT%d' % i, [128, T], BF16) for i in range(2)]
                cS = [sb('n_cS%d' % i, [128, T], BF16) for i in range(2)]
                Vs = sb('n_Vs', [128, 16, 2, 64], BF16)
                Vw = sb('n_Vw', [128, 16, 2, 64], BF16)
                wv = sb('n_wv', [128, 8, 64], BF16)
                gTh = sb('n_gTh', [12, T], BF16)
                kcmpT = [sb('n_kcmpT%d' % i, [128, 128], BF16) for i in range(2)]
                Vc = sb('n_Vc', [128, 2, 64], BF16)
                hidT = [sb('n_hid%d' % i, [128, 2, 128], BF16) for i in range(2)]
                negselT = sb('n_negselT', [128, T], BF16)
                Pn = sb('n_Pn', [128, 4, 128], F32)
                Ps8 = sb('n_Ps8', [128, 8, 128], F32)
                imp8 = sb('n_imp8', [128, 8, 32], F32)
                sc2 = sb('n_sc2', [128, 32], F32)
                m1 = sb('n_m1', [128, 8], F32)
                m2 = sb('n_m2', [128, 8, 8], F32)
                den4 = sb('n_den4', [128, 8, 4], F32)
                negsel = sb('n_negsel', [128, 8, 32], BF16)
                acc = [sb('n_acc%d' % i, [64, 512], F32) for i in range(2)]
                ctmp = [sb('n_ctmp%d' % i, [64, 512], F32) for i in range(2)]
                P.op('pool', 'memset', dict(ap=Vs[:, :, 1, :], constant=1.0), writes=[('Vsones',)])
                P.op('pool', 'memset', dict(ap=Vw[:, :, 1, :], constant=1.0), writes=[('Vwones',)])
                P.op('pool', 'memset', dict(ap=Vc[:, 1, :], constant=1.0), writes=[('Vc1',)])
                P.op('pool', 'memset', dict(ap=Vc[:, 0, :], constant=0.0), writes=[('Vc0',)])
                for i in range(2):
                    P.op('pool', 'memset', dict(ap=kcmpT[i][:], constant=0.0), writes=[('kcmpT', i)])
                    P.op('pool', 'memset', dict(ap=ksT[i][64 * (1 - i):64 * (1 - i) + 64, :], constant=0.0), writes=[('ksz', i)])
                    P.op('pool', 'memset', dict(ap=kwT[i][64 * (1 - i):64 * (1 - i) + 64, :], constant=0.0), writes=[('kwz', i)])
                P.op('pool', 'memset', dict(ap=negselT[:], constant=0.0), writes=[('negselT',)])
                for i in range(2):
                    P.op('pool', 'memset', dict(ap=hidT[i][:], constant=0.0), writes=[('hidT', i)])

                for j in range(2):
                    E.proj_fm([(g * 256 + j * 128, 128)], True,
                              E.evac_to(lambda tq, j=j: qT[:, j, tq * 512:(tq + 1) * 512],
                                        lambda tq, j=j: ('qT', j, tq), True))
                E.proj_fm([(1536 + g * 64, 64)] * 2, True,
                          E.evac_halves(lambda tq: ksT[0][:, tq * 512:(tq + 1) * 512], lambda tq: ksT[1][:, tq * 512:(tq + 1) * 512],
                                        lambda tq, hf: ('ksT', hf, tq), True))
                E.proj_fm([(2048 + g * 64, 64)] * 2, True,
                          E.evac_halves(lambda tq: kwT[0][:, tq * 512:(tq + 1) * 512], lambda tq: kwT[1][:, tq * 512:(tq + 1) * 512],
                                        lambda tq, hf: ('kwT', hf, tq), True))

                def evac_shift(i, rope):
                    def f(tq, src, skeys):
                        lo, hi = tq * 512, (tq + 1) * 512
                        if rope:
                            a, b = src
                            P.op('pool', 'tensor_tensor', dict(out=cS[i][0:64, lo:hi], in0=a[0:64, :], in1=b[0:64, :],
                                                               op=ALU.add), reads=skeys, writes=[('cS', i, tq, 0)])
                            if tq == 0:
                                P.op('pool', 'tensor_tensor', dict(out=cS[i][64:128, 0:511], in0=a[64:128, 1:512],
                                                                   in1=b[64:128, 1:512], op=ALU.add),
                                     reads=skeys, writes=[('cS', i, tq, 1)])
                            else:
                                P.op('pool', 'tensor_tensor', dict(out=cS[i][64:128, lo - 1:hi - 1], in0=a[64:128, :],
                                                                   in1=b[64:128, :], op=ALU.add),
                                     reads=skeys, writes=[('cS', i, tq, 1)])
                        else:
                            P.op('act', 'copy', dict(out=cS[i][0:64, lo:hi], in_=src[0:64, :]), reads=[skeys],
                                 writes=[('cS', i, tq, 0)])
                            if tq == 0:
                                P.op('act', 'copy', dict(out=cS[i][64:128, 0:511], in_=src[64:128, 1:512]),
                                     reads=[skeys], writes=[('cS', i, tq, 1)])
                            else:
                                P.op('act', 'copy', dict(out=cS[i][64:128, lo - 1:hi - 1], in_=src[64:128, :]),
                                     reads=[skeys], writes=[('cS', i, tq, 1)])
                    return f
                E.proj_fm([(1024 + g * 64, 64)] * 2, True, evac_shift(0, True))
                E.proj_fm([(1280 + g * 64, 64)] * 2, False, evac_shift(1, False))

                def evac_gate(tq, src, skey):
                    ri = A.rt.next()
                    gf = E.rtmp[2 * ri]
                    gk = ('rtmp', 2 * ri)
                    P.op('act', 'activation', dict(out=gf[0:12, :], in_=src, func=AF.Sigmoid), reads=[skey], writes=[gk])
                    P.op('dve', 'tensor_copy', dict(out=gTh[0:12, tq * 512:(tq + 1) * 512], in_=gf[0:12, :]),
                         reads=[gk], writes=[('gTh', tq)])
                E.proj_fm([(2560 + 12 * g, 12)], False, evac_gate)
                E.proj_tm(1792 + g * 64, 64, lambda k4: Vs[:, k4 * 4:(k4 + 1) * 4, 0, :], 'Vs', wv)
                E.proj_tm(2304 + g * 64, 64, lambda k4: Vw[:, k4 * 4:(k4 + 1) * 4, 0, :], 'Vw', wv)

                cSkeys = lambda i: [('cS', i, tq, x) for tq in range(4) for x in range(2)]
                for kv in range(2):
                    for hc in range(2):
                        for c2 in range(16):
                            P.op('pe', 'matmul',
                                 dict(out=C.ps[7][:, hc * 128:hc * 128 + 127], lhsT=w1[kv][:, c2, hc * 128:(hc + 1) * 128],
                                      rhs=cS[kv][:, 2 * c2:2 * c2 + 2017:16], start=(c2 == 0), stop=(c2 == 15),
                                      skip_group_check=True),
                                 reads=[('w1', kv)] + cSkeys(kv), writes=[('ps', 7)])
                        P.op('act', 'activation',
                             dict(out=hidT[kv][:, hc, 0:127], in_=C.ps[7][:, hc * 128:hc * 128 + 127], func=AF.Silu,
                                  bias=biasT[:, kv, hc:hc + 1], scale=1.0),
                             reads=[('ps', 7), ('biasT', kv, hc)], writes=[('hidT', kv)])
                for hc in range(2):
                    P.op('pe', 'matmul', dict(out=C.ps[6][:, 0:127], lhsT=w2k[:, hc, :], rhs=hidT[0][:, hc, 0:127],
                                              start=(hc == 0), stop=(hc == 1)),
                         reads=[('w2k', 0), ('w2k', 1), ('hidT', 0)], writes=[('ps', 6)])
                for i in range(2):
                    P.op('act', 'copy', dict(out=kcmpT[i][64 * i:64 * i + 64, 0:127], in_=C.ps[6][64 * i:64 * i + 64, 0:127]),
                         reads=[('ps', 6), ('kcmpT', i)], writes=[('kcmpT', i)])
                for hc in range(2):
                    P.op('pe', 'matmul', dict(out=C.ps[6][0:127, 256:320], lhsT=hidT[1][:, hc, 0:127], rhs=w2v[:, hc, :],
                                              start=(hc == 0), stop=(hc == 1), skip_group_check=True),
                         reads=[('w2v',), ('hidT', 1)], writes=[('ps', 6)])
                P.op('act', 'copy', dict(out=Vc[0:127, 0, :], in_=C.ps[6][0:127, 256:320]), reads=[('ps', 6), ('Vc0',)],
                     writes=[('Vc0',)])

                for tt in range(8):
                    t0 = 1024 + tt * 128
                    for r in range(4):
                        j, half = r // 2, r % 2
                        rows = slice(64 * half, 64 * half + 64)
                        P.op('pe', 'matmul', dict(out=C.ps[5][:, r * 128:(r + 1) * 128], lhsT=qT[:, j, t0:t0 + 128],
                                                  rhs=kcmpT[half][:, :], start=True, stop=False, skip_group_check=True),
                             reads=[('qT', j, t0 // 512), ('kcmpT', half)], writes=[('ps', 5)])
                        P.op('pe', 'matmul', dict(out=C.ps[5][:, r * 128:(r + 1) * 128], lhsT=C.ident[:],
                                                  rhs=cmask[:, tt, :], start=False, stop=True, skip_group_check=True),
                             reads=[('ident',), ('cmask',)], writes=[('ps', 5)])
                    for r in range(4):
                        P.op('act', 'activation', dict(out=Pn[:, r, :], in_=C.ps[5][:, r * 128:(r + 1) * 128],
                                                       func=AF.Exp, scale=0.125, accum_out=den4[:, tt, r:r + 1]),
                             reads=[('ps', 5)], writes=[('Pn', r), ('den4', tt, r)])
                    dk = [('den4', tt, r) for r in range(4)]
                    P.op('dve', 'tensor_scalar', dict(out=den4[:, tt, :], in0=den4[:, tt, :], scalar1=1e-30, scalar2=None,
                                                      op0=ALU.max), reads=dk, writes=[('den4', tt)])
                    P.op('dve', 'reciprocal', dict(out=den4[:, tt, :], in_=den4[:, tt, :]), reads=[('den4', tt)],
                         writes=[('den4', tt)])
                    P.op('dve', 'tensor_scalar', dict(out=Ps8[:, tt, :], in0=Pn[:, 0, :], scalar1=den4[:, tt, 0:1],
                                                      scalar2=None, op0=ALU.mult),
                         reads=[('Pn', 0), ('den4', tt)], writes=[('Ps8', tt)])
                    for r in range(1, 4):
                        P.op('dve', 'scalar_tensor_tensor',
                             dict(out=Ps8[:, tt, :], in0=Pn[:, r, :], scalar=den4[:, tt, r:r + 1], in1=Ps8[:, tt, :],
                                  op0=ALU.mult, op1=ALU.add),
                             reads=[('Pn', r), ('den4', tt), ('Ps8', tt)], writes=[('Ps8', tt)])
                pk = [('Ps8', tt) for tt in range(8)]
                Pv = Ps8[:].rearrange('p t (n i) -> p t n i', i=4)
                P.op('dve', 'tensor_tensor', dict(out=imp8[:], in0=Pv[:, :, :, 0], in1=Pv[:, :, :, 1], op=ALU.add),
                     reads=pk, writes=[('imp8',)])
                P.op('dve', 'tensor_tensor', dict(out=imp8[:], in0=imp8[:], in1=Pv[:, :, :, 2], op=ALU.add),
                     reads=pk + [('imp8',)], writes=[('imp8',)])
                P.op('dve', 'scalar_tensor_tensor', dict(out=imp8[:], in0=Pv[:, :, :, 3], scalar=0.5, in1=imp8[:],
                                                         op0=ALU.mult, op1=ALU.add),
                     reads=pk + [('imp8',)], writes=[('imp8',)])
                P.op('dve', 'scalar_tensor_tensor', dict(out=imp8[:, :, 1:32], in0=Pv[:, :, 0:31, 3], scalar=0.5,
                                                         in1=imp8[:, :, 1:32], op0=ALU.mult, op1=ALU.add),
                     reads=pk + [('imp8',)], writes=[('imp8',)])
                P.op('dve', 'tensor_tensor', dict(out=imp8[:], in0=imp8[:], in1=bonus[:], op=ALU.add),
                     reads=[('imp8',), ('bonus',)], writes=[('imp8',)])
                for tt in range(8):
                    P.op('dve', 'max', dict(out=m1[:], in_=imp8[:, tt, :]), reads=[('imp8',)], writes=[('m1',)])
                    P.op('dve', 'match_replace', dict(out=sc2[:], in_to_replace=m1[:], in_values=imp8[:, tt, :],
                                                      imm_value=-1e30), reads=[('m1',), ('imp8',)], writes=[('sc2',)])
                    P.op('dve', 'max', dict(out=m2[:, tt, :], in_=sc2[:]), reads=[('sc2',)], writes=[('m2', tt)])
                    P.op('dve', 'tensor_scalar', dict(out=negsel[:, tt, :], in0=imp8[:, tt, :], scalar1=m2[:, tt, 7:8],
                                                      scalar2=NEG, op0=ALU.is_lt, op1=ALU.mult),
                         reads=[('imp8',), ('m2', tt)], writes=[('negsel', tt)])
                    P.op('pe', 'transpose', dict(out=C.ps_bf[6][0:32, tt, :], in_=negsel[:, tt, :], identity=C.ident[:]),
                         reads=[('negsel', tt), ('ident',)], writes=[('ps', 6)])
                P.op('act', 'copy', dict(out=negselT[0:32, 1024:2048],
                                         in_=C.ps_bf[6][0:32, :, :].rearrange('p a b -> p (a b)')),
                     reads=[('ps', 6)], writes=[('negselT',)])

                jobs = []
                first_gate = [True]

                def gate_pre(r, qi, par):
                    q0 = qi * 512
                    gk = [('gTh', qi), ('gsel',)]
                    b01 = 5 if par == 0 else 7
                    P.op('pe', 'matmul', dict(out=C.ps[b01][:, :], lhsT=gsel[0:12, (3 * r) * 64:(3 * r + 2) * 64],
                                              rhs=gTh[0:12, q0:q0 + 512], start=True, stop=True),
                         reads=gk, writes=[('ps', b01)])
                    wk = [('ps6h', par)]
                    if first_gate[0]:
                        wk = [('ps', 6), ('ps6h', 0), ('ps6h', 1)]
                        first_gate[0] = False
                    P.op('pe', 'matmul', dict(out=C.ps[6][64 * par:64 * par + 64, :],
                                              lhsT=gsel[0:12, (3 * r + 2) * 64:(3 * r + 3) * 64],
                                              rhs=gTh[0:12, q0:q0 + 512], start=True, stop=True,
                                              skip_group_check=True),
                         reads=gk, writes=wk)

                def combine(b, bo, r, qi, par, rows, j):
                    q0 = qi * 512
                    b01 = 5 if par == 0 else 7
                    gap = [C.ps[b01][0:64, :], C.ps[b01][64:128, :], C.ps[6][64 * par:64 * par + 64, :]][b]
                    gkey = [('ps', b01), ('ps', b01), ('ps6h', par)][b]
                    ai = par
                    fi = A.fd.next()
                    fd = E.fden[fi]
                    fk = ('fden', fi)
                    P.op('act', 'activation', dict(out=fd[64:128, :], in_=C.ps[bo][64:128, :], func=AF.Ln),
                         reads=[('ps', bo)], writes=[fk])
                    P.op('act', 'activation', dict(out=fd[64:128, :], in_=fd[64:128, :], func=AF.Exp, scale=-1.0),
                         reads=[fk], writes=[fk])
                    P.op('dve', 'tensor_tensor', dict(out=fd[0:64, :], in0=gap, in1=fd[64:128, :], op=ALU.mult),
                         reads=[gkey, fk], writes=[fk])
                    if b == 0:
                        P.op('dve', 'tensor_tensor', dict(out=acc[ai][:], in0=C.ps[bo][0:64, :], in1=fd[0:64, :],
                                                          op=ALU.mult),
                             reads=[('ps', bo), fk], writes=[('acc', ai)])
                    else:
                        ci = b - 1
                        P.op('dve', 'tensor_tensor', dict(out=ctmp[ci][:], in0=C.ps[bo][0:64, :], in1=fd[0:64, :],
                                                          op=ALU.mult),
                             reads=[('ps', bo), fk], writes=[('ctmp', ci)])
                        if b == 1:
                            P.op('pool', 'tensor_tensor', dict(out=acc[ai][:], in0=acc[ai][:], in1=ctmp[ci][:],
                                                               op=ALU.add),
                                 reads=[('acc', ai), ('ctmp', ci)], writes=[('acc', ai)])
                        else:
                            P.op('pool', 'tensor_tensor',
                                 dict(out=E.OT[rows, 2 * g + j, q0:q0 + 512], in0=acc[ai][:], in1=ctmp[ci][:],
                                      op=ALU.add),
                                 reads=[('acc', ai), ('ctmp', ci)], writes=[('OT', 2 * g + j, qi)])

                for r in range(4):
                    j, half = r // 2, r % 2
                    rows = slice(64 * half, 64 * half + 64)
                    qsrc = lambda c0, c1, j=j: (qT[:, j, c0:c1], [('qT', j, c0 // 512)])
                    for qi in range(4):
                        q0 = qi * 512
                        par = (r * 4 + qi) % 2
                        for b in range(3):
                            bo = 3 + A.psO.next()
                            if b == 0:
                                tiles = [(0, 0, 512, [])]
                                ksrc = lambda kt, half=half: (kcmpT[half][:, :], [('kcmpT', half)])
                                extra = lambda kt, c0, c1: (C.ident[:], cmaskT[:, c0:c1], [('ident',), ('cmaskT',)])
                                vfn = lambda kt: (Vc[:, :, :].rearrange('p a d -> p (a d)'), [('Vc0',), ('Vc1',)])
                            elif b == 1:
                                tiles = E.band_tiles(qi, None, True)
                                extra = None
                                if qi >= 2:
                                    extra = lambda kt, c0, c1: (eexp[:, kt * 128:(kt + 1) * 128], negselT[:, c0:c1],
                                                                [('eexp',), ('negselT',)])
                                ksrc = lambda kt, half=half: (ksT[half][:, kt * 128:(kt + 1) * 128], [('ksT', half, kt // 4), ('ksz', half)])
                                vfn = lambda kt: (Vs[:, kt, :, :].rearrange('p a d -> p (a d)'),
                                                  [('Vs', kt // 4), ('Vsones',)])
                            else:
                                tiles = E.band_tiles(qi, 4, False)
                                extra = None
                                ksrc = lambda kt, half=half: (kwT[half][:, kt * 128:(kt + 1) * 128], [('kwT', half, kt // 4), ('kwz', half)])
                                vfn = lambda kt: (Vw[:, kt, :, :].rearrange('p a d -> p (a d)'),
                                                  [('Vw', kt // 4), ('Vwones',)])
                            for n_, (kt, ca, cb, masks) in enumerate(tiles):
                                job = dict(
                                    st=lambda qsrc=qsrc, ksrc=ksrc, extra=extra, kt=kt, q0=q0, ca=ca, cb=cb, masks=masks:
                                    E.st_tile(qsrc, ksrc, kt, q0, ca, cb, masks, extra),
                                    ep=lambda bk, vfn=vfn, kt=kt, ca=ca, cb=cb, bo=bo, f=(n_ == 0), l=(n_ == len(tiles) - 1):
                                    E.exp_pv(bk, ca, cb, vfn(kt)[0], vfn(kt)[1], bo, f, l))
                                if b == 0 and n_ == 0:
                                    job['pre'] = lambda r=r, qi=qi, par=par: gate_pre(r, qi, par)
                                if n_ == len(tiles) - 1:
                                    job['fin'] = (lambda b=b, bo=bo, r=r, qi=qi, par=par, rows=rows, j=j:
                                                  combine(b, bo, r, qi, par, rows, j))
                                jobs.append(job)
                run_jobs(jobs)
                P.barrier()


def build_program(layers=(0, 1, 2, 3), do_attn=True, do_mlp=True):
    nc = bass.Bass("TRN2", target_bir_lowering=False)
    C = Ctx()
    C.nc = nc
    C.t = declare_inputs(nc)
    for name, shp in CONST_SHAPES.items():
        C.t[name] = nc.dram_tensor(name, list(shp), F32, kind="ExternalInput").ap()
    C.scrQ = nc.dram_tensor('scrQ', [6, 16, T], BF16, kind="Internal").ap()
    C.scrK = nc.dram_tensor('scrK', [6, 16, T], BF16, kind="Internal").ap()
    with ExitStack() as es:
        P = Prog(nc, es)
        C.P = P
        C.ps = [es.enter_context(nc.psum_tensor('ps%d' % i, [128, 512], F32)) for i in range(8)]
        C.ps_bf = [C.ps[i][:].bitcast(BF16).rearrange('p (c t) -> p c t', c=8) for i in range(8)]
        C.ident = es.enter_context(nc.sbuf_tensor('ident', [128, 128], BF16))
        C.identf = es.enter_context(nc.sbuf_tensor('identf', [128, 128], F32))
        C.eps = es.enter_context(nc.sbuf_tensor('eps', [128, 1], F32))
        C.one = es.enter_context(nc.sbuf_tensor('one', [128, 1], F32))
        P.op('pool', 'memset', dict(ap=C.identf[:], constant=0.0), writes=[('identf',)])
        P.op('pool', 'affine_select',
             dict(out=C.identf[:], in_=C.identf[:], pattern=[[-1, 128]], compare_op=ALU.not_equal,
                  fill=1.0, base=0, channel_multiplier=1),
             reads=[('identf',)], writes=[('identf',)])
        P.op('pool', 'tensor_copy', dict(out=C.ident[:], in_=C.identf[:]), reads=[('identf',)], writes=[('ident',)])
        P.op('pool', 'memset', dict(ap=C.eps[:], constant=EPS), writes=[('eps',)])
        P.op('pool', 'memset', dict(ap=C.one[:], constant=1.0), writes=[('one',)])
        P.barrier()
        src = C.t['x']
        for L in layers:
            if do_attn:
                attn_phase(P, C, L, src)
                src = C.t['y']
            if do_mlp:
                mlp_phase(P, C, L, src)
                src = C.t['y']
        P.barrier()
        P.emit()
    return nc, P


_CONSTS = None


def kernel(**inputs):
    global _CONSTS
    if _CONSTS is None:
        _CONSTS = host_consts()
    nc, P = build_program()
    x = np.ascontiguousarray(np.asarray(inputs['x'], dtype=np.float32))
    shared = {k: np.ascontiguousarray(np.asarray(v, dtype=np.float32)) for k, v in inputs.items() if k != 'x'}
    shared.update(_CONSTS)
    in_maps = []
    for c in range(NCORES):
        m = dict(shared)
        m['x'] = x[c * SEQ_PER_CORE:(c + 1) * SEQ_PER_CORE].reshape(NTOK, D)
        in_maps.append(m)
    res = run_bass_kernel_spmd(nc, in_maps, core_ids=list(range(NCORES)))
    out = np.stack([np.asarray(r['y']).reshape(SEQ_PER_CORE, T, D) for r in res.results], axis=0)
    return out.reshape(NCORES * SEQ_PER_CORE, T, D).astype(np.float32)
```

```python
import numpy as np
from contextlib import ExitStack
import concourse.bass as bass
import concourse.mybir as mybir
from concourse.bass_utils import run_bass_kernel_spmd

F32 = mybir.dt.float32
BF16 = mybir.dt.bfloat16
AF = mybir.ActivationFunctionType
ALU = mybir.AluOpType
AX = mybir.AxisListType

NCORES = 8
D = 1024
T = 2048
SEQ_PER_CORE = 2
NTOK = T * SEQ_PER_CORE
DFF = 4096
H = 16
DH = 64
EPS = 1e-6
NEG = -30000.0


_UNIQ = [0]


def uniq(name):
    _UNIQ[0] += 1
    return '%s_%d' % (name, _UNIQ[0])


class Chan:
    def __init__(self, sem):
        self.sem = sem
        self.count = 0


class Prog:
    def __init__(self, nc, es):
        self.nc = nc
        self.es = es
        self.names = ('pe', 'act', 'dve', 'pool', 'sp')
        self.ins = {e: [] for e in self.names}
        self.res = {}
        self.known = {e: {} for e in self.names}
        self.chans = {}
        self.epoch = 0
        self.KS = 4
        self.RS = 4096
        self.esem = {e: [self.es.enter_context(self.nc.semaphore('s_%s_%d' % (e, k))) for k in range(self.KS)]
                     for e in self.names}

    def chan(self, name):
        c = self.chans.get(name)
        if c is None:
            c = Chan(self.es.enter_context(self.nc.semaphore('c_' + name)))
            self.chans[name] = c
        return c

    def _collect(self, eng, reads, writes):
        need = {}

        def add(src, idx):
            if src[0] == 'd':
                idx = self.chans[src[1]].count
            if need.get(src, -1) < idx:
                need[src] = idx

        for k in reads:
            st = self.res.get(k)
            if st is not None and st[0] is not None:
                add(*st[0])
        for k in writes:
            st = self.res.get(k)
            if st is not None:
                if st[0] is not None:
                    add(*st[0])
                for src, idx in st[1].items():
                    add(src, idx)
        waits = []
        kn = self.known[eng]
        for src, idx in need.items():
            if eng == 'pe' and src == ('e', 'pe'):
                continue
            if kn.get(src, -1) >= idx:
                continue
            kn[src] = idx
            waits.append((src, idx))
            if src[0] == 'e':
                self.ins[src[1]][idx]['sig'] = True
        return waits

    def _update(self, ev, reads, writes):
        for k in reads:
            st = self.res.get(k)
            if st is None:
                st = [None, {}]
                self.res[k] = st
            st[1][ev[0]] = ev[1]
        for k in writes:
            self.res[k] = [ev, {}]

    def op(self, eng, method, kw, reads=(), writes=()):
        waits = self._collect(eng, reads, writes)
        idx = len(self.ins[eng])
        self.ins[eng].append(dict(m=method, kw=kw, waits=waits, sig=False, dma=None, ep=self.epoch))
        self._update((('e', eng), idx), reads, writes)

    def dma(self, q, ch, kw, reads=(), writes=()):
        c = self.chan(ch)
        waits = self._collect(q, reads, writes)
        c.count += 1
        self.ins[q].append(dict(m='dma_start', kw=kw, waits=waits, sig=False, dma=c.sem, ep=self.epoch))
        self._update((('d', ch), c.count), reads, writes)

    def barrier(self):
        last = {}
        for e in self.names:
            for i in range(len(self.ins[e]) - 1, -1, -1):
                it = self.ins[e][i]
                if it['ep'] != self.epoch:
                    break
                if it['dma'] is None and it['m'] != 'wait_only':
                    last[e] = i
                    it['sig'] = True
                    break
        for e in self.names:
            waits = []
            for y, i in last.items():
                if e == 'pe' and y == 'pe':
                    continue
                waits.append((('e', y), i))
            for name, c in self.chans.items():
                if c.count > 0:
                    waits.append((('d', name), c.count))
            self.ins[e].append(dict(m='wait_only', kw=None, waits=waits, sig=False, dma=None, ep=self.epoch))
        self.res = {}
        self.known = {e: {} for e in self.names}
        self.epoch += 1

    def emit(self):
        cnt = {}
        for e in self.names:
            n = 0
            arr = []
            for it in self.ins[e]:
                if it['sig']:
                    b = n // self.RS
                    arr.append((b % self.KS, (b // self.KS) * self.RS + (n % self.RS) + 1))
                    n += 1
                else:
                    arr.append(None)
            cnt[e] = arr
        stats = {e: (len(self.ins[e]), sum(len(it['waits']) for it in self.ins[e])) for e in self.names}
        self.stats = stats

        def mk(e):
            def body(engobj):
                for idx_, it in enumerate(self.ins[e]):
                    for (src, idx) in it['waits']:
                        if src[0] == 'e':
                            y = src[1]
                            k, v = cnt[y][idx]
                            engobj.wait_ge(self.esem[y][k], v)
                        else:
                            engobj.wait_ge(self.chans[src[1]].sem, 16 * idx)
                    if it['m'] == 'wait_only':
                        continue
                    r = getattr(engobj, it['m'])(**it['kw'])
                    if it['dma'] is not None:
                        r.then_inc(it['dma'], 16)
                    elif it['sig']:
                        r.then_inc(self.esem[e][cnt[e][idx_][0]], 1)
            return body

        with self.nc.Block() as block:
            block.tensor(mk('pe'))
            block.scalar(mk('act'))
            block.vector(mk('dve'))
            block.gpsimd(mk('pool'))
            block.sync(mk('sp'))


class Ctx:
    pass


def declare_inputs(nc):
    t = {}

    def inp(name, shape):
        t[name] = nc.dram_tensor(name, list(shape), F32, kind="ExternalInput").ap()

    inp('x', (NTOK, D))
    inp('norm_g', (4, 4, D))
    inp('mlp_w_up', (4, D, DFF))
    inp('mlp_w_down', (4, DFF, D))
    inp('nsa_w_in', (2, D, 2608))
    inp('nsa_cmp_pos', (2, 2, 32, 64))
    inp('nsa_cmp_w1', (2, 2, 2048, 256))
    inp('nsa_cmp_w2', (2, 2, 256, 64))
    inp('nsa_w_out', (2, D, D))
    inp('fox_w_in', (1, D, 3088))
    inp('fox_b_f', (1, 16))
    inp('fox_w_out', (1, D, D))
    inp('swa_w_in', (1, D, 1280))
    inp('swa_b_in', (1, 1280))
    inp('swa_sinks', (1, 16))
    inp('swa_w_out', (1, D, D))
    inp('swa_b_out', (1, D))
    t['y'] = nc.dram_tensor('y', [NTOK, D], F32, kind="ExternalOutput").ap()
    return t


def load_w_cast(P, ch, out_ap, in_ap, writes):
    P.dma('pool', ch, dict(out=out_ap, in_=in_ap, max_dma_last_dim=4096), reads=(), writes=writes)


def rstd_from_ss(P, C, ss, rstd, n, key_ss, key_rstd):
    P.op('act', 'activation', dict(out=rstd, in_=ss, func=AF.Ln, scale=1.0 / D, bias=C.eps[:, 0:1]),
         reads=[key_ss], writes=[key_rstd])
    P.op('act', 'activation', dict(out=rstd, in_=rstd, func=AF.Exp, scale=-0.5),
         reads=[key_rstd], writes=[key_rstd])


def mlp_phase(P, C, L, ysrc):
    nc = C.nc
    tn = C.t
    TT = 256
    NS = TT // 128
    ntiles = NTOK // TT
    with ExitStack() as es:
        sb = lambda name, shape, dt: es.enter_context(nc.sbuf_tensor(uniq(name), shape, dt))
        wup = sb('wup', [128, 8, DFF], BF16)
        wdn = sb('wdn', [128, 32, D], BF16)
        g3 = sb('g3', [128, D], F32)
        g4 = sb('g4', [128, D], F32)
        xts = [sb('xt%d' % i, [128, NS, D], F32) for i in range(2)]
        hb = [sb('hb%d' % i, [128, D], BF16) for i in range(2)]
        hT = [sb('hT%d' % i, [128, 8, TT], BF16) for i in range(2)]
        aT = sb('aT', [128, 32, TT], BF16)
        rl = [sb('rl%d' % i, [128, TT], F32) for i in range(2)]
        junk = sb('junk', [128, D], BF16)
        tmp = [sb('tmp%d' % i, [128, D], F32) for i in range(2)]
        ss = [sb('ss%d' % i, [128, 4], F32) for i in range(2)]
        rs = [sb('rs%d' % i, [128, 4], F32) for i in range(2)]
        ss4 = [sb('ss4%d' % i, [128, 4], F32) for i in range(2)]
        rs4 = [sb('rs4%d' % i, [128, 4], F32) for i in range(2)]

        for k in range(8):
            for hf in range(2):
                load_w_cast(P, 'wA', wup[:, k, hf * 2048:(hf + 1) * 2048],
                            tn['mlp_w_up'][L, k * 128:(k + 1) * 128, hf * 2048:(hf + 1) * 2048],
                            writes=[('wup', k)])
        for c0 in range(0, 32, 4):
            load_w_cast(P, 'wB', wdn[:, c0:c0 + 4, :],
                        tn['mlp_w_down'][L, c0 * 128:(c0 + 4) * 128, :].rearrange('(c p) n -> p c n', p=128),
                        writes=[('wdn', c0 // 4)])
        P.dma('sp', 'g', dict(out=g3[:], in_=tn['norm_g'][L, 2, :].partition_broadcast(128)), writes=[('g3',)])
        P.dma('sp', 'g', dict(out=g4[:], in_=tn['norm_g'][L, 3, :].partition_broadcast(128)), writes=[('g4',)])

        def load_x(ti):
            sl = ti % 2
            P.dma('sp', 'x%d' % sl,
                  dict(out=xts[sl][:], in_=ysrc[ti * TT:(ti + 1) * TT, :].rearrange('(s p) d -> p s d', p=128)),
                  reads=[('y', ti)], writes=[('xt', sl)])

        load_x(0)
        for ti in range(ntiles):
            sl = ti % 2
            xt = xts[sl]
            if ti + 1 < ntiles:
                load_x(ti + 1)
            for s in range(NS):
                P.op('act', 'activation', dict(out=junk[:], in_=xt[:, s, :], func=AF.Square,
                                               accum_out=ss[sl][:, s:s + 1]),
                     reads=[('xt', sl)], writes=[('junk',), ('ss', sl)])
            rstd_from_ss(P, C, ss[sl][:, 0:NS], rs[sl][:, 0:NS], NS, ('ss', sl), ('rs', sl))
            for s in range(NS):
                hs = (ti * NS + s) % 2
                P.op('dve', 'scalar_tensor_tensor',
                     dict(out=hb[hs][:], in0=xt[:, s, :], scalar=rs[sl][:, s:s + 1], in1=g3[:],
                          op0=ALU.mult, op1=ALU.mult),
                     reads=[('xt', sl), ('rs', sl), ('g3',)], writes=[('hb', hs)])
                pb = C.ps_bf[hs]
                for c in range(8):
                    P.op('pe', 'transpose', dict(out=pb[:, c, :], in_=hb[hs][:, c * 128:(c + 1) * 128],
                                                 identity=C.ident[:]),
                         reads=[('hb', hs), ('ident',)], writes=[('ps', hs)])
                P.op('act', 'copy', dict(out=hT[sl][:, :, s * 128:(s + 1) * 128], in_=pb[:, :, :]),
                     reads=[('ps', hs)], writes=[('hT', sl)])
            for fc in range(32):
                bk = 2 + (fc % 2)
                for k in range(8):
                    P.op('pe', 'matmul', dict(out=C.ps[bk][:, 0:TT], lhsT=wup[:, k, fc * 128:(fc + 1) * 128],
                                              rhs=hT[sl][:, k, :], start=(k == 0), stop=(k == 7)),
                         reads=[('wup', k), ('hT', sl)], writes=[('ps', bk)])
                r = rl[fc % 2]
                P.op('act', 'activation', dict(out=r[:], in_=C.ps[bk][:, 0:TT], func=AF.Relu),
                     reads=[('ps', bk)], writes=[('rl', fc % 2)])
                P.op('dve', 'tensor_tensor', dict(out=aT[:, fc, :], in0=r[:], in1=r[:], op=ALU.mult),
                     reads=[('rl', fc % 2)], writes=[('aT', fc)])
            for s in range(NS):
                for dh in range(2):
                    bk = 4 + dh
                    for fc in range(32):
                        P.op('pe', 'matmul',
                             dict(out=C.ps[bk][:, :], lhsT=aT[:, fc, s * 128:(s + 1) * 128],
                                  rhs=wdn[:, fc, dh * 512:(dh + 1) * 512], start=(fc == 0), stop=(fc == 31)),
                             reads=[('aT', fc), ('wdn', fc // 4)], writes=[('ps', bk)])
                    P.op('act', 'activation', dict(out=junk[:, 0:512], in_=C.ps[bk][:, :], func=AF.Square,
                                                   accum_out=ss4[sl][:, 2 * s + dh:2 * s + dh + 1]),
                         reads=[('ps', bk)], writes=[('junk',), ('ss4', sl, s, dh)])
                P.op('dve', 'tensor_tensor', dict(out=ss4[sl][:, 2 * s:2 * s + 1], in0=ss4[sl][:, 2 * s:2 * s + 1],
                                                  in1=ss4[sl][:, 2 * s + 1:2 * s + 2], op=ALU.add),
                     reads=[('ss4', sl, s, 0), ('ss4', sl, s, 1)], writes=[('ss4', sl, s, 0)])
                rstd_from_ss(P, C, ss4[sl][:, 2 * s:2 * s + 1], rs4[sl][:, s:s + 1], 1,
                             ('ss4', sl, s, 0), ('rs4', sl, s))
                tm = tmp[s % 2]
                for dh in range(2):
                    bk = 4 + dh
                    P.op('dve', 'scalar_tensor_tensor',
                         dict(out=tm[:, dh * 512:(dh + 1) * 512], in0=C.ps[bk][:, :], scalar=rs4[sl][:, s:s + 1],
                              in1=g4[:, dh * 512:(dh + 1) * 512], op0=ALU.mult, op1=ALU.mult),
                         reads=[('ps', bk), ('rs4', sl, s), ('g4',)], writes=[('tmp', s % 2, dh)])
                P.op('pool', 'tensor_tensor', dict(out=xt[:, s, :], in0=xt[:, s, :], in1=tm[:], op=ALU.add),
                     reads=[('xt', sl), ('tmp', s % 2, 0), ('tmp', s % 2, 1)], writes=[('xt', sl)])
            P.dma('sp', 'yo%d' % sl,
                  dict(out=C.t['y'][ti * TT:(ti + 1) * TT, :].rearrange('(s p) d -> p s d', p=128), in_=xt[:]),
                  reads=[('xt', sl)], writes=[('y', ti)])
        P.barrier()


CONST_SHAPES = {
    'c_rope': (2, 128, T),
    'c_maskC': (128, 128),
    'c_maskW': (128, 128),
    'c_cmaskT': (128, T),
    'c_cmask': (128, 8, 128),
    'c_eexp': (32, T),
    'c_bonus': (128, 8, 32),
    'c_gsel': (12, 768),
}


def host_consts():
    c = {}
    inv = (np.float32(10000.0) ** (-np.arange(0, DH, 2, dtype=np.float32) / np.float32(DH))).astype(np.float32)
    ang = (np.arange(T, dtype=np.float32)[:, None] * inv[None, :]).astype(np.float32)
    cos = np.cos(ang).astype(np.float32).T
    sin = np.sin(ang).astype(np.float32).T
    c['c_rope'] = np.stack([np.tile(cos, (4, 1)), np.tile(sin, (4, 1))]).astype(np.float32)
    s = np.arange(128)[:, None]
    t = np.arange(128)[None, :]
    c['c_maskC'] = np.where(t >= s, 0.0, NEG).astype(np.float32)
    c['c_maskW'] = np.where(t < s, 0.0, NEG).astype(np.float32)
    cc = np.arange(128)[:, None]
    tt = np.arange(T)[None, :]
    c['c_cmaskT'] = np.where(16 * cc + 31 <= tt, 0.0, NEG).astype(np.float32)
    c['c_cmaskT'][127, :] = -332.0
    tq = 1024 + np.arange(8)[None, :, None] * 128 + np.arange(128)[:, None, None]
    c['c_cmask'] = np.where(16 * np.arange(128)[None, None, :] + 31 <= tq, 0.0, NEG).astype(np.float32)
    c['c_eexp'] = (np.arange(T)[None, :] // 64 == np.arange(32)[:, None]).astype(np.float32)
    tblk = tq // 64
    n = np.arange(32)[None, None, :]
    forced = (n == 0) | (n == tblk) | (n == tblk - 1)
    c['c_bonus'] = np.where(n <= tblk, np.where(forced, 1000.0, 0.0), -1.0).astype(np.float32)
    c['c_gsel'] = (np.arange(768)[None, :] // 64 == np.arange(12)[:, None]).astype(np.float32)
    return c


class Rot:
    def __init__(self, n):
        self.n = n
        self.i = 0

    def next(self):
        v = self.i % self.n
        self.i += 1
        return v


def attn_phase(P, C, L, ysrc):
    nc = C.nc
    tn = C.t
    kind = L % 3
    slot = L // 3
    if kind == 0:
        win, wout_d = tn['nsa_w_in'][slot], tn['nsa_w_out'][slot]
    elif kind == 1:
        win, wout_d = tn['fox_w_in'][slot], tn['fox_w_out'][slot]
    else:
        win, wout_d = tn['swa_w_in'][slot], tn['swa_w_out'][slot]
    ydst = tn['y']

    with ExitStack() as es:
        sb = lambda name, shape, dt: es.enter_context(nc.sbuf_tensor(uniq(name), shape, dt))
        hT = sb('a_hT', [128, 8, T], BF16)
        OT = sb('a_OT', [128, 8, T], BF16)
        maskC = sb('a_maskC', [128, 128], BF16)
        maskW = sb('a_maskW', [128, 128], BF16)
        ones_row = sb('a_ones', [1, 512], BF16)
        wch = [sb('a_wch%d' % i, [128, 8, 128], BF16) for i in range(2)]
        wrot = [sb('a_wrot%d' % i, [128, 8, 128], BF16) for i in range(2)]
        bch = [sb('a_bch%d' % i, [1, 128], BF16) for i in range(2)]
        brot = [sb('a_brot%d' % i, [1, 128], BF16) for i in range(2)]
        rtmp = [sb('a_rtmp%d' % i, [128, 512], F32) for i in range(4)]
        pT = [sb('a_pT%d' % i, [128, 512], BF16) for i in range(3)]
        fden = [sb('a_fden%d' % i, [128, 512], F32) for i in range(2)]
        if kind != 1:
            ropeC = sb('a_ropeC', [128, T], F32)
            ropeS = sb('a_ropeS', [128, T], F32)
            P.dma('sp', 'cst', dict(out=ropeC[:], in_=tn['c_rope'][0]), writes=[('ropeC',)])
            P.dma('sp', 'cst', dict(out=ropeS[:], in_=tn['c_rope'][1]), writes=[('ropeS',)])
        load_w_cast(P, 'cstp', maskC[:], tn['c_maskC'], [('maskC',)])
        load_w_cast(P, 'cstp', maskW[:], tn['c_maskW'], [('maskW',)])
        P.op('pool', 'memset', dict(ap=ones_row[:], constant=1.0), writes=[('ones_row',)])
        A = Ctx()
        A.jobrot = Rot(2)
        A.psA = Rot(2)
        A.rt = Rot(2)
        A.ptr = Rot(3)
        A.psS = Rot(3)
        A.psO = Rot(2)
        A.fd = Rot(2)

        def load_chunk(segs, bias_d):
            bi = A.jobrot.next()
            off = 0
            for (c0, n) in segs:
                load_w_cast(P, 'wc%d' % bi, wch[bi][:, :, off:off + n],
                            win[:, c0:c0 + n].rearrange('(k p) n -> p k n', p=128), writes=[('wch', bi)])
                if bias_d is not None:
                    load_w_cast(P, 'wc%d' % bi, bch[bi][0:1, off:off + n], bias_d(c0, n).unsqueeze(0),
                                writes=[('bch', bi)])
                off += n
            return bi, off

        def make_rot(bi, rows, has_bias):
            nb = rows // 64
            v = wch[bi][:, :, 0:rows].rearrange('p k (b two r) -> p k b two r', two=2, r=32)
            w = wrot[bi][:, :, 0:rows].rearrange('p k (b two r) -> p k b two r', two=2, r=32)
            for b in range(nb):
                P.op('pool', 'tensor_scalar', dict(out=w[:, :, b, 0, :], in0=v[:, :, b, 1, :], scalar1=-1.0,
                                                   scalar2=None, op0=ALU.mult),
                     reads=[('wch', bi)], writes=[('wrot', bi, b, 0)])
                P.op('pool', 'tensor_copy', dict(out=w[:, :, b, 1, :], in_=v[:, :, b, 0, :]),
                     reads=[('wch', bi)], writes=[('wrot', bi, b, 1)])
            if has_bias:
                v = bch[bi][0:1, 0:rows].rearrange('p (b two r) -> p b two r', two=2, r=32)
                w = brot[bi][0:1, 0:rows].rearrange('p (b two r) -> p b two r', two=2, r=32)
                P.op('pool', 'tensor_scalar', dict(out=w[:, :, 0, :], in0=v[:, :, 1, :], scalar1=-1.0,
                                                   scalar2=None, op0=ALU.mult),
                     reads=[('bch', bi)], writes=[('brot', bi, 0)])
                P.op('pool', 'tensor_copy', dict(out=w[:, :, 1, :], in_=v[:, :, 0, :]),
                     reads=[('bch', bi)], writes=[('brot', bi, 1)])

        def proj_fm_load(segs, rope, evac, bias_d=None):
            bi, rows = load_chunk(segs, bias_d)
            if rope:
                make_rot(bi, rows, bias_d is not None)
            return bi, rows

        def proj_fm(segs, rope, evac, bias_d=None):
            proj_fm_compute(proj_fm_load(segs, rope, evac, bias_d), segs, rope, evac, bias_d)

        def run_fm(jobs):
            hs = {}
            for i in range(len(jobs) + 1):
                if i < len(jobs):
                    hs[i] = proj_fm_load(*jobs[i])
                if i >= 1:
                    proj_fm_compute(hs.pop(i - 1), *jobs[i - 1])

        def proj_fm_compute(h, segs, rope, evac, bias_d=None):
            bi, rows = h
            rotkeys = [('wrot', bi, b, x) for b in range(rows // 64) for x in range(2)]
            for tq in range(4):
                bkA = 2 + A.psA.next()
                for k in range(8):
                    P.op('pe', 'matmul', dict(out=C.ps[bkA][0:rows, :], lhsT=wch[bi][:, k, 0:rows],
                                              rhs=hT[:, k, tq * 512:(tq + 1) * 512], start=(k == 0),
                                              stop=(k == 7 and bias_d is None)),
                         reads=[('wch', bi), ('hT', tq)], writes=[('ps', bkA)])
                if bias_d is not None:
                    P.op('pe', 'matmul', dict(out=C.ps[bkA][0:rows, :], lhsT=bch[bi][0:1, 0:rows],
                                              rhs=ones_row[0:1, :], start=False, stop=True),
                         reads=[('bch', bi), ('ones_row',)], writes=[('ps', bkA)])
                if not rope:
                    evac(tq, C.ps[bkA][0:rows, :], ('ps', bkA))
                    continue
                bkB = bkA + 2
                for k in range(8):
                    P.op('pe', 'matmul', dict(out=C.ps[bkB][0:rows, :], lhsT=wrot[bi][:, k, 0:rows],
                                              rhs=hT[:, k, tq * 512:(tq + 1) * 512], start=(k == 0),
                                              stop=(k == 7 and bias_d is None)),
                         reads=rotkeys + [('hT', tq)], writes=[('ps', bkB)])
                if bias_d is not None:
                    P.op('pe', 'matmul', dict(out=C.ps[bkB][0:rows, :], lhsT=brot[bi][0:1, 0:rows],
                                              rhs=ones_row[0:1, :], start=False, stop=True),
                         reads=[('brot', bi, 0), ('brot', bi, 1), ('ones_row',)], writes=[('ps', bkB)])
                ri = A.rt.next()
                t1, t2 = rtmp[2 * ri], rtmp[2 * ri + 1]
                P.op('dve', 'tensor_tensor', dict(out=t1[0:rows, :], in0=C.ps[bkA][0:rows, :],
                                                  in1=ropeC[0:rows, tq * 512:(tq + 1) * 512], op=ALU.mult),
                     reads=[('ps', bkA), ('ropeC',)], writes=[('rtmp', 2 * ri)])
                P.op('dve', 'tensor_tensor', dict(out=t2[0:rows, :], in0=C.ps[bkB][0:rows, :],
                                                  in1=ropeS[0:rows, tq * 512:(tq + 1) * 512], op=ALU.mult),
                     reads=[('ps', bkB), ('ropeS',)], writes=[('rtmp', 2 * ri + 1)])
                evac(tq, (t1[0:rows, :], t2[0:rows, :]), [('rtmp', 2 * ri), ('rtmp', 2 * ri + 1)])

        def evac_to(dst_fn, wkey_fn, rope):
            def f(tq, src, skeys):
                if rope:
                    P.op('dve', 'tensor_tensor', dict(out=dst_fn(tq), in0=src[0], in1=src[1], op=ALU.add),
                         reads=skeys, writes=[wkey_fn(tq)])
                else:
                    P.op('act', 'copy', dict(out=dst_fn(tq), in_=src), reads=[skeys], writes=[wkey_fn(tq)])
            return f

        def evac_halves(lo_fn, hi_fn, wkey_fn, rope):
            def f(tq, src, skeys):
                for hf, fn in ((0, lo_fn), (1, hi_fn)):
                    rs_ = slice(64 * hf, 64 * hf + 64)
                    if rope:
                        P.op('dve', 'tensor_tensor', dict(out=fn(tq)[rs_, :], in0=src[0][rs_, :], in1=src[1][rs_, :],
                                                           op=ALU.add), reads=skeys, writes=[wkey_fn(tq, hf)])
                    else:
                        P.op('act', 'copy', dict(out=fn(tq)[rs_, :], in_=src[rs_, :]), reads=[skeys],
                             writes=[wkey_fn(tq, hf)])
            return f

        def proj_tm(c0, ncol, out_fn, vkey, wv, bias_d=None):
            load_w_cast(P, 'wv', wv[:, :, 0:ncol], win[:, c0:c0 + ncol].rearrange('(k p) n -> p k n', p=128),
                        writes=[('wv',)])
            if bias_d is not None:
                load_w_cast(P, 'wv', bch[0][0:1, 0:ncol], bias_d(c0, ncol).unsqueeze(0), writes=[('bch', 0)])
            for k4 in range(4):
                bk = 6 + (k4 % 2)
                for j in range(4):
                    kt = k4 * 4 + j
                    for k in range(8):
                        P.op('pe', 'matmul', dict(out=C.ps[bk][:, j * 64:(j + 1) * 64],
                                                  lhsT=hT[:, k, kt * 128:(kt + 1) * 128], rhs=wv[:, k, 0:ncol],
                                                  start=(k == 0), stop=(k == 7 and bias_d is None),
                                                  skip_group_check=True),
                             reads=[('wv',), ('hT', kt // 4)], writes=[('ps', bk)])
                    if bias_d is not None:
                        P.op('pe', 'matmul', dict(out=C.ps[bk][:, j * 64:(j + 1) * 64], lhsT=ones_row[0:1, 0:128],
                                                  rhs=bch[0][0:1, 0:ncol], start=False, stop=True,
                                                  skip_group_check=True),
                             reads=[('bch', 0), ('ones_row',)], writes=[('ps', bk)])
                P.op('act', 'copy', dict(out=out_fn(k4),
                                         in_=C.ps[bk][:, 0:256].rearrange('p (j d) -> p j d', d=64)),
                     reads=[('ps', bk)], writes=[(vkey, k4)])

        def st_tile(qsrc, ksrc, kt, q0, ca, cb, masks, extra=None):
            bk = A.psS.next()
            rhs, rkeys = qsrc(q0 + ca, q0 + cb)
            lhs, lkeys = ksrc(kt)
            last = (not masks) and (extra is None)
            P.op('pe', 'matmul', dict(out=C.ps[bk][:, ca:cb], lhsT=lhs, rhs=rhs, start=True, stop=last,
                                      skip_group_check=True),
                 reads=rkeys + lkeys, writes=[('ps', bk)])
            if extra is not None:
                elhs, erhs, ekeys = extra(kt, q0 + ca, q0 + cb)
                P.op('pe', 'matmul', dict(out=C.ps[bk][:, ca:cb], lhsT=elhs, rhs=erhs, start=False,
                                          stop=(not masks), skip_group_check=True),
                     reads=ekeys, writes=[('ps', bk)])
            for mi, (mt, mkey, bc) in enumerate(masks):
                P.op('pe', 'matmul', dict(out=C.ps[bk][:, bc:bc + 128], lhsT=C.ident[:], rhs=mt, start=False,
                                          stop=(mi == len(masks) - 1), skip_group_check=True),
                     reads=[('ident',), mkey], writes=[('ps', bk)])
            return bk

        def exp_pv(bk, ca, cb, vlhs, vkeys, bo, first, lastpv):
            pi = A.ptr.next()
            P.op('act', 'activation', dict(out=pT[pi][:, ca:cb], in_=C.ps[bk][:, ca:cb], func=AF.Exp, scale=0.125),
                 reads=[('ps', bk)], writes=[('pT', pi)])
            P.op('pe', 'matmul', dict(out=C.ps[bo][:, ca:cb], lhsT=vlhs, rhs=pT[pi][:, ca:cb], start=first,
                                      stop=lastpv, skip_group_check=True),
                 reads=[('pT', pi)] + vkeys, writes=[('ps', bo)])

        def band_tiles(qi, window_tiles, causal_only):
            out = []
            if causal_only:
                js = list(range(-4 * qi, 4))
            else:
                js = [j for j in range(-window_tiles, 4) if 4 * qi + j >= 0]
            js.sort(key=lambda j: (0 if j <= 0 and (causal_only or j + window_tiles >= 3) else 1, j))
            for j in js:
                kt = 4 * qi + j
                ca = max(0, 128 * j)
                masks = []
                if j >= 0:
                    masks.append((maskC[:], ('maskC',), 128 * j))
                if causal_only:
                    cb = 512
                else:
                    cb = min(512, 128 * (j + window_tiles) + 128)
                    jb = j + window_tiles
                    if 0 <= jb <= 3:
                        masks.append((maskW[:], ('maskW',), 128 * jb))
                out.append((kt, ca, cb, masks))
            return out

        for sq in range(SEQ_PER_CORE):
            tb = sq * T
            with ExitStack() as es1:
                sb1 = lambda name, shape, dt: es1.enter_context(nc.sbuf_tensor(uniq(name), shape, dt))
                xts = [sb1('n_xt%d' % i, [128, 2, D], F32) for i in range(3)]
                hb = [sb1('n_hb%d' % i, [128, D], BF16) for i in range(2)]
                junk = sb1('n_junk', [128, D], BF16)
                ss = [sb1('n_ss%d' % i, [128, 2], F32) for i in range(3)]
                rs = [sb1('n_rs%d' % i, [128, 2], F32) for i in range(3)]
                g1 = sb1('a_g1', [128, D], F32)
                P.dma('sp', 'g', dict(out=g1[:], in_=tn['norm_g'][L, 0, :].partition_broadcast(128)), writes=[('g1',)])

                def load_x(ti):
                    sl = ti % 3
                    P.dma('sp', 'x%d' % sl,
                          dict(out=xts[sl][:], in_=ysrc[tb + ti * 256:tb + (ti + 1) * 256, :]
                               .rearrange('(s p) d -> p s d', p=128)),
                          reads=[('y', sq, ti // 2)], writes=[('xt', sl)])

                def stats(ti):
                    sl = ti % 3
                    for s in range(2):
                        P.op('act', 'activation', dict(out=junk[:], in_=xts[sl][:, s, :], func=AF.Square,
                                                       accum_out=ss[sl][:, s:s + 1]),
                             reads=[('xt', sl)], writes=[('junk',), ('ss', sl)])
                    rstd_from_ss(P, C, ss[sl][:, 0:2], rs[sl][:, 0:2], 2, ('ss', sl), ('rs', sl))

                def proc(ti):
                    sl = ti % 3
                    for s in range(2):
                        hs = (ti * 2 + s) % 2
                        P.op('dve', 'scalar_tensor_tensor',
                             dict(out=hb[hs][:], in0=xts[sl][:, s, :], scalar=rs[sl][:, s:s + 1], in1=g1[:],
                                  op0=ALU.mult, op1=ALU.mult),
                             reads=[('xt', sl), ('rs', sl), ('g1',)], writes=[('hb', hs)])
                        pb = C.ps_bf[hs]
                        for c in range(8):
                            P.op('pe', 'transpose', dict(out=pb[:, c, :], in_=hb[hs][:, c * 128:(c + 1) * 128],
                                                         identity=C.ident[:]),
                                 reads=[('hb', hs), ('ident',)], writes=[('ps', hs)])
                        col = ti * 256 + s * 128
                        P.op('act', 'copy', dict(out=hT[:, :, col:col + 128], in_=pb[:, :, :]),
                             reads=[('ps', hs)], writes=[('hT', col // 512)])

                load_x(0)
                load_x(1)
                stats(0)
                for ti in range(8):
                    if ti + 2 < 8:
                        load_x(ti + 2)
                    if ti + 1 < 8:
                        stats(ti + 1)
                    proc(ti)
                P.barrier()

            if kind == 2:
                swa_seq(P, C, A, locals())
            elif kind == 1:
                fox_seq(P, C, A, locals())
            else:
                nsa_seq(P, C, A, locals())

            with ExitStack() as es3:
                sb3 = lambda name, shape, dt: es3.enter_context(nc.sbuf_tensor(uniq(name), shape, dt))
                xt = [sb3('c_xt%d' % i, [128, 2, D], F32) for i in range(2)]
                tmp = [sb3('c_tmp%d' % i, [128, D], F32) for i in range(2)]
                junk = sb3('c_junk', [128, 512], BF16)
                ss2 = sb3('c_ss', [128, 64], F32)
                rs2 = sb3('c_rs', [128, 32], F32)
                bo = sb3('c_bo', [1, D], BF16)
                wo = sb3('a_wo', [128, 8, D], BF16)
                g2 = sb3('a_g2', [128, D], F32)
                for c0 in range(0, 8, 4):
                    load_w_cast(P, 'wA', wo[:, c0:c0 + 4, :],
                                wout_d[c0 * 128:(c0 + 4) * 128, :].rearrange('(c p) n -> p c n', p=128),
                                writes=[('wo', c0 // 4)])
                P.dma('sp', 'g', dict(out=g2[:], in_=tn['norm_g'][L, 1, :].partition_broadcast(128)), writes=[('g2',)])
                if kind == 2:
                    load_w_cast(P, 'wv', bo[0:1, :], tn['swa_b_out'][slot].unsqueeze(0), writes=[('bo',)])
                for ti in range(8):
                    sl = ti % 2
                    P.dma('sp', 'x%d' % sl,
                          dict(out=xt[sl][:], in_=ysrc[tb + ti * 256:tb + (ti + 1) * 256, :]
                               .rearrange('(s p) d -> p s d', p=128)),
                          reads=[('y', sq, ti // 2)], writes=[('cxt', sl)])
                    for s in range(2):
                        kt = ti * 2 + s
                        for dh in range(2):
                            bk = 2 + dh
                            for c in range(8):
                                P.op('pe', 'matmul',
                                     dict(out=C.ps[bk][:, :], lhsT=OT[:, c, kt * 128:(kt + 1) * 128],
                                          rhs=wo[:, c, dh * 512:(dh + 1) * 512], start=(c == 0),
                                          stop=(c == 7 and kind != 2)),
                                     reads=[('OT', c, kt // 4), ('wo', c // 4)], writes=[('ps', bk)])
                            if kind == 2:
                                P.op('pe', 'matmul',
                                     dict(out=C.ps[bk][:, :], lhsT=ones_row[0:1, 0:128],
                                          rhs=bo[0:1, dh * 512:(dh + 1) * 512], start=False, stop=True),
                                     reads=[('bo',), ('ones_row',)], writes=[('ps', bk)])
                            P.op('act', 'activation', dict(out=junk[:, :], in_=C.ps[bk][:, :], func=AF.Square,
                                                           accum_out=ss2[:, 2 * kt + dh:2 * kt + dh + 1]),
                                 reads=[('ps', bk)], writes=[('cjunk',), ('css', kt, dh)])
                        P.op('dve', 'tensor_tensor', dict(out=ss2[:, 2 * kt:2 * kt + 1], in0=ss2[:, 2 * kt:2 * kt + 1],
                                                          in1=ss2[:, 2 * kt + 1:2 * kt + 2], op=ALU.add),
                             reads=[('css', kt, 0), ('css', kt, 1)], writes=[('css', kt, 0)])
                        rstd_from_ss(P, C, ss2[:, 2 * kt:2 * kt + 1], rs2[:, kt:kt + 1], 1, ('css', kt, 0), ('crs', kt))
                        tm = tmp[s % 2]
                        for dh in range(2):
                            bk = 2 + dh
                            P.op('dve', 'scalar_tensor_tensor',
                                 dict(out=tm[:, dh * 512:(dh + 1) * 512], in0=C.ps[bk][:, :], scalar=rs2[:, kt:kt + 1],
                                      in1=g2[:, dh * 512:(dh + 1) * 512], op0=ALU.mult, op1=ALU.mult),
                                 reads=[('ps', bk), ('crs', kt), ('g2',)], writes=[('ctmp', s % 2, dh)])
                        P.op('pool', 'tensor_tensor', dict(out=xt[sl][:, s, :], in0=xt[sl][:, s, :], in1=tm[:],
                                                           op=ALU.add),
                             reads=[('cxt', sl), ('ctmp', s % 2, 0), ('ctmp', s % 2, 1)], writes=[('cxt', sl)])
                    P.dma('sp', 'yo%d' % sl,
                          dict(out=ydst[tb + ti * 256:tb + (ti + 1) * 256, :].rearrange('(s p) d -> p s d', p=128),
                               in_=xt[sl][:]),
                          reads=[('cxt', sl)], writes=[('y', sq, ti // 2)])
                P.barrier()


def run_jobs(jobs, LA=2):
    banks = {}
    n = len(jobs)
    for i in range(n + LA + 1):
        if i < n:
            if jobs[i].get('pre'):
                jobs[i]['pre']()
            banks[i] = jobs[i]['st']()
        k = i - LA
        if 0 <= k < n:
            jobs[k]['ep'](banks.pop(k))
        if 0 <= k - 1 < n and jobs[k - 1].get('fin'):
            jobs[k - 1]['fin']()


class NS:
    def __init__(self, d):
        self.__dict__.update(d)


def finish_simple(P, C, A, E, bo, rows, chunk, q0, addk):
    fi = A.fd.next()
    fd = E.fden[fi]
    fkey = ('fden', fi)
    if addk is not None:
        P.op('act', 'activation', dict(out=fd[64:128, :], in_=C.ps[bo][64:128, :], func=AF.Ln, bias=addk, scale=1.0),
             reads=[('ps', bo), ('esk',)], writes=[fkey])
    else:
        P.op('act', 'activation', dict(out=fd[64:128, :], in_=C.ps[bo][64:128, :], func=AF.Ln),
             reads=[('ps', bo)], writes=[fkey])
    P.op('act', 'activation', dict(out=fd[64:128, :], in_=fd[64:128, :], func=AF.Exp, scale=-1.0), reads=[fkey],
         writes=[fkey])
    P.op('dve', 'tensor_tensor', dict(out=E.OT[rows, chunk, q0:q0 + 512], in0=C.ps[bo][0:64, :], in1=fd[64:128, :],
                                      op=ALU.mult),
         reads=[('ps', bo), fkey], writes=[('OT', chunk, q0 // 512)])


def swa_seq(P, C, A, Ed):
    E = NS(Ed)
    nc, tn = C.nc, C.t
    b_in = tn['swa_b_in'][E.slot]
    bias_fn = lambda c0, n: b_in[c0:c0 + n]
    for g in range(2):
        with ExitStack() as es2:
            sb = lambda name, shape, dt: es2.enter_context(nc.sbuf_tensor(uniq(name), shape, dt))
            qT = sb('s_qT', [128, 4, T], BF16)
            kdl = sb('s_kdl', [128, T], BF16)
            kdh = sb('s_kdh', [128, T], BF16)
            Vt = sb('s_V', [128, 16, 2, 64], BF16)
            wv = sb('s_wv', [128, 8, 64], BF16)
            esk = sb('s_esk', [128, 16], F32)
            P.op('pool', 'memset', dict(ap=Vt[:, :, 1, :], constant=1.0), writes=[('Vones',)])
            P.op('pool', 'memset', dict(ap=kdl[64:128, :], constant=0.0), writes=[('kdz', 0)])
            P.op('pool', 'memset', dict(ap=kdh[0:64, :], constant=0.0), writes=[('kdz', 1)])
            P.dma('sp', 'g', dict(out=esk[:], in_=tn['swa_sinks'][E.slot].partition_broadcast(128)), writes=[('esk',)])
            P.op('act', 'activation', dict(out=esk[:], in_=esk[:], func=AF.Exp), reads=[('esk',)], writes=[('esk',)])
            pj = []
            for j in range(4):
                pj.append(([(g * 512 + j * 128, 128)], True,
                          E.evac_to(lambda tq, j=j: qT[:, j, tq * 512:(tq + 1) * 512],
                                    lambda tq, j=j: ('qT', j, tq), True), bias_fn))
            pj.append(([(1024 + g * 64, 64)] * 2, True,
                      E.evac_halves(lambda tq: kdl[:, tq * 512:(tq + 1) * 512], lambda tq: kdh[:, tq * 512:(tq + 1) * 512],
                                    lambda tq, hf: ('kd', hf, tq), True), bias_fn))
            E.run_fm(pj)
            E.proj_tm(1152 + g * 64, 64, lambda k4: Vt[:, k4 * 4:(k4 + 1) * 4, 0, :], 'Vt', wv, bias_fn)
            jobs = []
            for r in range(8):
                h = 8 * g + r
                j, half = r // 2, r % 2
                rows = slice(64 * half, 64 * half + 64)
                qsrc = lambda c0, c1, j=j: (qT[:, j, c0:c1], [('qT', j, c0 // 512)])
                ksrc = lambda kt, half=half: ((kdh if half else kdl)[:, kt * 128:(kt + 1) * 128], [('kd', half, kt // 4), ('kdz', half)])
                for qi in range(4):
                    tiles = E.band_tiles(qi, 1, False)
                    bo = 3 + A.psO.next()
                    for n_, (kt, ca, cb, masks) in enumerate(tiles):
                        job = dict(
                            st=lambda qsrc=qsrc, ksrc=ksrc, kt=kt, qi=qi, ca=ca, cb=cb, masks=masks:
                            E.st_tile(qsrc, ksrc, kt, qi * 512, ca, cb, masks),
                            ep=lambda bk, kt=kt, ca=ca, cb=cb, bo=bo, f=(n_ == 0), l=(n_ == len(tiles) - 1):
                            E.exp_pv(bk, ca, cb, Vt[:, kt, :, :].rearrange('p a d -> p (a d)'),
                                     [('Vt', kt // 4), ('Vones',)], bo, f, l))
                        if n_ == len(tiles) - 1:
                            job['fin'] = (lambda bo=bo, rows=rows, j=j, qi=qi, h=h:
                                          finish_simple(P, C, A, E, bo, rows, 4 * g + j, qi * 512, esk[64:128, h:h + 1]))
                        jobs.append(job)
            run_jobs(jobs)
            P.barrier()


def fox_seq(P, C, A, Ed):
    E = NS(Ed)
    nc, tn = C.nc, C.t
    b_f = tn['fox_b_f'][E.slot]
    scrQ, scrK = C.scrQ, C.scrK
    with ExitStack() as es2:
        sb = lambda name, shape, dt: es2.enter_context(nc.sbuf_tensor(uniq(name), shape, dt))
        spt = sb('f_spt', [16, T], F32)
        cs = sb('f_cs', [16, T], F32)
        ones16 = sb('f_ones', [16, T], F32)
        res_ = sb('f_res', [16, T], F32)
        parts = [sb('f_a%d' % i, [16, T], BF16) for i in range(3)]
        nparts = [sb('f_n%d' % i, [16, T], BF16) for i in range(3)]
        P.op('pool', 'memset', dict(ap=ones16[:], constant=1.0), writes=[('ones16',)])
        onesb = sb('f_onesb', [16, T], BF16)
        P.op('pool', 'memset', dict(ap=onesb[:], constant=1.0), writes=[('onesb',)])

        def evac_fl(tq, src, skey):
            sl = spt[0:16, tq * 512:(tq + 1) * 512]
            P.op('act', 'activation', dict(out=sl, in_=src, func=AF.Exp, scale=-1.0), reads=[skey],
                 writes=[('spt', tq)])
            P.op('act', 'activation', dict(out=sl, in_=sl, func=AF.Ln, bias=C.one[0:16, 0:1], scale=1.0),
                 reads=[('spt', tq)], writes=[('spt', tq)])
        E.proj_fm([(3072, 16)], False, evac_fl, lambda c0, n: b_f[0:16])
        P.op('dve', 'tensor_tensor_scan', dict(out=cs[:], data0=ones16[:], data1=spt[:], initial=0.0,
                                               op0=ALU.mult, op1=ALU.add),
             reads=[('ones16',)] + [('spt', tq) for tq in range(4)], writes=[('cs',)])
        P.op('dve', 'tensor_scalar', dict(out=cs[:], in0=cs[:], scalar1=8.0, scalar2=None, op0=ALU.mult),
             reads=[('cs',)], writes=[('cs',)])
        cur = cs
        ckey = ('cs',)
        for i in range(3):
            P.op('dve', 'tensor_copy', dict(out=parts[i][:], in_=cur[:]), reads=[ckey], writes=[('part', i)])
            P.op('dve', 'tensor_scalar', dict(out=nparts[i][:], in0=parts[i][:], scalar1=-1.0, scalar2=None,
                                              op0=ALU.mult), reads=[('part', i)], writes=[('npart', i)])
            if i < 2:
                P.op('dve', 'tensor_tensor', dict(out=res_[:], in0=cur[:], in1=parts[i][:], op=ALU.subtract),
                     reads=[ckey, ('part', i)], writes=[('res',)])
                cur, ckey = res_, ('res',)
            P.dma('sp', 'scr', dict(out=scrQ[i], in_=nparts[i][:]), reads=[('npart', i)], writes=[('scrQ', i)])
            P.dma('sp', 'scr', dict(out=scrK[3 + i], in_=parts[i][:]), reads=[('part', i)], writes=[('scrK', 3 + i)])
            P.dma('sp', 'scr', dict(out=scrQ[3 + i], in_=onesb[:]), reads=[('onesb',)], writes=[('scrQ', 3 + i)])
            P.dma('sp', 'scr', dict(out=scrK[i], in_=onesb[:]), reads=[('onesb',)], writes=[('scrK', i)])
        P.barrier()
    for gp in range(4):
        with ExitStack() as es2:
            sb = lambda name, shape, dt: es2.enter_context(nc.sbuf_tensor(uniq(name), shape, dt))
            qT = sb('f_qT', [128, 2, T], BF16)
            kTl = sb('f_kTl', [128, 2, T], BF16)
            kTh = sb('f_kTh', [128, 2, T], BF16)
            Vt = sb('f_V', [128, 16, 4, 2, 64], BF16)
            wv = sb('f_wv', [128, 8, 64], BF16)
            cq = sb('f_cq', [128, 4, T], BF16)
            ck = sb('f_ck', [128, 4, T], BF16)
            P.op('pool', 'memset', dict(ap=Vt[:, :, :, 1, :], constant=1.0), writes=[('Vones',)])
            P.op('pool', 'memset', dict(ap=cq[:], constant=0.0), writes=[('cq',)])
            P.op('pool', 'memset', dict(ap=ck[:], constant=0.0), writes=[('ck',)])
            P.op('pool', 'memset', dict(ap=kTl[64:128, :, :], constant=0.0), writes=[('kTz', 0)])
            P.op('pool', 'memset', dict(ap=kTh[0:64, :, :], constant=0.0), writes=[('kTz', 1)])
            P.dma('sp', 'scr2', dict(out=cq[0:6, :, :], in_=scrQ[:, 4 * gp:4 * gp + 4, :]),
                  reads=[('scrQ', i) for i in range(6)] + [('cq',)], writes=[('cq',)])
            P.dma('sp', 'scr2', dict(out=ck[0:6, :, :], in_=scrK[:, 4 * gp:4 * gp + 4, :]),
                  reads=[('scrK', i) for i in range(6)] + [('ck',)], writes=[('ck',)])
            pj = []
            for j in range(2):
                pj.append(([(gp * 256 + j * 128, 128)], False,
                          E.evac_to(lambda tq, j=j: qT[:, j, tq * 512:(tq + 1) * 512],
                                    lambda tq, j=j: ('qT', j, tq), False)))
                pj.append(([(1024 + gp * 256 + j * 128, 128)], False,
                          E.evac_halves(lambda tq, j=j: kTl[:, j, tq * 512:(tq + 1) * 512],
                                        lambda tq, j=j: kTh[:, j, tq * 512:(tq + 1) * 512],
                                        lambda tq, hf, j=j: ('kT', j, hf, tq), False)))
            E.run_fm(pj)
            for r in range(4):
                E.proj_tm(2048 + (4 * gp + r) * 64, 64, lambda k4, r=r: Vt[:, k4 * 4:(k4 + 1) * 4, r, 0, :],
                          ('Vt', r), wv)
            jobs = []
            for r in range(4):
                j, half = r // 2, r % 2
                rows = slice(64 * half, 64 * half + 64)
                qsrc = lambda c0, c1, j=j: (qT[:, j, c0:c1], [('qT', j, c0 // 512)])
                ksrc = lambda kt, half=half, j=j: ((kTh if half else kTl)[:, j, kt * 128:(kt + 1) * 128], [('kT', j, half, kt // 4), ('kTz', half)])
                extra = lambda kt, c0, c1, r=r: (ck[:, r, kt * 128:(kt + 1) * 128], cq[:, r, c0:c1], [('cq',), ('ck',)])
                for qi in range(4):
                    tiles = E.band_tiles(qi, None, True)
                    bo = 3 + A.psO.next()
                    for n_, (kt, ca, cb, masks) in enumerate(tiles):
                        job = dict(
                            st=lambda qsrc=qsrc, ksrc=ksrc, extra=extra, kt=kt, qi=qi, ca=ca, cb=cb, masks=masks:
                            E.st_tile(qsrc, ksrc, kt, qi * 512, ca, cb, masks, extra),
                            ep=lambda bk, kt=kt, ca=ca, cb=cb, bo=bo, r=r, f=(n_ == 0), l=(n_ == len(tiles) - 1):
                            E.exp_pv(bk, ca, cb, Vt[:, kt, r, :, :].rearrange('p a d -> p (a d)'),
                                     [(('Vt', r), kt // 4), ('Vones',)], bo, f, l))
                        if n_ == len(tiles) - 1:
                            job['fin'] = (lambda bo=bo, rows=rows, j=j, qi=qi:
                                          finish_simple(P, C, A, E, bo, rows, 2 * gp + j, qi * 512, None))
                        jobs.append(job)
            run_jobs(jobs)
            P.barrier()


def nsa_seq(P, C, A, Ed):
    E = NS(Ed)
    nc, tn = C.nc, C.t
    slot = E.slot
    with ExitStack() as esL:
        sbL = lambda name, shape, dt: esL.enter_context(nc.sbuf_tensor(uniq(name), shape, dt))
        w1 = [sbL('n_w1%d' % i, [128, 16, 256], BF16) for i in range(2)]
        w2k = sbL('n_w2k', [128, 2, 128], BF16)
        w2v = sbL('n_w2v', [128, 2, 64], BF16)
        posf = sbL('n_posf', [128, 2, 16], F32)
        posS = sbL('n_posS', [128, 2, 16, 2], BF16)
        biasT = sbL('n_biasT', [128, 2, 2], F32)
        cmaskT = sbL('n_cmaskT', [128, T], BF16)
        cmask = sbL('n_cmask', [128, 8, 128], BF16)
        eexp = sbL('n_eexp', [128, T], BF16)
        bonus = sbL('n_bonus', [128, 8, 32], F32)
        gsel = sbL('n_gsel', [12, 768], BF16)
        load_w_cast(P, 'cstp', cmaskT[:], tn['c_cmaskT'], [('cmaskT',)])
        load_w_cast(P, 'cstp', cmask[:], tn['c_cmask'], [('cmask',)])
        P.op('pool', 'memset', dict(ap=eexp[:], constant=0.0), writes=[('eexp',)])
        load_w_cast(P, 'cstp', eexp[0:32, :], tn['c_eexp'], [('eexp',)])
        load_w_cast(P, 'cstp', gsel[:], tn['c_gsel'], [('gsel',)])
        P.dma('sp', 'cst', dict(out=bonus[:], in_=tn['c_bonus']), writes=[('bonus',)])
        for kv in range(2):
            for c0 in range(0, 16, 8):
                load_w_cast(P, 'wA', w1[kv][:, c0:c0 + 8, :],
                            tn['nsa_cmp_w1'][slot, kv, c0 * 128:(c0 + 8) * 128, :]
                            .rearrange('(c p) n -> p c n', p=128), [('w1', kv)])
            pr = tn['nsa_cmp_pos'][slot, kv].rearrange('(c two) d -> two d c', two=2)
            for two in range(2):
                P.dma('sp', 'cst', dict(out=posf[64 * two:64 * two + 64, kv, :], in_=pr[two],
                                        allow_slow_non_contiguous=True), writes=[('posf', kv, two)])
            for x in range(2):
                P.op('pool', 'tensor_copy', dict(out=posS[:, kv, :, x], in_=posf[:, kv, :]),
                     reads=[('posf', kv, 0), ('posf', kv, 1)], writes=[('posS', kv, x)])
        w2d = tn['nsa_cmp_w2'][slot]
        for dup in range(2):
            load_w_cast(P, 'wA', w2k[:, :, 64 * dup:64 * dup + 64], w2d[0].rearrange('(c p) n -> p c n', p=128),
                        [('w2k', dup)])
        load_w_cast(P, 'wA', w2v[:, :, :], w2d[1].rearrange('(c p) n -> p c n', p=128), [('w2v',)])
        for kv in range(2):
            for hc in range(2):
                for c2 in range(16):
                    P.op('pe', 'matmul', dict(out=C.ps[7][:, 0:2], lhsT=w1[kv][:, c2, hc * 128:(hc + 1) * 128],
                                              rhs=posS[:, kv, c2, :], start=(c2 == 0), stop=(c2 == 15)),
                         reads=[('w1', kv), ('posS', kv, 0), ('posS', kv, 1)], writes=[('ps', 7)])
                P.op('act', 'copy', dict(out=biasT[:, kv, hc:hc + 1], in_=C.ps[7][:, 0:1]),
                     reads=[('ps', 7)], writes=[('biasT', kv, hc)])
        P.barrier()

        for g in range(4):
            with ExitStack() as es2:
                sb = lambda name, shape, dt: es2.enter_context(nc.sbuf_tensor(uniq(name), shape, dt))
                qT = sb('n_qT', [128, 2, T], BF16)
                ksT = [sb('n_ksT%d' % i, [128, T], BF16) for i in range(2)]
                kwT = [sb('n_kwT%d' % i, [128, T], BF16) for i in range(2)]
                cS = [sb('n_cS%d' % i, [128, T], BF16) for i in range(2)]
                Vs = sb('n_Vs', [128, 16, 2, 64], BF16)
                Vw = sb('n_Vw', [128, 16, 2, 64], BF16)
                wv = sb('n_wv', [128, 8, 64], BF16)
                gTh = sb('n_gTh', [12, T], BF16)
                kcmpT = [sb('n_kcmpT%d' % i, [128, 128], BF16) for i in range(2)]
                Vc = sb('n_Vc', [128, 2, 64], BF16)
                hidT = [sb('n_hid%d' % i, [128, 2, 128], BF16) for i in range(2)]
                negselT = sb('n_negselT', [128, T], BF16)
                Pn = sb('n_Pn', [128, 4, 128], F32)
                Ps8 = sb('n_Ps8', [128, 8, 128], F32)
                imp8 = sb('n_imp8', [128, 8, 32], F32)
                sc2 = sb('n_sc2', [128, 32], F32)
                m1 = sb('n_m1', [128, 8], F32)
                m2 = sb('n_m2', [128, 8, 8], F32)
                den4 = sb('n_den4', [128, 8, 4], F32)
                negsel = sb('n_negsel', [128, 8, 32], BF16)
                acc = [sb('n_acc%d' % i, [64, 512], F32) for i in range(2)]
                ctmp = [sb('n_ctmp%d' % i, [64, 512], F32) for i in range(2)]
                P.op('pool', 'memset', dict(ap=Vs[:, :, 1, :], constant=1.0), writes=[('Vsones',)])
                P.op('pool', 'memset', dict(ap=Vw[:, :, 1, :], constant=1.0), writes=[('Vwones',)])
                P.op('pool', 'memset', dict(ap=Vc[:, 1, :], constant=1.0), writes=[('Vc1',)])
                P.op('pool', 'memset', dict(ap=Vc[:, 0, :], constant=0.0), writes=[('Vc0',)])
                for i in range(2):
                    P.op('pool', 'memset', dict(ap=kcmpT[i][:], constant=0.0), writes=[('kcmpT', i)])
                    P.op('pool', 'memset', dict(ap=ksT[i][64 * (1 - i):64 * (1 - i) + 64, :], constant=0.0), writes=[('ksz', i)])
                    P.op('pool', 'memset', dict(ap=kwT[i][64 * (1 - i):64 * (1 - i) + 64, :], constant=0.0), writes=[('kwz', i)])
                P.op('pool', 'memset', dict(ap=negselT[:], constant=0.0), writes=[('negselT',)])
                for i in range(2):
                    P.op('pool', 'memset', dict(ap=hidT[i][:], constant=0.0), writes=[('hidT', i)])

                pj = []
                for j in range(2):
                    pj.append(([(g * 256 + j * 128, 128)], True,
                              E.evac_to(lambda tq, j=j: qT[:, j, tq * 512:(tq + 1) * 512],
                                        lambda tq, j=j: ('qT', j, tq), True)))
                pj.append(([(1536 + g * 64, 64)] * 2, True,
                          E.evac_halves(lambda tq: ksT[0][:, tq * 512:(tq + 1) * 512], lambda tq: ksT[1][:, tq * 512:(tq + 1) * 512],
                                        lambda tq, hf: ('ksT', hf, tq), True)))
                pj.append(([(2048 + g * 64, 64)] * 2, True,
                          E.evac_halves(lambda tq: kwT[0][:, tq * 512:(tq + 1) * 512], lambda tq: kwT[1][:, tq * 512:(tq + 1) * 512],
                                        lambda tq, hf: ('kwT', hf, tq), True)))

                def evac_shift(i, rope):
                    def f(tq, src, skeys):
                        lo, hi = tq * 512, (tq + 1) * 512
                        if rope:
                            a, b = src
                            P.op('dve', 'tensor_tensor', dict(out=cS[i][0:64, lo:hi], in0=a[0:64, :], in1=b[0:64, :],
                                                               op=ALU.add), reads=skeys, writes=[('cS', i, tq, 0)])
                            if tq == 0:
                                P.op('dve', 'tensor_tensor', dict(out=cS[i][64:128, 0:511], in0=a[64:128, 1:512],
                                                                   in1=b[64:128, 1:512], op=ALU.add),
                                     reads=skeys, writes=[('cS', i, tq, 1)])
                            else:
                                P.op('dve', 'tensor_tensor', dict(out=cS[i][64:128, lo - 1:hi - 1], in0=a[64:128, :],
                                                                   in1=b[64:128, :], op=ALU.add),
                                     reads=skeys, writes=[('cS', i, tq, 1)])
                        else:
                            P.op('act', 'copy', dict(out=cS[i][0:64, lo:hi], in_=src[0:64, :]), reads=[skeys],
                                 writes=[('cS', i, tq, 0)])
                            if tq == 0:
                                P.op('act', 'copy', dict(out=cS[i][64:128, 0:511], in_=src[64:128, 1:512]),
                                     reads=[skeys], writes=[('cS', i, tq, 1)])
                            else:
                                P.op('act', 'copy', dict(out=cS[i][64:128, lo - 1:hi - 1], in_=src[64:128, :]),
                                     reads=[skeys], writes=[('cS', i, tq, 1)])
                    return f
                pj.append(([(1024 + g * 64, 64)] * 2, True, evac_shift(0, True)))
                pj.append(([(1280 + g * 64, 64)] * 2, False, evac_shift(1, False)))

                def evac_gate(tq, src, skey):
                    ri = A.rt.next()
                    gf = E.rtmp[2 * ri]
                    gk = ('rtmp', 2 * ri)
                    P.op('act', 'activation', dict(out=gf[0:12, :], in_=src, func=AF.Sigmoid), reads=[skey], writes=[gk])
                    P.op('dve', 'tensor_copy', dict(out=gTh[0:12, tq * 512:(tq + 1) * 512], in_=gf[0:12, :]),
                         reads=[gk], writes=[('gTh', tq)])
                pj.append(([(2560 + 12 * g, 12)], False, evac_gate))
                E.run_fm(pj)
                E.proj_tm(1792 + g * 64, 64, lambda k4: Vs[:, k4 * 4:(k4 + 1) * 4, 0, :], 'Vs', wv)
                E.proj_tm(2304 + g * 64, 64, lambda k4: Vw[:, k4 * 4:(k4 + 1) * 4, 0, :], 'Vw', wv)

                cSkeys = lambda i: [('cS', i, tq, x) for tq in range(4) for x in range(2)]
                for kv in range(2):
                    for hc in range(2):
                        for c2 in range(16):
                            P.op('pe', 'matmul',
                                 dict(out=C.ps[7][:, hc * 128:hc * 128 + 127], lhsT=w1[kv][:, c2, hc * 128:(hc + 1) * 128],
                                      rhs=cS[kv][:, 2 * c2:2 * c2 + 2017:16], start=(c2 == 0), stop=(c2 == 15),
                                      skip_group_check=True),
                                 reads=[('w1', kv)] + cSkeys(kv), writes=[('ps', 7)])
                        P.op('act', 'activation',
                             dict(out=hidT[kv][:, hc, 0:127], in_=C.ps[7][:, hc * 128:hc * 128 + 127], func=AF.Silu,
                                  bias=biasT[:, kv, hc:hc + 1], scale=1.0),
                             reads=[('ps', 7), ('biasT', kv, hc)], writes=[('hidT', kv)])
                for hc in range(2):
                    P.op('pe', 'matmul', dict(out=C.ps[6][:, 0:127], lhsT=w2k[:, hc, :], rhs=hidT[0][:, hc, 0:127],
                                              start=(hc == 0), stop=(hc == 1)),
                         reads=[('w2k', 0), ('w2k', 1), ('hidT', 0)], writes=[('ps', 6)])
                for i in range(2):
                    P.op('act', 'copy', dict(out=kcmpT[i][64 * i:64 * i + 64, 0:127], in_=C.ps[6][64 * i:64 * i + 64, 0:127]),
                         reads=[('ps', 6), ('kcmpT', i)], writes=[('kcmpT', i)])
                for hc in range(2):
                    P.op('pe', 'matmul', dict(out=C.ps[6][0:127, 256:320], lhsT=hidT[1][:, hc, 0:127], rhs=w2v[:, hc, :],
                                              start=(hc == 0), stop=(hc == 1), skip_group_check=True),
                         reads=[('w2v',), ('hidT', 1)], writes=[('ps', 6)])
                P.op('act', 'copy', dict(out=Vc[0:127, 0, :], in_=C.ps[6][0:127, 256:320]), reads=[('ps', 6), ('Vc0',)],
                     writes=[('Vc0',)])

                for tt in range(8):
                    t0 = 1024 + tt * 128
                    for r in range(4):
                        j, half = r // 2, r % 2
                        rows = slice(64 * half, 64 * half + 64)
                        P.op('pe', 'matmul', dict(out=C.ps[5][:, r * 128:(r + 1) * 128], lhsT=qT[:, j, t0:t0 + 128],
                                                  rhs=kcmpT[half][:, :], start=True, stop=False, skip_group_check=True),
                             reads=[('qT', j, t0 // 512), ('kcmpT', half)], writes=[('ps', 5)])
                        P.op('pe', 'matmul', dict(out=C.ps[5][:, r * 128:(r + 1) * 128], lhsT=C.ident[:],
                                                  rhs=cmask[:, tt, :], start=False, stop=True, skip_group_check=True),
                             reads=[('ident',), ('cmask',)], writes=[('ps', 5)])
                    for r in range(4):
                        P.op('act', 'activation', dict(out=Pn[:, r, :], in_=C.ps[5][:, r * 128:(r + 1) * 128],
                                                       func=AF.Exp, scale=0.125, accum_out=den4[:, tt, r:r + 1]),
                             reads=[('ps', 5)], writes=[('Pn', r), ('den4', tt, r)])
                    dk = [('den4', tt, r) for r in range(4)]
                    P.op('dve', 'tensor_scalar', dict(out=den4[:, tt, :], in0=den4[:, tt, :], scalar1=1e-30, scalar2=None,
                                                      op0=ALU.max), reads=dk, writes=[('den4', tt)])
                    P.op('dve', 'reciprocal', dict(out=den4[:, tt, :], in_=den4[:, tt, :]), reads=[('den4', tt)],
                         writes=[('den4', tt)])
                    P.op('dve', 'tensor_scalar', dict(out=Ps8[:, tt, :], in0=Pn[:, 0, :], scalar1=den4[:, tt, 0:1],
                                                      scalar2=None, op0=ALU.mult),
                         reads=[('Pn', 0), ('den4', tt)], writes=[('Ps8', tt)])
                    for r in range(1, 4):
                        P.op('dve', 'scalar_tensor_tensor',
                             dict(out=Ps8[:, tt, :], in0=Pn[:, r, :], scalar=den4[:, tt, r:r + 1], in1=Ps8[:, tt, :],
                                  op0=ALU.mult, op1=ALU.add),
                             reads=[('Pn', r), ('den4', tt), ('Ps8', tt)], writes=[('Ps8', tt)])
                pk = [('Ps8', tt) for tt in range(8)]
                Pv = Ps8[:].rearrange('p t (n i) -> p t n i', i=4)
                P.op('dve', 'tensor_tensor', dict(out=imp8[:], in0=Pv[:, :, :, 0], in1=Pv[:, :, :, 1], op=ALU.add),
                     reads=pk, writes=[('imp8',)])
                P.op('dve', 'tensor_tensor', dict(out=imp8[:], in0=imp8[:], in1=Pv[:, :, :, 2], op=ALU.add),
                     reads=pk + [('imp8',)], writes=[('imp8',)])
                P.op('dve', 'scalar_tensor_tensor', dict(out=imp8[:], in0=Pv[:, :, :, 3], scalar=0.5, in1=imp8[:],
                                                         op0=ALU.mult, op1=ALU.add),
                     reads=pk + [('imp8',)], writes=[('imp8',)])
                P.op('dve', 'scalar_tensor_tensor', dict(out=imp8[:, :, 1:32], in0=Pv[:, :, 0:31, 3], scalar=0.5,
                                                         in1=imp8[:, :, 1:32], op0=ALU.mult, op1=ALU.add),
                     reads=pk + [('imp8',)], writes=[('imp8',)])
                P.op('dve', 'tensor_tensor', dict(out=imp8[:], in0=imp8[:], in1=bonus[:], op=ALU.add),
                     reads=[('imp8',), ('bonus',)], writes=[('imp8',)])
                for tt in range(8):
                    P.op('dve', 'max', dict(out=m1[:], in_=imp8[:, tt, :]), reads=[('imp8',)], writes=[('m1',)])
                    P.op('dve', 'match_replace', dict(out=sc2[:], in_to_replace=m1[:], in_values=imp8[:, tt, :],
                                                      imm_value=-1e30), reads=[('m1',), ('imp8',)], writes=[('sc2',)])
                    P.op('dve', 'max', dict(out=m2[:, tt, :], in_=sc2[:]), reads=[('sc2',)], writes=[('m2', tt)])
                    P.op('dve', 'tensor_scalar', dict(out=negsel[:, tt, :], in0=imp8[:, tt, :], scalar1=m2[:, tt, 7:8],
                                                      scalar2=NEG, op0=ALU.is_lt, op1=ALU.mult),
                         reads=[('imp8',), ('m2', tt)], writes=[('negsel', tt)])
                    P.op('pe', 'transpose', dict(out=C.ps_bf[6][0:32, tt, :], in_=negsel[:, tt, :], identity=C.ident[:]),
                         reads=[('negsel', tt), ('ident',)], writes=[('ps', 6)])
                P.op('act', 'copy', dict(out=negselT[0:32, 1024:2048],
                                         in_=C.ps_bf[6][0:32, :, :].rearrange('p a b -> p (a b)')),
                     reads=[('ps', 6)], writes=[('negselT',)])

                jobs = []
                first_gate = [True]

                def gate_pre(r, qi, par):
                    q0 = qi * 512
                    gk = [('gTh', qi), ('gsel',)]
                    b01 = 5 if par == 0 else 7
                    P.op('pe', 'matmul', dict(out=C.ps[b01][:, :], lhsT=gsel[0:12, (3 * r) * 64:(3 * r + 2) * 64],
                                              rhs=gTh[0:12, q0:q0 + 512], start=True, stop=True),
                         reads=gk, writes=[('ps', b01)])
                    wk = [('ps6h', par)]
                    if first_gate[0]:
                        wk = [('ps', 6), ('ps6h', 0), ('ps6h', 1)]
                        first_gate[0] = False
                    P.op('pe', 'matmul', dict(out=C.ps[6][64 * par:64 * par + 64, :],
                                              lhsT=gsel[0:12, (3 * r + 2) * 64:(3 * r + 3) * 64],
                                              rhs=gTh[0:12, q0:q0 + 512], start=True, stop=True,
                                              skip_group_check=True),
                         reads=gk, writes=wk)

                def combine(b, bo, r, qi, par, rows, j):
                    q0 = qi * 512
                    b01 = 5 if par == 0 else 7
                    gap = [C.ps[b01][0:64, :], C.ps[b01][64:128, :], C.ps[6][64 * par:64 * par + 64, :]][b]
                    gkey = [('ps', b01), ('ps', b01), ('ps6h', par)][b]
                    ai = par
                    fi = A.fd.next()
                    fd = E.fden[fi]
                    fk = ('fden', fi)
                    P.op('act', 'activation', dict(out=fd[64:128, :], in_=C.ps[bo][64:128, :], func=AF.Ln),
                         reads=[('ps', bo)], writes=[fk])
                    P.op('act', 'activation', dict(out=fd[64:128, :], in_=fd[64:128, :], func=AF.Exp, scale=-1.0),
                         reads=[fk], writes=[fk])
                    P.op('dve', 'tensor_tensor', dict(out=fd[0:64, :], in0=gap, in1=fd[64:128, :], op=ALU.mult),
                         reads=[gkey, fk], writes=[fk])
                    if b == 0:
                        P.op('dve', 'tensor_tensor', dict(out=acc[ai][:], in0=C.ps[bo][0:64, :], in1=fd[0:64, :],
                                                          op=ALU.mult),
                             reads=[('ps', bo), fk], writes=[('acc', ai)])
                    else:
                        ci = b - 1
                        P.op('dve', 'tensor_tensor', dict(out=ctmp[ci][:], in0=C.ps[bo][0:64, :], in1=fd[0:64, :],
                                                          op=ALU.mult),
                             reads=[('ps', bo), fk], writes=[('ctmp', ci)])
                        if b == 1:
                            P.op('pool', 'tensor_tensor', dict(out=acc[ai][:], in0=acc[ai][:], in1=ctmp[ci][:],
                                                               op=ALU.add),
                                 reads=[('acc', ai), ('ctmp', ci)], writes=[('acc', ai)])
                        else:
                            P.op('pool', 'tensor_tensor',
                                 dict(out=E.OT[rows, 2 * g + j, q0:q0 + 512], in0=acc[ai][:], in1=ctmp[ci][:],
                                      op=ALU.add),
                                 reads=[('acc', ai), ('ctmp', ci)], writes=[('OT', 2 * g + j, qi)])

                for r in range(4):
                    j, half = r // 2, r % 2
                    rows = slice(64 * half, 64 * half + 64)
                    qsrc = lambda c0, c1, j=j: (qT[:, j, c0:c1], [('qT', j, c0 // 512)])
                    for qi in range(4):
                        q0 = qi * 512
                        par = (r * 4 + qi) % 2
                        for b in range(3):
                            bo = 3 + A.psO.next()
                            if b == 0:
                                tiles = [(0, 0, 512, [])]
                                ksrc = lambda kt, half=half: (kcmpT[half][:, :], [('kcmpT', half)])
                                extra = lambda kt, c0, c1: (C.ident[:], cmaskT[:, c0:c1], [('ident',), ('cmaskT',)])
                                vfn = lambda kt: (Vc[:, :, :].rearrange('p a d -> p (a d)'), [('Vc0',), ('Vc1',)])
                            elif b == 1:
                                tiles = E.band_tiles(qi, None, True)
                                extra = None
                                if qi >= 2:
                                    extra = lambda kt, c0, c1: (eexp[:, kt * 128:(kt + 1) * 128], negselT[:, c0:c1],
                                                                [('eexp',), ('negselT',)])
                                ksrc = lambda kt, half=half: (ksT[half][:, kt * 128:(kt + 1) * 128], [('ksT', half, kt // 4), ('ksz', half)])
                                vfn = lambda kt: (Vs[:, kt, :, :].rearrange('p a d -> p (a d)'),
                                                  [('Vs', kt // 4), ('Vsones',)])
                            else:
                                tiles = E.band_tiles(qi, 4, False)
                                extra = None
                                ksrc = lambda kt, half=half: (kwT[half][:, kt * 128:(kt + 1) * 128], [('kwT', half, kt // 4), ('kwz', half)])
                                vfn = lambda kt: (Vw[:, kt, :, :].rearrange('p a d -> p (a d)'),
                                                  [('Vw', kt // 4), ('Vwones',)])
                            for n_, (kt, ca, cb, masks) in enumerate(tiles):
                                job = dict(
                                    st=lambda qsrc=qsrc, ksrc=ksrc, extra=extra, kt=kt, q0=q0, ca=ca, cb=cb, masks=masks:
                                    E.st_tile(qsrc, ksrc, kt, q0, ca, cb, masks, extra),
                                    ep=lambda bk, vfn=vfn, kt=kt, ca=ca, cb=cb, bo=bo, f=(n_ == 0), l=(n_ == len(tiles) - 1):
                                    E.exp_pv(bk, ca, cb, vfn(kt)[0], vfn(kt)[1], bo, f, l))
                                if b == 0 and n_ == 0:
                                    job['pre'] = lambda r=r, qi=qi, par=par: gate_pre(r, qi, par)
                                if n_ == len(tiles) - 1:
                                    job['fin'] = (lambda b=b, bo=bo, r=r, qi=qi, par=par, rows=rows, j=j:
                                                  combine(b, bo, r, qi, par, rows, j))
                                jobs.append(job)
                run_jobs(jobs)
                P.barrier()


def build_program(layers=(0, 1, 2, 3), do_attn=True, do_mlp=True):
    nc = bass.Bass("TRN2", target_bir_lowering=False)
    C = Ctx()
    C.nc = nc
    C.t = declare_inputs(nc)
    for name, shp in CONST_SHAPES.items():
        C.t[name] = nc.dram_tensor(name, list(shp), F32, kind="ExternalInput").ap()
    C.scrQ = nc.dram_tensor('scrQ', [6, 16, T], BF16, kind="Internal").ap()
    C.scrK = nc.dram_tensor('scrK', [6, 16, T], BF16, kind="Internal").ap()
    with ExitStack() as es:
        P = Prog(nc, es)
        C.P = P
        C.ps = [es.enter_context(nc.psum_tensor('ps%d' % i, [128, 512], F32)) for i in range(8)]
        C.ps_bf = [C.ps[i][:].bitcast(BF16).rearrange('p (c t) -> p c t', c=8) for i in range(8)]
        C.ident = es.enter_context(nc.sbuf_tensor('ident', [128, 128], BF16))
        C.identf = es.enter_context(nc.sbuf_tensor('identf', [128, 128], F32))
        C.eps = es.enter_context(nc.sbuf_tensor('eps', [128, 1], F32))
        C.one = es.enter_context(nc.sbuf_tensor('one', [128, 1], F32))
        P.op('pool', 'memset', dict(ap=C.identf[:], constant=0.0), writes=[('identf',)])
        P.op('pool', 'affine_select',
             dict(out=C.identf[:], in_=C.identf[:], pattern=[[-1, 128]], compare_op=ALU.not_equal,
                  fill=1.0, base=0, channel_multiplier=1),
             reads=[('identf',)], writes=[('identf',)])
        P.op('pool', 'tensor_copy', dict(out=C.ident[:], in_=C.identf[:]), reads=[('identf',)], writes=[('ident',)])
        P.op('pool', 'memset', dict(ap=C.eps[:], constant=EPS), writes=[('eps',)])
        P.op('pool', 'memset', dict(ap=C.one[:], constant=1.0), writes=[('one',)])
        P.barrier()
        src = C.t['x']
        for L in layers:
            if do_attn:
                attn_phase(P, C, L, src)
                src = C.t['y']
            if do_mlp:
                mlp_phase(P, C, L, src)
                src = C.t['y']
        P.barrier()
        P.emit()
    return nc, P


_CONSTS = None


def kernel(**inputs):
    global _CONSTS
    if _CONSTS is None:
        _CONSTS = host_consts()
    nc, P = build_program()
    x = np.ascontiguousarray(np.asarray(inputs['x'], dtype=np.float32))
    shared = {k: np.ascontiguousarray(np.asarray(v, dtype=np.float32)) for k, v in inputs.items() if k != 'x'}
    shared.update(_CONSTS)
    in_maps = []
    for c in range(NCORES):
        m = dict(shared)
        m['x'] = x[c * SEQ_PER_CORE:(c + 1) * SEQ_PER_CORE].reshape(NTOK, D)
        in_maps.append(m)
    res = run_bass_kernel_spmd(nc, in_maps, core_ids=list(range(NCORES)))
    out = np.stack([np.asarray(r['y']).reshape(SEQ_PER_CORE, T, D) for r in res.results], axis=0)
    return out.reshape(NCORES * SEQ_PER_CORE, T, D).astype(np.float32)
```

```python
import numpy as np
from contextlib import ExitStack
import concourse.bass as bass
import concourse.mybir as mybir
from concourse.bass_utils import run_bass_kernel_spmd

F32 = mybir.dt.float32
BF16 = mybir.dt.bfloat16
AF = mybir.ActivationFunctionType
ALU = mybir.AluOpType
AX = mybir.AxisListType

NCORES = 8
D = 1024
T = 2048
SEQ_PER_CORE = 2
NTOK = T * SEQ_PER_CORE
DFF = 4096
H = 16
DH = 64
EPS = 1e-6
NEG = -30000.0


_UNIQ = [0]


def uniq(name):
    _UNIQ[0] += 1
    return '%s_%d' % (name, _UNIQ[0])


class Chan:
    def __init__(self, sem):
        self.sem = sem
        self.count = 0


class Prog:
    def __init__(self, nc, es):
        self.nc = nc
        self.es = es
        self.names = ('pe', 'act', 'dve', 'pool', 'sp')
        self.ins = {e: [] for e in self.names}
        self.res = {}
        self.known = {e: {} for e in self.names}
        self.chans = {}
        self.epoch = 0
        self.KS = 4
        self.RS = 4096
        self.esem = {e: [self.es.enter_context(self.nc.semaphore('s_%s_%d' % (e, k))) for k in range(self.KS)]
                     for e in self.names}

    def chan(self, name):
        c = self.chans.get(name)
        if c is None:
            c = Chan(self.es.enter_context(self.nc.semaphore('c_' + name)))
            self.chans[name] = c
        return c

    def _collect(self, eng, reads, writes):
        need = {}

        def add(src, idx):
            if src[0] == 'd':
                idx = self.chans[src[1]].count
            if need.get(src, -1) < idx:
                need[src] = idx

        for k in reads:
            st = self.res.get(k)
            if st is not None and st[0] is not None:
                add(*st[0])
        for k in writes:
            st = self.res.get(k)
            if st is not None:
                if st[0] is not None:
                    add(*st[0])
                for src, idx in st[1].items():
                    add(src, idx)
        waits = []
        kn = self.known[eng]
        for src, idx in need.items():
            if eng == 'pe' and src == ('e', 'pe'):
                continue
            if kn.get(src, -1) >= idx:
                continue
            kn[src] = idx
            waits.append((src, idx))
            if src[0] == 'e':
                self.ins[src[1]][idx]['sig'] = True
        return waits

    def _update(self, ev, reads, writes):
        for k in reads:
            st = self.res.get(k)
            if st is None:
                st = [None, {}]
                self.res[k] = st
            st[1][ev[0]] = ev[1]
        for k in writes:
            self.res[k] = [ev, {}]

    def op(self, eng, method, kw, reads=(), writes=()):
        waits = self._collect(eng, reads, writes)
        idx = len(self.ins[eng])
        self.ins[eng].append(dict(m=method, kw=kw, waits=waits, sig=False, dma=None, ep=self.epoch))
        self._update((('e', eng), idx), reads, writes)

    def dma(self, q, ch, kw, reads=(), writes=()):
        c = self.chan(ch)
        waits = self._collect(q, reads, writes)
        c.count += 1
        self.ins[q].append(dict(m='dma_start', kw=kw, waits=waits, sig=False, dma=c.sem, ep=self.epoch))
        self._update((('d', ch), c.count), reads, writes)

    def barrier(self):
        last = {}
        for e in self.names:
            for i in range(len(self.ins[e]) - 1, -1, -1):
                it = self.ins[e][i]
                if it['ep'] != self.epoch:
                    break
                if it['dma'] is None and it['m'] != 'wait_only':
                    last[e] = i
                    it['sig'] = True
                    break
        for e in self.names:
            waits = []
            for y, i in last.items():
                if e == 'pe' and y == 'pe':
                    continue
                waits.append((('e', y), i))
            for name, c in self.chans.items():
                if c.count > 0:
                    waits.append((('d', name), c.count))
            self.ins[e].append(dict(m='wait_only', kw=None, waits=waits, sig=False, dma=None, ep=self.epoch))
        self.res = {}
        self.known = {e: {} for e in self.names}
        self.epoch += 1

    def emit(self):
        cnt = {}
        for e in self.names:
            n = 0
            arr = []
            for it in self.ins[e]:
                if it['sig']:
                    b = n // self.RS
                    arr.append((b % self.KS, (b // self.KS) * self.RS + (n % self.RS) + 1))
                    n += 1
                else:
                    arr.append(None)
            cnt[e] = arr
        stats = {e: (len(self.ins[e]), sum(len(it['waits']) for it in self.ins[e])) for e in self.names}
        self.stats = stats

        def mk(e):
            def body(engobj):
                for idx_, it in enumerate(self.ins[e]):
                    for (src, idx) in it['waits']:
                        if src[0] == 'e':
                            y = src[1]
                            k, v = cnt[y][idx]
                            engobj.wait_ge(self.esem[y][k], v)
                        else:
                            engobj.wait_ge(self.chans[src[1]].sem, 16 * idx)
                    if it['m'] == 'wait_only':
                        continue
                    r = getattr(engobj, it['m'])(**it['kw'])
                    if it['dma'] is not None:
                        r.then_inc(it['dma'], 16)
                    elif it['sig']:
                        r.then_inc(self.esem[e][cnt[e][idx_][0]], 1)
            return body

        with self.nc.Block() as block:
            block.tensor(mk('pe'))
            block.scalar(mk('act'))
            block.vector(mk('dve'))
            block.gpsimd(mk('pool'))
            block.sync(mk('sp'))


class Ctx:
    pass


def declare_inputs(nc):
    t = {}

    def inp(name, shape):
        t[name] = nc.dram_tensor(name, list(shape), F32, kind="ExternalInput").ap()

    inp('x', (NTOK, D))
    inp('norm_g', (4, 4, D))
    inp('mlp_w_up', (4, D, DFF))
    inp('mlp_w_down', (4, DFF, D))
    inp('nsa_w_in', (2, D, 2608))
    inp('nsa_cmp_pos', (2, 2, 32, 64))
    inp('nsa_cmp_w1', (2, 2, 2048, 256))
    inp('nsa_cmp_w2', (2, 2, 256, 64))
    inp('nsa_w_out', (2, D, D))
    inp('fox_w_in', (1, D, 3088))
    inp('fox_b_f', (1, 16))
    inp('fox_w_out', (1, D, D))
    inp('swa_w_in', (1, D, 1280))
    inp('swa_b_in', (1, 1280))
    inp('swa_sinks', (1, 16))
    inp('swa_w_out', (1, D, D))
    inp('swa_b_out', (1, D))
    t['y'] = nc.dram_tensor('y', [NTOK, D], F32, kind="ExternalOutput").ap()
    return t


def load_w_cast(P, ch, out_ap, in_ap, writes):
    P.dma('pool', ch, dict(out=out_ap, in_=in_ap, max_dma_last_dim=4096), reads=(), writes=writes)


def rstd_from_ss(P, C, ss, rstd, n, key_ss, key_rstd):
    P.op('act', 'activation', dict(out=rstd, in_=ss, func=AF.Ln, scale=1.0 / D, bias=C.eps[:, 0:1]),
         reads=[key_ss], writes=[key_rstd])
    P.op('act', 'activation', dict(out=rstd, in_=rstd, func=AF.Exp, scale=-0.5),
         reads=[key_rstd], writes=[key_rstd])


def mlp_phase(P, C, L, ysrc):
    nc = C.nc
    tn = C.t
    TT = 256
    NS = TT // 128
    ntiles = NTOK // TT
    with ExitStack() as es:
        sb = lambda name, shape, dt: es.enter_context(nc.sbuf_tensor(uniq(name), shape, dt))
        wup = sb('wup', [128, 8, DFF], BF16)
        wdn = sb('wdn', [128, 32, D], BF16)
        g3 = sb('g3', [128, D], F32)
        g4 = sb('g4', [128, D], F32)
        xts = [sb('xt%d' % i, [128, NS, D], F32) for i in range(2)]
        hb = [sb('hb%d' % i, [128, D], BF16) for i in range(2)]
        hT = [sb('hT%d' % i, [128, 8, TT], BF16) for i in range(2)]
        aT = sb('aT', [128, 32, TT], BF16)
        rl = [sb('rl%d' % i, [128, TT], F32) for i in range(2)]
        junk = sb('junk', [128, D], BF16)
        tmp = [sb('tmp%d' % i, [128, D], F32) for i in range(2)]
        ss = [sb('ss%d' % i, [128, 4], F32) for i in range(2)]
        rs = [sb('rs%d' % i, [128, 4], F32) for i in range(2)]
        ss4 = [sb('ss4%d' % i, [128, 4], F32) for i in range(2)]
        rs4 = [sb('rs4%d' % i, [128, 4], F32) for i in range(2)]

        for hf in range(2):
            for k in range(8):
                load_w_cast(P, 'wA', wup[:, k, hf * 2048:(hf + 1) * 2048],
                            tn['mlp_w_up'][L, k * 128:(k + 1) * 128, hf * 2048:(hf + 1) * 2048],
                            writes=[('wup', k, hf)])
        for c0 in range(0, 32, 4):
            load_w_cast(P, 'wB', wdn[:, c0:c0 + 4, :],
                        tn['mlp_w_down'][L, c0 * 128:(c0 + 4) * 128, :].rearrange('(c p) n -> p c n', p=128),
                        writes=[('wdn', c0 // 4)])
        P.dma('sp', 'g', dict(out=g3[:], in_=tn['norm_g'][L, 2, :].partition_broadcast(128)), writes=[('g3',)])
        P.dma('sp', 'g', dict(out=g4[:], in_=tn['norm_g'][L, 3, :].partition_broadcast(128)), writes=[('g4',)])

        def load_x(ti):
            sl = ti % 2
            P.dma('sp', 'x%d' % sl,
                  dict(out=xts[sl][:], in_=ysrc[ti * TT:(ti + 1) * TT, :].rearrange('(s p) d -> p s d', p=128)),
                  reads=[('y', ti)], writes=[('xt', sl)])

        load_x(0)
        for ti in range(ntiles):
            sl = ti % 2
            xt = xts[sl]
            if ti + 1 < ntiles:
                load_x(ti + 1)
            for s in range(NS):
                P.op('act', 'activation', dict(out=junk[:], in_=xt[:, s, :], func=AF.Square,
                                               accum_out=ss[sl][:, s:s + 1]),
                     reads=[('xt', sl)], writes=[('junk',), ('ss', sl)])
            rstd_from_ss(P, C, ss[sl][:, 0:NS], rs[sl][:, 0:NS], NS, ('ss', sl), ('rs', sl))
            for s in range(NS):
                hs = (ti * NS + s) % 2
                P.op('dve', 'scalar_tensor_tensor',
                     dict(out=hb[hs][:], in0=xt[:, s, :], scalar=rs[sl][:, s:s + 1], in1=g3[:],
                          op0=ALU.mult, op1=ALU.mult),
                     reads=[('xt', sl), ('rs', sl), ('g3',)], writes=[('hb', hs)])
                pb = C.ps_bf[hs]
                for c in range(8):
                    P.op('pe', 'transpose', dict(out=pb[:, c, :], in_=hb[hs][:, c * 128:(c + 1) * 128],
                                                 identity=C.ident[:]),
                         reads=[('hb', hs), ('ident',)], writes=[('ps', hs)])
                P.op('act', 'copy', dict(out=hT[sl][:, :, s * 128:(s + 1) * 128], in_=pb[:, :, :]),
                     reads=[('ps', hs)], writes=[('hT', sl)])
            for fc in range(32):
                bk = 2 + (fc % 2)
                for k in range(8):
                    P.op('pe', 'matmul', dict(out=C.ps[bk][:, 0:TT], lhsT=wup[:, k, fc * 128:(fc + 1) * 128],
                                              rhs=hT[sl][:, k, :], start=(k == 0), stop=(k == 7)),
                         reads=[('wup', k, fc // 16), ('hT', sl)], writes=[('ps', bk)])
                r = rl[fc % 2]
                P.op('act', 'activation', dict(out=r[:], in_=C.ps[bk][:, 0:TT], func=AF.Relu),
                     reads=[('ps', bk)], writes=[('rl', fc % 2)])
                P.op('dve', 'tensor_tensor', dict(out=aT[:, fc, :], in0=r[:], in1=r[:], op=ALU.mult),
                     reads=[('rl', fc % 2)], writes=[('aT', fc)])
            for s in range(NS):
                for dh in range(2):
                    bk = 4 + dh
                    for fc in range(32):
                        P.op('pe', 'matmul',
                             dict(out=C.ps[bk][:, :], lhsT=aT[:, fc, s * 128:(s + 1) * 128],
                                  rhs=wdn[:, fc, dh * 512:(dh + 1) * 512], start=(fc == 0), stop=(fc == 31)),
                             reads=[('aT', fc), ('wdn', fc // 4)], writes=[('ps', bk)])
                    P.op('act', 'activation', dict(out=junk[:, 0:512], in_=C.ps[bk][:, :], func=AF.Square,
                                                   accum_out=ss4[sl][:, 2 * s + dh:2 * s + dh + 1]),
                         reads=[('ps', bk)], writes=[('junk',), ('ss4', sl, s, dh)])
                P.op('dve', 'tensor_tensor', dict(out=ss4[sl][:, 2 * s:2 * s + 1], in0=ss4[sl][:, 2 * s:2 * s + 1],
                                                  in1=ss4[sl][:, 2 * s + 1:2 * s + 2], op=ALU.add),
                     reads=[('ss4', sl, s, 0), ('ss4', sl, s, 1)], writes=[('ss4', sl, s, 0)])
                rstd_from_ss(P, C, ss4[sl][:, 2 * s:2 * s + 1], rs4[sl][:, s:s + 1], 1,
                             ('ss4', sl, s, 0), ('rs4', sl, s))
                tm = tmp[s % 2]
                for dh in range(2):
                    bk = 4 + dh
                    P.op('dve', 'scalar_tensor_tensor',
                         dict(out=tm[:, dh * 512:(dh + 1) * 512], in0=C.ps[bk][:, :], scalar=rs4[sl][:, s:s + 1],
                              in1=g4[:, dh * 512:(dh + 1) * 512], op0=ALU.mult, op1=ALU.mult),
                         reads=[('ps', bk), ('rs4', sl, s), ('g4',)], writes=[('tmp', s % 2, dh)])
                P.op('pool', 'tensor_tensor', dict(out=xt[:, s, :], in0=xt[:, s, :], in1=tm[:], op=ALU.add),
                     reads=[('xt', sl), ('tmp', s % 2, 0), ('tmp', s % 2, 1)], writes=[('xt', sl)])
            P.dma('sp', 'yo%d' % sl,
                  dict(out=C.t['y'][ti * TT:(ti + 1) * TT, :].rearrange('(s p) d -> p s d', p=128), in_=xt[:]),
                  reads=[('xt', sl)], writes=[('y', ti)])
        P.barrier()


CONST_SHAPES = {
    'c_rope': (2, 128, T),
    'c_maskC': (128, 128),
    'c_maskW': (128, 128),
    'c_cmaskT': (128, T),
    'c_cmask': (128, 8, 128),
    'c_eexp': (32, T),
    'c_bonus': (128, 8, 32),
    'c_gsel': (12, 768),
}


def host_consts():
    c = {}
    inv = (np.float32(10000.0) ** (-np.arange(0, DH, 2, dtype=np.float32) / np.float32(DH))).astype(np.float32)
    ang = (np.arange(T, dtype=np.float32)[:, None] * inv[None, :]).astype(np.float32)
    cos = np.cos(ang).astype(np.float32).T
    sin = np.sin(ang).astype(np.float32).T
    c['c_rope'] = np.stack([np.tile(cos, (4, 1)), np.tile(sin, (4, 1))]).astype(np.float32)
    s = np.arange(128)[:, None]
    t = np.arange(128)[None, :]
    c['c_maskC'] = np.where(t >= s, 0.0, NEG).astype(np.float32)
    c['c_maskW'] = np.where(t < s, 0.0, NEG).astype(np.float32)
    cc = np.arange(128)[:, None]
    tt = np.arange(T)[None, :]
    c['c_cmaskT'] = np.where(16 * cc + 31 <= tt, 0.0, NEG).astype(np.float32)
    c['c_cmaskT'][127, :] = -332.0
    tq = 1024 + np.arange(8)[None, :, None] * 128 + np.arange(128)[:, None, None]
    c['c_cmask'] = np.where(16 * np.arange(128)[None, None, :] + 31 <= tq, 0.0, NEG).astype(np.float32)
    c['c_eexp'] = (np.arange(T)[None, :] // 64 == np.arange(32)[:, None]).astype(np.float32)
    tblk = tq // 64
    n = np.arange(32)[None, None, :]
    forced = (n == 0) | (n == tblk) | (n == tblk - 1)
    c['c_bonus'] = np.where(n <= tblk, np.where(forced, 1000.0, 0.0), -1.0).astype(np.float32)
    c['c_gsel'] = (np.arange(768)[None, :] // 64 == np.arange(12)[:, None]).astype(np.float32)
    return c


class Rot:
    def __init__(self, n):
        self.n = n
        self.i = 0

    def next(self):
        v = self.i % self.n
        self.i += 1
        return v


def attn_phase(P, C, L, ysrc):
    nc = C.nc
    tn = C.t
    kind = L % 3
    slot = L // 3
    if kind == 0:
        win, wout_d = tn['nsa_w_in'][slot], tn['nsa_w_out'][slot]
    elif kind == 1:
        win, wout_d = tn['fox_w_in'][slot], tn['fox_w_out'][slot]
    else:
        win, wout_d = tn['swa_w_in'][slot], tn['swa_w_out'][slot]
    ydst = tn['y']

    with ExitStack() as es:
        sb = lambda name, shape, dt: es.enter_context(nc.sbuf_tensor(uniq(name), shape, dt))
        hT = sb('a_hT', [128, 8, T], BF16)
        OT = sb('a_OT', [128, 8, T], BF16)
        maskC = sb('a_maskC', [128, 128], BF16)
        maskW = sb('a_maskW', [128, 128], BF16)
        ones_row = sb('a_ones', [1, 512], BF16)
        wch = [sb('a_wch%d' % i, [128, 8, 128], BF16) for i in range(2)]
        wrot = [sb('a_wrot%d' % i, [128, 8, 128], BF16) for i in range(2)]
        bch = [sb('a_bch%d' % i, [1, 128], BF16) for i in range(2)]
        brot = [sb('a_brot%d' % i, [1, 128], BF16) for i in range(2)]
        rtmp = [sb('a_rtmp%d' % i, [128, 512], F32) for i in range(4)]
        pT = [sb('a_pT%d' % i, [128, 512], BF16) for i in range(4)]
        fden = [sb('a_fden%d' % i, [128, 512], F32) for i in range(2)]
        if kind != 1:
            ropeC = sb('a_ropeC', [128, T], F32)
            ropeS = sb('a_ropeS', [128, T], F32)
            P.dma('sp', 'cst', dict(out=ropeC[:], in_=tn['c_rope'][0]), writes=[('ropeC',)])
            P.dma('sp', 'cst', dict(out=ropeS[:], in_=tn['c_rope'][1]), writes=[('ropeS',)])
        load_w_cast(P, 'cstp', maskC[:], tn['c_maskC'], [('maskC',)])
        load_w_cast(P, 'cstp', maskW[:], tn['c_maskW'], [('maskW',)])
        P.op('pool', 'memset', dict(ap=ones_row[:], constant=1.0), writes=[('ones_row',)])
        A = Ctx()
        A.jobrot = Rot(2)
        A.psA = Rot(2)
        A.rt = Rot(2)
        A.ptr = Rot(4 if kind == 1 else 3)
        A.psS = Rot(4 if kind == 1 else 3)
        A.sbanks = [0, 1, 2, 5] if kind == 1 else [0, 1, 2]
        A.LA = 3 if kind == 1 else 2
        A.psO = Rot(2)
        A.fd = Rot(2)

        def load_chunk(segs, bias_d):
            bi = A.jobrot.next()
            off = 0
            for (c0, n) in segs:
                load_w_cast(P, 'wc%d' % bi, wch[bi][:, :, off:off + n],
                            win[:, c0:c0 + n].rearrange('(k p) n -> p k n', p=128), writes=[('wch', bi)])
                if bias_d is not None:
                    load_w_cast(P, 'wc%d' % bi, bch[bi][0:1, off:off + n], bias_d(c0, n).unsqueeze(0),
                                writes=[('bch', bi)])
                off += n
            return bi, off

        def make_rot(bi, rows, has_bias):
            nb = rows // 64
            v = wch[bi][:, :, 0:rows].rearrange('p k (b two r) -> p k b two r', two=2, r=32)
            w = wrot[bi][:, :, 0:rows].rearrange('p k (b two r) -> p k b two r', two=2, r=32)
            for b in range(nb):
                P.op('pool', 'tensor_scalar', dict(out=w[:, :, b, 0, :], in0=v[:, :, b, 1, :], scalar1=-1.0,
                                                   scalar2=None, op0=ALU.mult),
                     reads=[('wch', bi)], writes=[('wrot', bi, b, 0)])
                P.op('pool', 'tensor_copy', dict(out=w[:, :, b, 1, :], in_=v[:, :, b, 0, :]),
                     reads=[('wch', bi)], writes=[('wrot', bi, b, 1)])
            if has_bias:
                v = bch[bi][0:1, 0:rows].rearrange('p (b two r) -> p b two r', two=2, r=32)
                w = brot[bi][0:1, 0:rows].rearrange('p (b two r) -> p b two r', two=2, r=32)
                P.op('pool', 'tensor_scalar', dict(out=w[:, :, 0, :], in0=v[:, :, 1, :], scalar1=-1.0,
                                                   scalar2=None, op0=ALU.mult),
                     reads=[('bch', bi)], writes=[('brot', bi, 0)])
                P.op('pool', 'tensor_copy', dict(out=w[:, :, 1, :], in_=v[:, :, 0, :]),
                     reads=[('bch', bi)], writes=[('brot', bi, 1)])

        def proj_fm_load(segs, rope, evac, bias_d=None):
            bi, rows = load_chunk(segs, bias_d)
            if rope:
                make_rot(bi, rows, bias_d is not None)
            return bi, rows

        def proj_fm(segs, rope, evac, bias_d=None):
            proj_fm_compute(proj_fm_load(segs, rope, evac, bias_d), segs, rope, evac, bias_d)

        def run_fm(jobs):
            hs = {}
            for i in range(len(jobs) + 1):
                if i < len(jobs):
                    hs[i] = proj_fm_load(*jobs[i])
                if i >= 1:
                    proj_fm_compute(hs.pop(i - 1), *jobs[i - 1])

        def proj_fm_compute(h, segs, rope, evac, bias_d=None):
            bi, rows = h
            rotkeys = [('wrot', bi, b, x) for b in range(rows // 64) for x in range(2)]
            for tq in range(4):
                bkA = 2 + A.psA.next()
                for k in range(8):
                    P.op('pe', 'matmul', dict(out=C.ps[bkA][0:rows, :], lhsT=wch[bi][:, k, 0:rows],
                                              rhs=hT[:, k, tq * 512:(tq + 1) * 512], start=(k == 0),
                                              stop=(k == 7 and bias_d is None)),
                         reads=[('wch', bi), ('hT', tq)], writes=[('ps', bkA)])
                if bias_d is not None:
                    P.op('pe', 'matmul', dict(out=C.ps[bkA][0:rows, :], lhsT=bch[bi][0:1, 0:rows],
                                              rhs=ones_row[0:1, :], start=False, stop=True),
                         reads=[('bch', bi), ('ones_row',)], writes=[('ps', bkA)])
                if not rope:
                    evac(tq, C.ps[bkA][0:rows, :], ('ps', bkA))
                    continue
                bkB = bkA + 2
                for k in range(8):
                    P.op('pe', 'matmul', dict(out=C.ps[bkB][0:rows, :], lhsT=wrot[bi][:, k, 0:rows],
                                              rhs=hT[:, k, tq * 512:(tq + 1) * 512], start=(k == 0),
                                              stop=(k == 7 and bias_d is None)),
                         reads=rotkeys + [('hT', tq)], writes=[('ps', bkB)])
                if bias_d is not None:
                    P.op('pe', 'matmul', dict(out=C.ps[bkB][0:rows, :], lhsT=brot[bi][0:1, 0:rows],
                                              rhs=ones_row[0:1, :], start=False, stop=True),
                         reads=[('brot', bi, 0), ('brot', bi, 1), ('ones_row',)], writes=[('ps', bkB)])
                ri = A.rt.next()
                t1, t2 = rtmp[2 * ri], rtmp[2 * ri + 1]
                P.op('dve', 'tensor_tensor', dict(out=t1[0:rows, :], in0=C.ps[bkA][0:rows, :],
                                                  in1=ropeC[0:rows, tq * 512:(tq + 1) * 512], op=ALU.mult),
                     reads=[('ps', bkA), ('ropeC',)], writes=[('rtmp', 2 * ri)])
                P.op('dve', 'tensor_tensor', dict(out=t2[0:rows, :], in0=C.ps[bkB][0:rows, :],
                                                  in1=ropeS[0:rows, tq * 512:(tq + 1) * 512], op=ALU.mult),
                     reads=[('ps', bkB), ('ropeS',)], writes=[('rtmp', 2 * ri + 1)])
                evac(tq, (t1[0:rows, :], t2[0:rows, :]), [('rtmp', 2 * ri), ('rtmp', 2 * ri + 1)])

        def evac_to(dst_fn, wkey_fn, rope):
            def f(tq, src, skeys):
                if rope:
                    P.op('dve', 'tensor_tensor', dict(out=dst_fn(tq), in0=src[0], in1=src[1], op=ALU.add),
                         reads=skeys, writes=[wkey_fn(tq)])
                else:
                    P.op('act', 'copy', dict(out=dst_fn(tq), in_=src), reads=[skeys], writes=[wkey_fn(tq)])
            return f

        def evac_halves(lo_fn, hi_fn, wkey_fn, rope):
            def f(tq, src, skeys):
                for hf, fn in ((0, lo_fn), (1, hi_fn)):
                    rs_ = slice(64 * hf, 64 * hf + 64)
                    if rope:
                        P.op('dve', 'tensor_tensor', dict(out=fn(tq)[rs_, :], in0=src[0][rs_, :], in1=src[1][rs_, :],
                                                           op=ALU.add), reads=skeys, writes=[wkey_fn(tq, hf)])
                    else:
                        P.op('act', 'copy', dict(out=fn(tq)[rs_, :], in_=src[rs_, :]), reads=[skeys],
                             writes=[wkey_fn(tq, hf)])
            return f

        def proj_tm(c0, ncol, out_fn, vkey, wv, bias_d=None):
            load_w_cast(P, 'wv', wv[:, :, 0:ncol], win[:, c0:c0 + ncol].rearrange('(k p) n -> p k n', p=128),
                        writes=[('wv',)])
            if bias_d is not None:
                load_w_cast(P, 'wv', bch[0][0:1, 0:ncol], bias_d(c0, ncol).unsqueeze(0), writes=[('bch', 0)])
            for k4 in range(4):
                bk = 6 + (k4 % 2)
                for j in range(4):
                    kt = k4 * 4 + j
                    for k in range(8):
                        P.op('pe', 'matmul', dict(out=C.ps[bk][:, j * 64:(j + 1) * 64],
                                                  lhsT=hT[:, k, kt * 128:(kt + 1) * 128], rhs=wv[:, k, 0:ncol],
                                                  start=(k == 0), stop=(k == 7 and bias_d is None),
                                                  skip_group_check=True),
                             reads=[('wv',), ('hT', kt // 4)], writes=[('ps', bk)])
                    if bias_d is not None:
                        P.op('pe', 'matmul', dict(out=C.ps[bk][:, j * 64:(j + 1) * 64], lhsT=ones_row[0:1, 0:128],
                                                  rhs=bch[0][0:1, 0:ncol], start=False, stop=True,
                                                  skip_group_check=True),
                             reads=[('bch', 0), ('ones_row',)], writes=[('ps', bk)])
                P.op('act', 'copy', dict(out=out_fn(k4),
                                         in_=C.ps[bk][:, 0:256].rearrange('p (j d) -> p j d', d=64)),
                     reads=[('ps', bk)], writes=[(vkey, k4)])

        def st_tile(qsrc, ksrc, kt, q0, ca, cb, masks, extra=None):
            bk = A.sbanks[A.psS.next()]
            rhs, rkeys = qsrc(q0 + ca, q0 + cb)
            lhs, lkeys = ksrc(kt)
            last = (not masks) and (extra is None)
            P.op('pe', 'matmul', dict(out=C.ps[bk][:, ca:cb], lhsT=lhs, rhs=rhs, start=True, stop=last,
                                      skip_group_check=True),
                 reads=rkeys + lkeys, writes=[('ps', bk)])
            if extra is not None:
                elhs, erhs, ekeys = extra(kt, q0 + ca, q0 + cb)
                P.op('pe', 'matmul', dict(out=C.ps[bk][:, ca:cb], lhsT=elhs, rhs=erhs, start=False,
                                          stop=(not masks), skip_group_check=True),
                     reads=ekeys, writes=[('ps', bk)])
            for mi, (mt, mkey, bc) in enumerate(masks):
                P.op('pe', 'matmul', dict(out=C.ps[bk][:, bc:bc + 128], lhsT=C.ident[:], rhs=mt, start=False,
                                          stop=(mi == len(masks) - 1), skip_group_check=True),
                     reads=[('ident',), mkey], writes=[('ps', bk)])
            return bk

        def exp_pv(bk, ca, cb, vlhs, vkeys, bo, first, lastpv):
            pi = A.ptr.next()
            P.op('act', 'activation', dict(out=pT[pi][:, ca:cb], in_=C.ps[bk][:, ca:cb], func=AF.Exp, scale=0.125),
                 reads=[('ps', bk)], writes=[('pT', pi)])
            P.op('pe', 'matmul', dict(out=C.ps[bo][:, ca:cb], lhsT=vlhs, rhs=pT[pi][:, ca:cb], start=first,
                                      stop=lastpv, skip_group_check=True),
                 reads=[('pT', pi)] + vkeys, writes=[('ps', bo)])

        def band_tiles(qi, window_tiles, causal_only):
            out = []
            if causal_only:
                js = list(range(-4 * qi, 4))
            else:
                js = [j for j in range(-window_tiles, 4) if 4 * qi + j >= 0]
            js.sort(key=lambda j: (0 if j <= 0 and (causal_only or j + window_tiles >= 3) else 1, j))
            for j in js:
                kt = 4 * qi + j
                ca = max(0, 128 * j)
                masks = []
                if j >= 0:
                    masks.append((maskC[:], ('maskC',), 128 * j))
                if causal_only:
                    cb = 512
                else:
                    cb = min(512, 128 * (j + window_tiles) + 128)
                    jb = j + window_tiles
                    if 0 <= jb <= 3:
                        masks.append((maskW[:], ('maskW',), 128 * jb))
                out.append((kt, ca, cb, masks))
            return out

        for sq in range(SEQ_PER_CORE):
            tb = sq * T
            with ExitStack() as es1:
                sb1 = lambda name, shape, dt: es1.enter_context(nc.sbuf_tensor(uniq(name), shape, dt))
                xts = [sb1('n_xt%d' % i, [128, 2, D], F32) for i in range(3)]
                hb = [sb1('n_hb%d' % i, [128, D], BF16) for i in range(2)]
                junk = sb1('n_junk', [128, D], BF16)
                ss = [sb1('n_ss%d' % i, [128, 2], F32) for i in range(3)]
                rs = [sb1('n_rs%d' % i, [128, 2], F32) for i in range(3)]
                g1 = sb1('a_g1', [128, D], F32)
                P.dma('sp', 'g', dict(out=g1[:], in_=tn['norm_g'][L, 0, :].partition_broadcast(128)), writes=[('g1',)])

                def load_x(ti):
                    sl = ti % 3
                    P.dma('sp', 'x%d' % sl,
                          dict(out=xts[sl][:], in_=ysrc[tb + ti * 256:tb + (ti + 1) * 256, :]
                               .rearrange('(s p) d -> p s d', p=128)),
                          reads=[('y', sq, ti // 2)], writes=[('xt', sl)])

                def stats(ti):
                    sl = ti % 3
                    for s in range(2):
                        P.op('act', 'activation', dict(out=junk[:], in_=xts[sl][:, s, :], func=AF.Square,
                                                       accum_out=ss[sl][:, s:s + 1]),
                             reads=[('xt', sl)], writes=[('junk',), ('ss', sl)])
                    rstd_from_ss(P, C, ss[sl][:, 0:2], rs[sl][:, 0:2], 2, ('ss', sl), ('rs', sl))

                def proc(ti):
                    sl = ti % 3
                    for s in range(2):
                        hs = (ti * 2 + s) % 2
                        P.op('dve', 'scalar_tensor_tensor',
                             dict(out=hb[hs][:], in0=xts[sl][:, s, :], scalar=rs[sl][:, s:s + 1], in1=g1[:],
                                  op0=ALU.mult, op1=ALU.mult),
                             reads=[('xt', sl), ('rs', sl), ('g1',)], writes=[('hb', hs)])
                        pb = C.ps_bf[hs]
                        for c in range(8):
                            P.op('pe', 'transpose', dict(out=pb[:, c, :], in_=hb[hs][:, c * 128:(c + 1) * 128],
                                                         identity=C.ident[:]),
                                 reads=[('hb', hs), ('ident',)], writes=[('ps', hs)])
                        col = ti * 256 + s * 128
                        P.op('act', 'copy', dict(out=hT[:, :, col:col + 128], in_=pb[:, :, :]),
                             reads=[('ps', hs)], writes=[('hT', col // 512)])

                load_x(0)
                load_x(1)
                stats(0)
                for ti in range(8):
                    if ti + 2 < 8:
                        load_x(ti + 2)
                    if ti + 1 < 8:
                        stats(ti + 1)
                    proc(ti)
                P.barrier()

            if kind == 2:
                swa_seq(P, C, A, locals())
            elif kind == 1:
                fox_seq(P, C, A, locals())
            else:
                nsa_seq(P, C, A, locals())

            with ExitStack() as es3:
                sb3 = lambda name, shape, dt: es3.enter_context(nc.sbuf_tensor(uniq(name), shape, dt))
                xt = [sb3('c_xt%d' % i, [128, 2, D], F32) for i in range(2)]
                tmp = [sb3('c_tmp%d' % i, [128, D], F32) for i in range(2)]
                junk = sb3('c_junk', [128, 512], BF16)
                ss2 = sb3('c_ss', [128, 64], F32)
                rs2 = sb3('c_rs', [128, 32], F32)
                bo = sb3('c_bo', [1, D], BF16)
                wo = sb3('a_wo', [128, 8, D], BF16)
                g2 = sb3('a_g2', [128, D], F32)
                for c0 in range(0, 8, 4):
                    load_w_cast(P, 'wA', wo[:, c0:c0 + 4, :],
                                wout_d[c0 * 128:(c0 + 4) * 128, :].rearrange('(c p) n -> p c n', p=128),
                                writes=[('wo', c0 // 4)])
                P.dma('sp', 'g', dict(out=g2[:], in_=tn['norm_g'][L, 1, :].partition_broadcast(128)), writes=[('g2',)])
                if kind == 2:
                    load_w_cast(P, 'wv', bo[0:1, :], tn['swa_b_out'][slot].unsqueeze(0), writes=[('bo',)])
                for ti in range(8):
                    sl = ti % 2
                    P.dma('sp', 'x%d' % sl,
                          dict(out=xt[sl][:], in_=ysrc[tb + ti * 256:tb + (ti + 1) * 256, :]
                               .rearrange('(s p) d -> p s d', p=128)),
                          reads=[('y', sq, ti // 2)], writes=[('cxt', sl)])
                    for s in range(2):
                        kt = ti * 2 + s
                        for dh in range(2):
                            bk = 2 + dh
                            for c in range(8):
                                P.op('pe', 'matmul',
                                     dict(out=C.ps[bk][:, :], lhsT=OT[:, c, kt * 128:(kt + 1) * 128],
                                          rhs=wo[:, c, dh * 512:(dh + 1) * 512], start=(c == 0),
                                          stop=(c == 7 and kind != 2)),
                                     reads=[('OT', c, kt // 4), ('wo', c // 4)], writes=[('ps', bk)])
                            if kind == 2:
                                P.op('pe', 'matmul',
                                     dict(out=C.ps[bk][:, :], lhsT=ones_row[0:1, 0:128],
                                          rhs=bo[0:1, dh * 512:(dh + 1) * 512], start=False, stop=True),
                                     reads=[('bo',), ('ones_row',)], writes=[('ps', bk)])
                            P.op('act', 'activation', dict(out=junk[:, :], in_=C.ps[bk][:, :], func=AF.Square,
                                                           accum_out=ss2[:, 2 * kt + dh:2 * kt + dh + 1]),
                                 reads=[('ps', bk)], writes=[('cjunk',), ('css', kt, dh)])
                        P.op('dve', 'tensor_tensor', dict(out=ss2[:, 2 * kt:2 * kt + 1], in0=ss2[:, 2 * kt:2 * kt + 1],
                                                          in1=ss2[:, 2 * kt + 1:2 * kt + 2], op=ALU.add),
                             reads=[('css', kt, 0), ('css', kt, 1)], writes=[('css', kt, 0)])
                        rstd_from_ss(P, C, ss2[:, 2 * kt:2 * kt + 1], rs2[:, kt:kt + 1], 1, ('css', kt, 0), ('crs', kt))
                        tm = tmp[s % 2]
                        for dh in range(2):
                            bk = 2 + dh
                            P.op('dve', 'scalar_tensor_tensor',
                                 dict(out=tm[:, dh * 512:(dh + 1) * 512], in0=C.ps[bk][:, :], scalar=rs2[:, kt:kt + 1],
                                      in1=g2[:, dh * 512:(dh + 1) * 512], op0=ALU.mult, op1=ALU.mult),
                                 reads=[('ps', bk), ('crs', kt), ('g2',)], writes=[('ctmp', s % 2, dh)])
                        P.op('pool', 'tensor_tensor', dict(out=xt[sl][:, s, :], in0=xt[sl][:, s, :], in1=tm[:],
                                                           op=ALU.add),
                             reads=[('cxt', sl), ('ctmp', s % 2, 0), ('ctmp', s % 2, 1)], writes=[('cxt', sl)])
                    P.dma('sp', 'yo%d' % sl,
                          dict(out=ydst[tb + ti * 256:tb + (ti + 1) * 256, :].rearrange('(s p) d -> p s d', p=128),
                               in_=xt[sl][:]),
                          reads=[('cxt', sl)], writes=[('y', sq, ti // 2)])
                P.barrier()


def run_jobs(jobs, LA=2):
    banks = {}
    n = len(jobs)
    for i in range(n + LA + 1):
        if i < n:
            if jobs[i].get('pre'):
                jobs[i]['pre']()
            banks[i] = jobs[i]['st']()
        k = i - LA
        if 0 <= k < n:
            jobs[k]['ep'](banks.pop(k))
        if 0 <= k - 1 < n and jobs[k - 1].get('fin'):
            jobs[k - 1]['fin']()


class NS:
    def __init__(self, d):
        self.__dict__.update(d)


def finish_simple(P, C, A, E, bo, rows, chunk, q0, addk):
    fi = A.fd.next()
    fd = E.fden[fi]
    fkey = ('fden', fi)
    if addk is not None:
        P.op('act', 'activation', dict(out=fd[64:128, :], in_=C.ps[bo][64:128, :], func=AF.Ln, bias=addk, scale=1.0),
             reads=[('ps', bo), ('esk',)], writes=[fkey])
    else:
        P.op('act', 'activation', dict(out=fd[64:128, :], in_=C.ps[bo][64:128, :], func=AF.Ln),
             reads=[('ps', bo)], writes=[fkey])
    P.op('act', 'activation', dict(out=fd[64:128, :], in_=fd[64:128, :], func=AF.Exp, scale=-1.0), reads=[fkey],
         writes=[fkey])
    P.op('dve', 'tensor_tensor', dict(out=E.OT[rows, chunk, q0:q0 + 512], in0=C.ps[bo][0:64, :], in1=fd[64:128, :],
                                      op=ALU.mult),
         reads=[('ps', bo), fkey], writes=[('OT', chunk, q0 // 512)])


def swa_seq(P, C, A, Ed):
    E = NS(Ed)
    nc, tn = C.nc, C.t
    b_in = tn['swa_b_in'][E.slot]
    bias_fn = lambda c0, n: b_in[c0:c0 + n]
    for g in range(2):
        with ExitStack() as es2:
            sb = lambda name, shape, dt: es2.enter_context(nc.sbuf_tensor(uniq(name), shape, dt))
            qT = sb('s_qT', [128, 4, T], BF16)
            kdl = sb('s_kdl', [128, T], BF16)
            kdh = sb('s_kdh', [128, T], BF16)
            Vt = sb('s_V', [128, 16, 2, 64], BF16)
            wv = sb('s_wv', [128, 8, 64], BF16)
            esk = sb('s_esk', [128, 16], F32)
            P.op('pool', 'memset', dict(ap=Vt[:, :, 1, :], constant=1.0), writes=[('Vones',)])
            P.op('pool', 'memset', dict(ap=kdl[64:128, :], constant=0.0), writes=[('kdz', 0)])
            P.op('pool', 'memset', dict(ap=kdh[0:64, :], constant=0.0), writes=[('kdz', 1)])
            P.dma('sp', 'g', dict(out=esk[:], in_=tn['swa_sinks'][E.slot].partition_broadcast(128)), writes=[('esk',)])
            P.op('act', 'activation', dict(out=esk[:], in_=esk[:], func=AF.Exp), reads=[('esk',)], writes=[('esk',)])
            pj = []
            for j in range(4):
                pj.append(([(g * 512 + j * 128, 128)], True,
                          E.evac_to(lambda tq, j=j: qT[:, j, tq * 512:(tq + 1) * 512],
                                    lambda tq, j=j: ('qT', j, tq), True), bias_fn))
            pj.append(([(1024 + g * 64, 64)] * 2, True,
                      E.evac_halves(lambda tq: kdl[:, tq * 512:(tq + 1) * 512], lambda tq: kdh[:, tq * 512:(tq + 1) * 512],
                                    lambda tq, hf: ('kd', hf, tq), True), bias_fn))
            E.run_fm(pj)
            E.proj_tm(1152 + g * 64, 64, lambda k4: Vt[:, k4 * 4:(k4 + 1) * 4, 0, :], 'Vt', wv, bias_fn)
            jobs = []
            for r in range(8):
                h = 8 * g + r
                j, half = r // 2, r % 2
                rows = slice(64 * half, 64 * half + 64)
                qsrc = lambda c0, c1, j=j: (qT[:, j, c0:c1], [('qT', j, c0 // 512)])
                ksrc = lambda kt, half=half: ((kdh if half else kdl)[:, kt * 128:(kt + 1) * 128], [('kd', half, kt // 4), ('kdz', half)])
                for qi in range(4):
                    tiles = E.band_tiles(qi, 1, False)
                    bo = 3 + A.psO.next()
                    for n_, (kt, ca, cb, masks) in enumerate(tiles):
                        job = dict(
                            st=lambda qsrc=qsrc, ksrc=ksrc, kt=kt, qi=qi, ca=ca, cb=cb, masks=masks:
                            E.st_tile(qsrc, ksrc, kt, qi * 512, ca, cb, masks),
                            ep=lambda bk, kt=kt, ca=ca, cb=cb, bo=bo, f=(n_ == 0), l=(n_ == len(tiles) - 1):
                            E.exp_pv(bk, ca, cb, Vt[:, kt, :, :].rearrange('p a d -> p (a d)'),
                                     [('Vt', kt // 4), ('Vones',)], bo, f, l))
                        if n_ == len(tiles) - 1:
                            job['fin'] = (lambda bo=bo, rows=rows, j=j, qi=qi, h=h:
                                          finish_simple(P, C, A, E, bo, rows, 4 * g + j, qi * 512, esk[64:128, h:h + 1]))
                        jobs.append(job)
            run_jobs(jobs, A.LA)
            P.barrier()


def fox_seq(P, C, A, Ed):
    E = NS(Ed)
    nc, tn = C.nc, C.t
    b_f = tn['fox_b_f'][E.slot]
    scrQ, scrK = C.scrQ, C.scrK
    with ExitStack() as es2:
        sb = lambda name, shape, dt: es2.enter_context(nc.sbuf_tensor(uniq(name), shape, dt))
        spt = sb('f_spt', [16, T], F32)
        cs = sb('f_cs', [16, T], F32)
        ones16 = sb('f_ones', [16, T], F32)
        res_ = sb('f_res', [16, T], F32)
        parts = [sb('f_a%d' % i, [16, T], BF16) for i in range(3)]
        nparts = [sb('f_n%d' % i, [16, T], BF16) for i in range(3)]
        P.op('pool', 'memset', dict(ap=ones16[:], constant=1.0), writes=[('ones16',)])
        onesb = sb('f_onesb', [16, T], BF16)
        P.op('pool', 'memset', dict(ap=onesb[:], constant=1.0), writes=[('onesb',)])

        def evac_fl(tq, src, skey):
            sl = spt[0:16, tq * 512:(tq + 1) * 512]
            P.op('act', 'activation', dict(out=sl, in_=src, func=AF.Exp, scale=-1.0), reads=[skey],
                 writes=[('spt', tq)])
            P.op('act', 'activation', dict(out=sl, in_=sl, func=AF.Ln, bias=C.one[0:16, 0:1], scale=1.0),
                 reads=[('spt', tq)], writes=[('spt', tq)])
        E.proj_fm([(3072, 16)], False, evac_fl, lambda c0, n: b_f[0:16])
        P.op('dve', 'tensor_tensor_scan', dict(out=cs[:], data0=ones16[:], data1=spt[:], initial=0.0,
                                               op0=ALU.mult, op1=ALU.add),
             reads=[('ones16',)] + [('spt', tq) for tq in range(4)], writes=[('cs',)])
        P.op('dve', 'tensor_scalar', dict(out=cs[:], in0=cs[:], scalar1=8.0, scalar2=None, op0=ALU.mult),
             reads=[('cs',)], writes=[('cs',)])
        cur = cs
        ckey = ('cs',)
        for i in range(3):
            P.op('dve', 'tensor_copy', dict(out=parts[i][:], in_=cur[:]), reads=[ckey], writes=[('part', i)])
            P.op('dve', 'tensor_scalar', dict(out=nparts[i][:], in0=parts[i][:], scalar1=-1.0, scalar2=None,
                                              op0=ALU.mult), reads=[('part', i)], writes=[('npart', i)])
            if i < 2:
                P.op('dve', 'tensor_tensor', dict(out=res_[:], in0=cur[:], in1=parts[i][:], op=ALU.subtract),
                     reads=[ckey, ('part', i)], writes=[('res',)])
                cur, ckey = res_, ('res',)
            P.dma('sp', 'scr', dict(out=scrQ[i], in_=nparts[i][:]), reads=[('npart', i)], writes=[('scrQ', i)])
            P.dma('sp', 'scr', dict(out=scrK[3 + i], in_=parts[i][:]), reads=[('part', i)], writes=[('scrK', 3 + i)])
            P.dma('sp', 'scr', dict(out=scrQ[3 + i], in_=onesb[:]), reads=[('onesb',)], writes=[('scrQ', 3 + i)])
            P.dma('sp', 'scr', dict(out=scrK[i], in_=onesb[:]), reads=[('onesb',)], writes=[('scrK', i)])
        P.barrier()
    for gp in range(4):
        with ExitStack() as es2:
            sb = lambda name, shape, dt: es2.enter_context(nc.sbuf_tensor(uniq(name), shape, dt))
            qT = sb('f_qT', [128, 2, T], BF16)
            kTl = sb('f_kTl', [128, 2, T], BF16)
            kTh = sb('f_kTh', [128, 2, T], BF16)
            Vt = sb('f_V', [128, 16, 4, 2, 64], BF16)
            wv = sb('f_wv', [128, 8, 64], BF16)
            cq = sb('f_cq', [128, 4, T], BF16)
            ck = sb('f_ck', [128, 4, T], BF16)
            P.op('pool', 'memset', dict(ap=Vt[:, :, :, 1, :], constant=1.0), writes=[('Vones',)])
            P.op('pool', 'memset', dict(ap=cq[:], constant=0.0), writes=[('cq',)])
            P.op('pool', 'memset', dict(ap=ck[:], constant=0.0), writes=[('ck',)])
            P.op('pool', 'memset', dict(ap=kTl[64:128, :, :], constant=0.0), writes=[('kTz', 0)])
            P.op('pool', 'memset', dict(ap=kTh[0:64, :, :], constant=0.0), writes=[('kTz', 1)])
            P.dma('sp', 'scr2', dict(out=cq[0:6, :, :], in_=scrQ[:, 4 * gp:4 * gp + 4, :]),
                  reads=[('scrQ', i) for i in range(6)] + [('cq',)], writes=[('cq',)])
            P.dma('sp', 'scr2', dict(out=ck[0:6, :, :], in_=scrK[:, 4 * gp:4 * gp + 4, :]),
                  reads=[('scrK', i) for i in range(6)] + [('ck',)], writes=[('ck',)])
            pj = []
            for j in range(2):
                pj.append(([(gp * 256 + j * 128, 128)], False,
                          E.evac_to(lambda tq, j=j: qT[:, j, tq * 512:(tq + 1) * 512],
                                    lambda tq, j=j: ('qT', j, tq), False)))
                pj.append(([(1024 + gp * 256 + j * 128, 128)], False,
                          E.evac_halves(lambda tq, j=j: kTl[:, j, tq * 512:(tq + 1) * 512],
                                        lambda tq, j=j: kTh[:, j, tq * 512:(tq + 1) * 512],
                                        lambda tq, hf, j=j: ('kT', j, hf, tq), False)))
            E.run_fm(pj)
            for r in range(4):
                E.proj_tm(2048 + (4 * gp + r) * 64, 64, lambda k4, r=r: Vt[:, k4 * 4:(k4 + 1) * 4, r, 0, :],
                          ('Vt', r), wv)
            jobs = []
            for r in range(4):
                j, half = r // 2, r % 2
                rows = slice(64 * half, 64 * half + 64)
                qsrc = lambda c0, c1, j=j: (qT[:, j, c0:c1], [('qT', j, c0 // 512)])
                ksrc = lambda kt, half=half, j=j: ((kTh if half else kTl)[:, j, kt * 128:(kt + 1) * 128], [('kT', j, half, kt // 4), ('kTz', half)])
                extra = lambda kt, c0, c1, r=r: (ck[:, r, kt * 128:(kt + 1) * 128], cq[:, r, c0:c1], [('cq',), ('ck',)])
                for qi in range(4):
                    tiles = E.band_tiles(qi, None, True)
                    bo = 3 + A.psO.next()
                    for n_, (kt, ca, cb, masks) in enumerate(tiles):
                        job = dict(
                            st=lambda qsrc=qsrc, ksrc=ksrc, extra=extra, kt=kt, qi=qi, ca=ca, cb=cb, masks=masks:
                            E.st_tile(qsrc, ksrc, kt, qi * 512, ca, cb, masks, extra),
                            ep=lambda bk, kt=kt, ca=ca, cb=cb, bo=bo, r=r, f=(n_ == 0), l=(n_ == len(tiles) - 1):
                            E.exp_pv(bk, ca, cb, Vt[:, kt, r, :, :].rearrange('p a d -> p (a d)'),
                                     [(('Vt', r), kt // 4), ('Vones',)], bo, f, l))
                        if n_ == len(tiles) - 1:
                            job['fin'] = (lambda bo=bo, rows=rows, j=j, qi=qi:
                                          finish_simple(P, C, A, E, bo, rows, 2 * gp + j, qi * 512, None))
                        jobs.append(job)
            run_jobs(jobs, A.LA)
            P.barrier()


def nsa_seq(P, C, A, Ed):
    E = NS(Ed)
    nc, tn = C.nc, C.t
    slot = E.slot
    with ExitStack() as esL:
        sbL = lambda name, shape, dt: esL.enter_context(nc.sbuf_tensor(uniq(name), shape, dt))
        w1 = [sbL('n_w1%d' % i, [128, 16, 256], BF16) for i in range(2)]
        w2k = sbL('n_w2k', [128, 2, 128], BF16)
        w2v = sbL('n_w2v', [128, 2, 64], BF16)
        posf = sbL('n_posf', [128, 2, 16], F32)
        posS = sbL('n_posS', [128, 2, 16, 2], BF16)
        biasT = sbL('n_biasT', [128, 2, 2], F32)
        cmaskT = sbL('n_cmaskT', [128, T], BF16)
        cmask = sbL('n_cmask', [128, 8, 128], BF16)
        eexp = sbL('n_eexp', [128, T], BF16)
        bonus = sbL('n_bonus', [128, 8, 32], F32)
        gsel = sbL('n_gsel', [12, 768], BF16)
        load_w_cast(P, 'cstp', cmaskT[:], tn['c_cmaskT'], [('cmaskT',)])
        load_w_cast(P, 'cstp', cmask[:], tn['c_cmask'], [('cmask',)])
        P.op('pool', 'memset', dict(ap=eexp[:], constant=0.0), writes=[('eexp',)])
        load_w_cast(P, 'cstp', eexp[0:32, :], tn['c_eexp'], [('eexp',)])
        load_w_cast(P, 'cstp', gsel[:], tn['c_gsel'], [('gsel',)])
        P.dma('sp', 'cst', dict(out=bonus[:], in_=tn['c_bonus']), writes=[('bonus',)])
        for kv in range(2):
            for c0 in range(0, 16, 8):
                load_w_cast(P, 'wA', w1[kv][:, c0:c0 + 8, :],
                            tn['nsa_cmp_w1'][slot, kv, c0 * 128:(c0 + 8) * 128, :]
                            .rearrange('(c p) n -> p c n', p=128), [('w1', kv)])
            pr = tn['nsa_cmp_pos'][slot, kv].rearrange('(c two) d -> two d c', two=2)
            for two in range(2):
                P.dma('sp', 'cst', dict(out=posf[64 * two:64 * two + 64, kv, :], in_=pr[two],
                                        allow_slow_non_contiguous=True), writes=[('posf', kv, two)])
            for x in range(2):
                P.op('pool', 'tensor_copy', dict(out=posS[:, kv, :, x], in_=posf[:, kv, :]),
                     reads=[('posf', kv, 0), ('posf', kv, 1)], writes=[('posS', kv, x)])
        w2d = tn['nsa_cmp_w2'][slot]
        for dup in range(2):
            load_w_cast(P, 'wA', w2k[:, :, 64 * dup:64 * dup + 64], w2d[0].rearrange('(c p) n -> p c n', p=128),
                        [('w2k', dup)])
        load_w_cast(P, 'wA', w2v[:, :, :], w2d[1].rearrange('(c p) n -> p c n', p=128), [('w2v',)])
        for kv in range(2):
            for hc in range(2):
                for c2 in range(16):
                    P.op('pe', 'matmul', dict(out=C.ps[7][:, 0:2], lhsT=w1[kv][:, c2, hc * 128:(hc + 1) * 128],
                                              rhs=posS[:, kv, c2, :], start=(c2 == 0), stop=(c2 == 15)),
                         reads=[('w1', kv), ('posS', kv, 0), ('posS', kv, 1)], writes=[('ps', 7)])
                P.op('act', 'copy', dict(out=biasT[:, kv, hc:hc + 1], in_=C.ps[7][:, 0:1]),
                     reads=[('ps', 7)], writes=[('biasT', kv, hc)])
        P.barrier()

        for g in range(4):
            with ExitStack() as es2:
                sb = lambda name, shape, dt: es2.enter_context(nc.sbuf_tensor(uniq(name), shape, dt))
                qT = sb('n_qT', [128, 2, T], BF16)
                ksT = [sb('n_ksT%d' % i, [128, T], BF16) for i in range(2)]
                kwT = [sb('n_kwT%d' % i, [128, T], BF16) for i in range(2)]
                cS = [sb('n_cS%d' % i, [128, T], BF16) for i in range(2)]
                Vs = sb('n_Vs', [128, 16, 2, 64], BF16)
                Vw = sb('n_Vw', [128, 16, 2, 64], BF16)
                wv = sb('n_wv', [128, 8, 64], BF16)
                gTh = sb('n_gTh', [12, T], BF16)
                kcmpT = [sb('n_kcmpT%d' % i, [128, 128], BF16) for i in range(2)]
                Vc = sb('n_Vc', [128, 2, 64], BF16)
                hidT = [sb('n_hid%d' % i, [128, 2, 128], BF16) for i in range(2)]
                negselT = sb('n_negselT', [128, T], BF16)
                Pn = sb('n_Pn', [128, 4, 128], F32)
                Ps8 = sb('n_Ps8', [128, 8, 128], F32)
                imp8 = sb('n_imp8', [128, 8, 32], F32)
                sc2 = sb('n_sc2', [128, 32], F32)
                m1 = sb('n_m1', [128, 8], F32)
                m2 = sb('n_m2', [128, 8, 8], F32)
                den4 = sb('n_den4', [128, 8, 4], F32)
                negsel = sb('n_negsel', [128, 8, 32], BF16)
                acc = [sb('n_acc%d' % i, [64, 512], F32) for i in range(2)]
                ctmp = [sb('n_ctmp%d' % i, [64, 512], F32) for i in range(2)]
                P.op('pool', 'memset', dict(ap=Vs[:, :, 1, :], constant=1.0), writes=[('Vsones',)])
                P.op('pool', 'memset', dict(ap=Vw[:, :, 1, :], constant=1.0), writes=[('Vwones',)])
                P.op('pool', 'memset', dict(ap=Vc[:, 1, :], constant=1.0), writes=[('Vc1',)])
                P.op('pool', 'memset', dict(ap=Vc[:, 0, :], constant=0.0), writes=[('Vc0',)])
                for i in range(2):
                    P.op('pool', 'memset', dict(ap=kcmpT[i][:], constant=0.0), writes=[('kcmpT', i)])
                    P.op('pool', 'memset', dict(ap=ksT[i][64 * (1 - i):64 * (1 - i) + 64, :], constant=0.0), writes=[('ksz', i)])
                    P.op('pool', 'memset', dict(ap=kwT[i][64 * (1 - i):64 * (1 - i) + 64, :], constant=0.0), writes=[('kwz', i)])
                P.op('pool', 'memset', dict(ap=negselT[:], constant=0.0), writes=[('negselT',)])
                for i in range(2):
                    P.op('pool', 'memset', dict(ap=hidT[i][:], constant=0.0), writes=[('hidT', i)])

                pj = []
                for j in range(2):
                    pj.append(([(g * 256 + j * 128, 128)], True,
                              E.evac_to(lambda tq, j=j: qT[:, j, tq * 512:(tq + 1) * 512],
                                        lambda tq, j=j: ('qT', j, tq), True)))
                pj.append(([(1536 + g * 64, 64)] * 2, True,
                          E.evac_halves(lambda tq: ksT[0][:, tq * 512:(tq + 1) * 512], lambda tq: ksT[1][:, tq * 512:(tq + 1) * 512],
                                        lambda tq, hf: ('ksT', hf, tq), True)))
                pj.append(([(2048 + g * 64, 64)] * 2, True,
                          E.evac_halves(lambda tq: kwT[0][:, tq * 512:(tq + 1) * 512], lambda tq: kwT[1][:, tq * 512:(tq + 1) * 512],
                                        lambda tq, hf: ('kwT', hf, tq), True)))

                def evac_shift(i, rope):
                    def f(tq, src, skeys):
                        lo, hi = tq * 512, (tq + 1) * 512
                        if rope:
                            a, b = src
                            P.op('dve', 'tensor_tensor', dict(out=cS[i][0:64, lo:hi], in0=a[0:64, :], in1=b[0:64, :],
                                                               op=ALU.add), reads=skeys, writes=[('cS', i, tq, 0)])
                            if tq == 0:
                                P.op('dve', 'tensor_tensor', dict(out=cS[i][64:128, 0:511], in0=a[64:128, 1:512],
                                                                   in1=b[64:128, 1:512], op=ALU.add),
                                     reads=skeys, writes=[('cS', i, tq, 1)])
                            else:
                                P.op('dve', 'tensor_tensor', dict(out=cS[i][64:128, lo - 1:hi - 1], in0=a[64:128, :],
                                                                   in1=b[64:128, :], op=ALU.add),
                                     reads=skeys, writes=[('cS', i, tq, 1)])
                        else:
                            P.op('act', 'copy', dict(out=cS[i][0:64, lo:hi], in_=src[0:64, :]), reads=[skeys],
                                 writes=[('cS', i, tq, 0)])
                            if tq == 0:
                                P.op('act', 'copy', dict(out=cS[i][64:128, 0:511], in_=src[64:128, 1:512]),
                                     reads=[skeys], writes=[('cS', i, tq, 1)])
                            else:
                                P.op('act', 'copy', dict(out=cS[i][64:128, lo - 1:hi - 1], in_=src[64:128, :]),
                                     reads=[skeys], writes=[('cS', i, tq, 1)])
                    return f
                pj.append(([(1024 + g * 64, 64)] * 2, True, evac_shift(0, True)))
                pj.append(([(1280 + g * 64, 64)] * 2, False, evac_shift(1, False)))

                def evac_gate(tq, src, skey):
                    ri = A.rt.next()
                    gf = E.rtmp[2 * ri]
                    gk = ('rtmp', 2 * ri)
                    P.op('act', 'activation', dict(out=gf[0:12, :], in_=src, func=AF.Sigmoid), reads=[skey], writes=[gk])
                    P.op('dve', 'tensor_copy', dict(out=gTh[0:12, tq * 512:(tq + 1) * 512], in_=gf[0:12, :]),
                         reads=[gk], writes=[('gTh', tq)])
                pj.append(([(2560 + 12 * g, 12)], False, evac_gate))
                E.run_fm(pj)
                E.proj_tm(1792 + g * 64, 64, lambda k4: Vs[:, k4 * 4:(k4 + 1) * 4, 0, :], 'Vs', wv)
                E.proj_tm(2304 + g * 64, 64, lambda k4: Vw[:, k4 * 4:(k4 + 1) * 4, 0, :], 'Vw', wv)

                cSkeys = lambda i: [('cS', i, tq, x) for tq in range(4) for x in range(2)]
                for kv in range(2):
                    for hc in range(2):
                        for c2 in range(16):
                            P.op('pe', 'matmul',
                                 dict(out=C.ps[7][:, hc * 128:hc * 128 + 127], lhsT=w1[kv][:, c2, hc * 128:(hc + 1) * 128],
                                      rhs=cS[kv][:, 2 * c2:2 * c2 + 2017:16], start=(c2 == 0), stop=(c2 == 15),
                                      skip_group_check=True),
                                 reads=[('w1', kv)] + cSkeys(kv), writes=[('ps', 7)])
                        P.op('act', 'activation',
                             dict(out=hidT[kv][:, hc, 0:127], in_=C.ps[7][:, hc * 128:hc * 128 + 127], func=AF.Silu,
                                  bias=biasT[:, kv, hc:hc + 1], scale=1.0),
                             reads=[('ps', 7), ('biasT', kv, hc)], writes=[('hidT', kv)])
                for hc in range(2):
                    P.op('pe', 'matmul', dict(out=C.ps[6][:, 0:127], lhsT=w2k[:, hc, :], rhs=hidT[0][:, hc, 0:127],
                                              start=(hc == 0), stop=(hc == 1)),
                         reads=[('w2k', 0), ('w2k', 1), ('hidT', 0)], writes=[('ps', 6)])
                for i in range(2):
                    P.op('act', 'copy', dict(out=kcmpT[i][64 * i:64 * i + 64, 0:127], in_=C.ps[6][64 * i:64 * i + 64, 0:127]),
                         reads=[('ps', 6), ('kcmpT', i)], writes=[('kcmpT', i)])
                for hc in range(2):
                    P.op('pe', 'matmul', dict(out=C.ps[6][0:127, 256:320], lhsT=hidT[1][:, hc, 0:127], rhs=w2v[:, hc, :],
                                              start=(hc == 0), stop=(hc == 1), skip_group_check=True),
                         reads=[('w2v',), ('hidT', 1)], writes=[('ps', 6)])
                P.op('act', 'copy', dict(out=Vc[0:127, 0, :], in_=C.ps[6][0:127, 256:320]), reads=[('ps', 6), ('Vc0',)],
                     writes=[('Vc0',)])

                for tt in range(8):
                    t0 = 1024 + tt * 128
                    for r in range(4):
                        j, half = r // 2, r % 2
                        rows = slice(64 * half, 64 * half + 64)
                        P.op('pe', 'matmul', dict(out=C.ps[5][:, r * 128:(r + 1) * 128], lhsT=qT[:, j, t0:t0 + 128],
                                                  rhs=kcmpT[half][:, :], start=True, stop=False, skip_group_check=True),
                             reads=[('qT', j, t0 // 512), ('kcmpT', half)], writes=[('ps', 5)])
                        P.op('pe', 'matmul', dict(out=C.ps[5][:, r * 128:(r + 1) * 128], lhsT=C.ident[:],
                                                  rhs=cmask[:, tt, :], start=False, stop=True, skip_group_check=True),
                             reads=[('ident',), ('cmask',)], writes=[('ps', 5)])
                    for r in range(4):
                        P.op('act', 'activation', dict(out=Pn[:, r, :], in_=C.ps[5][:, r * 128:(r + 1) * 128],
                                                       func=AF.Exp, scale=0.125, accum_out=den4[:, tt, r:r + 1]),
                             reads=[('ps', 5)], writes=[('Pn', r), ('den4', tt, r)])
                    dk = [('den4', tt, r) for r in range(4)]
                    P.op('dve', 'tensor_scalar', dict(out=den4[:, tt, :], in0=den4[:, tt, :], scalar1=1e-30, scalar2=None,
                                                      op0=ALU.max), reads=dk, writes=[('den4', tt)])
                    P.op('dve', 'reciprocal', dict(out=den4[:, tt, :], in_=den4[:, tt, :]), reads=[('den4', tt)],
                         writes=[('den4', tt)])
                    P.op('dve', 'tensor_scalar', dict(out=Ps8[:, tt, :], in0=Pn[:, 0, :], scalar1=den4[:, tt, 0:1],
                                                      scalar2=None, op0=ALU.mult),
                         reads=[('Pn', 0), ('den4', tt)], writes=[('Ps8', tt)])
                    for r in range(1, 4):
                        P.op('dve', 'scalar_tensor_tensor',
                             dict(out=Ps8[:, tt, :], in0=Pn[:, r, :], scalar=den4[:, tt, r:r + 1], in1=Ps8[:, tt, :],
                                  op0=ALU.mult, op1=ALU.add),
                             reads=[('Pn', r), ('den4', tt), ('Ps8', tt)], writes=[('Ps8', tt)])
                pk = [('Ps8', tt) for tt in range(8)]
                Pv = Ps8[:].rearrange('p t (n i) -> p t n i', i=4)
                P.op('dve', 'tensor_tensor', dict(out=imp8[:], in0=Pv[:, :, :, 0], in1=Pv[:, :, :, 1], op=ALU.add),
                     reads=pk, writes=[('imp8',)])
                P.op('dve', 'tensor_tensor', dict(out=imp8[:], in0=imp8[:], in1=Pv[:, :, :, 2], op=ALU.add),
                     reads=pk + [('imp8',)], writes=[('imp8',)])
                P.op('dve', 'scalar_tensor_tensor', dict(out=imp8[:], in0=Pv[:, :, :, 3], scalar=0.5, in1=imp8[:],
                                                         op0=ALU.mult, op1=ALU.add),
                     reads=pk + [('imp8',)], writes=[('imp8',)])
                P.op('dve', 'scalar_tensor_tensor', dict(out=imp8[:, :, 1:32], in0=Pv[:, :, 0:31, 3], scalar=0.5,
                                                         in1=imp8[:, :, 1:32], op0=ALU.mult, op1=ALU.add),
                     reads=pk + [('imp8',)], writes=[('imp8',)])
                P.op('dve', 'tensor_tensor', dict(out=imp8[:], in0=imp8[:], in1=bonus[:], op=ALU.add),
                     reads=[('imp8',), ('bonus',)], writes=[('imp8',)])
                for tt in range(8):
                    P.op('dve', 'max', dict(out=m1[:], in_=imp8[:, tt, :]), reads=[('imp8',)], writes=[('m1',)])
                    P.op('dve', 'match_replace', dict(out=sc2[:], in_to_replace=m1[:], in_values=imp8[:, tt, :],
                                                      imm_value=-1e30), reads=[('m1',), ('imp8',)], writes=[('sc2',)])
                    P.op('dve', 'max', dict(out=m2[:, tt, :], in_=sc2[:]), reads=[('sc2',)], writes=[('m2', tt)])
                    P.op('dve', 'tensor_scalar', dict(out=negsel[:, tt, :], in0=imp8[:, tt, :], scalar1=m2[:, tt, 7:8],
                                                      scalar2=NEG, op0=ALU.is_lt, op1=ALU.mult),
                         reads=[('imp8',), ('m2', tt)], writes=[('negsel', tt)])
                    P.op('pe', 'transpose', dict(out=C.ps_bf[6][0:32, tt, :], in_=negsel[:, tt, :], identity=C.ident[:]),
                         reads=[('negsel', tt), ('ident',)], writes=[('ps', 6)])
                P.op('act', 'copy', dict(out=negselT[0:32, 1024:2048],
                                         in_=C.ps_bf[6][0:32, :, :].rearrange('p a b -> p (a b)')),
                     reads=[('ps', 6)], writes=[('negselT',)])

                jobs = []
                first_gate = [True]

                def gate_pre(r, qi, par):
                    q0 = qi * 512
                    gk = [('gTh', qi), ('gsel',)]
                    b01 = 5 if par == 0 else 7
                    P.op('pe', 'matmul', dict(out=C.ps[b01][:, :], lhsT=gsel[0:12, (3 * r) * 64:(3 * r + 2) * 64],
                                              rhs=gTh[0:12, q0:q0 + 512], start=True, stop=True),
                         reads=gk, writes=[('ps', b01)])
                    wk = [('ps6h', par)]
                    if first_gate[0]:
                        wk = [('ps', 6), ('ps6h', 0), ('ps6h', 1)]
                        first_gate[0] = False
                    P.op('pe', 'matmul', dict(out=C.ps[6][64 * par:64 * par + 64, :],
                                              lhsT=gsel[0:12, (3 * r + 2) * 64:(3 * r + 3) * 64],
                                              rhs=gTh[0:12, q0:q0 + 512], start=True, stop=True,
                                              skip_group_check=True),
                         reads=gk, writes=wk)

                def combine(b, bo, r, qi, par, rows, j):
                    q0 = qi * 512
                    b01 = 5 if par == 0 else 7
                    gap = [C.ps[b01][0:64, :], C.ps[b01][64:128, :], C.ps[6][64 * par:64 * par + 64, :]][b]
                    gkey = [('ps', b01), ('ps', b01), ('ps6h', par)][b]
                    ai = par
                    fi = A.fd.next()
                    fd = E.fden[fi]
                    fk = ('fden', fi)
                    P.op('act', 'activation', dict(out=fd[64:128, :], in_=C.ps[bo][64:128, :], func=AF.Ln),
                         reads=[('ps', bo)], writes=[fk])
                    P.op('act', 'activation', dict(out=fd[64:128, :], in_=fd[64:128, :], func=AF.Exp, scale=-1.0),
                         reads=[fk], writes=[fk])
                    P.op('dve', 'tensor_tensor', dict(out=fd[0:64, :], in0=gap, in1=fd[64:128, :], op=ALU.mult),
                         reads=[gkey, fk], writes=[fk])
                    if b == 0:
                        P.op('dve', 'tensor_tensor', dict(out=acc[ai][:], in0=C.ps[bo][0:64, :], in1=fd[0:64, :],
                                                          op=ALU.mult),
                             reads=[('ps', bo), fk], writes=[('acc', ai)])
                    else:
                        ci = b - 1
                        P.op('dve', 'tensor_tensor', dict(out=ctmp[ci][:], in0=C.ps[bo][0:64, :], in1=fd[0:64, :],
                                                          op=ALU.mult),
                             reads=[('ps', bo), fk], writes=[('ctmp', ci)])
                        if b == 1:
                            P.op('pool', 'tensor_tensor', dict(out=acc[ai][:], in0=acc[ai][:], in1=ctmp[ci][:],
                                                               op=ALU.add),
                                 reads=[('acc', ai), ('ctmp', ci)], writes=[('acc', ai)])
                        else:
                            P.op('pool', 'tensor_tensor',
                                 dict(out=E.OT[rows, 2 * g + j, q0:q0 + 512], in0=acc[ai][:], in1=ctmp[ci][:],
                                      op=ALU.add),
                                 reads=[('acc', ai), ('ctmp', ci)], writes=[('OT', 2 * g + j, qi)])

                for r in range(4):
                    j, half = r // 2, r % 2
                    rows = slice(64 * half, 64 * half + 64)
                    qsrc = lambda c0, c1, j=j: (qT[:, j, c0:c1], [('qT', j, c0 // 512)])
                    for qi in range(4):
                        q0 = qi * 512
                        par = (r * 4 + qi) % 2
                        for b in range(3):
                            bo = 3 + A.psO.next()
                            if b == 0:
                                tiles = [(0, 0, 512, [])]
                                ksrc = lambda kt, half=half: (kcmpT[half][:, :], [('kcmpT', half)])
                                extra = lambda kt, c0, c1: (C.ident[:], cmaskT[:, c0:c1], [('ident',), ('cmaskT',)])
                                vfn = lambda kt: (Vc[:, :, :].rearrange('p a d -> p (a d)'), [('Vc0',), ('Vc1',)])
                            elif b == 1:
                                tiles = E.band_tiles(qi, None, True)
                                extra = None
                                if qi >= 2:
                                    extra = lambda kt, c0, c1: (eexp[:, kt * 128:(kt + 1) * 128], negselT[:, c0:c1],
                                                                [('eexp',), ('negselT',)])
                                ksrc = lambda kt, half=half: (ksT[half][:, kt * 128:(kt + 1) * 128], [('ksT', half, kt // 4), ('ksz', half)])
                                vfn = lambda kt: (Vs[:, kt, :, :].rearrange('p a d -> p (a d)'),
                                                  [('Vs', kt // 4), ('Vsones',)])
                            else:
                                tiles = E.band_tiles(qi, 4, False)
                                extra = None
                                ksrc = lambda kt, half=half: (kwT[half][:, kt * 128:(kt + 1) * 128], [('kwT', half, kt // 4), ('kwz', half)])
                                vfn = lambda kt: (Vw[:, kt, :, :].rearrange('p a d -> p (a d)'),
                                                  [('Vw', kt // 4), ('Vwones',)])
                            for n_, (kt, ca, cb, masks) in enumerate(tiles):
                                job = dict(
                                    st=lambda qsrc=qsrc, ksrc=ksrc, extra=extra, kt=kt, q0=q0, ca=ca, cb=cb, masks=masks:
                                    E.st_tile(qsrc, ksrc, kt, q0, ca, cb, masks, extra),
                                    ep=lambda bk, vfn=vfn, kt=kt, ca=ca, cb=cb, bo=bo, f=(n_ == 0), l=(n_ == len(tiles) - 1):
                                    E.exp_pv(bk, ca, cb, vfn(kt)[0], vfn(kt)[1], bo, f, l))
                                if b == 0 and n_ == 0:
                                    job['pre'] = lambda r=r, qi=qi, par=par: gate_pre(r, qi, par)
                                if n_ == len(tiles) - 1:
                                    job['fin'] = (lambda b=b, bo=bo, r=r, qi=qi, par=par, rows=rows, j=j:
                                                  combine(b, bo, r, qi, par, rows, j))
                                jobs.append(job)
                run_jobs(jobs, A.LA)
                P.barrier()


def build_program(layers=(0, 1, 2, 3), do_attn=True, do_mlp=True):
    nc = bass.Bass("TRN2", target_bir_lowering=False)
    C = Ctx()
    C.nc = nc
    C.t = declare_inputs(nc)
    for name, shp in CONST_SHAPES.items():
        C.t[name] = nc.dram_tensor(name, list(shp), F32, kind="ExternalInput").ap()
    C.scrQ = nc.dram_tensor('scrQ', [6, 16, T], BF16, kind="Internal").ap()
    C.scrK = nc.dram_tensor('scrK', [6, 16, T], BF16, kind="Internal").ap()
    with ExitStack() as es:
        P = Prog(nc, es)
        C.P = P
        C.ps = [es.enter_context(nc.psum_tensor('ps%d' % i, [128, 512], F32)) for i in range(8)]
        C.ps_bf = [C.ps[i][:].bitcast(BF16).rearrange('p (c t) -> p c t', c=8) for i in range(8)]
        C.ident = es.enter_context(nc.sbuf_tensor('ident', [128, 128], BF16))
        C.identf = es.enter_context(nc.sbuf_tensor('identf', [128, 128], F32))
        C.eps = es.enter_context(nc.sbuf_tensor('eps', [128, 1], F32))
        C.one = es.enter_context(nc.sbuf_tensor('one', [128, 1], F32))
        P.op('pool', 'memset', dict(ap=C.identf[:], constant=0.0), writes=[('identf',)])
        P.op('pool', 'affine_select',
             dict(out=C.identf[:], in_=C.identf[:], pattern=[[-1, 128]], compare_op=ALU.not_equal,
                  fill=1.0, base=0, channel_multiplier=1),
             reads=[('identf',)], writes=[('identf',)])
        P.op('pool', 'tensor_copy', dict(out=C.ident[:], in_=C.identf[:]), reads=[('identf',)], writes=[('ident',)])
        P.op('pool', 'memset', dict(ap=C.eps[:], constant=EPS), writes=[('eps',)])
        P.op('pool', 'memset', dict(ap=C.one[:], constant=1.0), writes=[('one',)])
        P.barrier()
        src = C.t['x']
        for L in layers:
            if do_attn:
                attn_phase(P, C, L, src)
                src = C.t['y']
            if do_mlp:
                mlp_phase(P, C, L, src)
                src = C.t['y']
        P.barrier()
        P.emit()
    return nc, P


_CONSTS = None


def kernel(**inputs):
    global _CONSTS
    if _CONSTS is None:
        _CONSTS = host_consts()
    nc, P = build_program()
    x = np.ascontiguousarray(np.asarray(inputs['x'], dtype=np.float32))
    shared = {k: np.ascontiguousarray(np.asarray(v, dtype=np.float32)) for k, v in inputs.items() if k != 'x'}
    shared.update(_CONSTS)
    in_maps = []
    for c in range(NCORES):
        m = dict(shared)
        m['x'] = x[c * SEQ_PER_CORE:(c + 1) * SEQ_PER_CORE].reshape(NTOK, D)
        in_maps.append(m)
    res = run_bass_kernel_spmd(nc, in_maps, core_ids=list(range(NCORES)))
    out = np.stack([np.asarray(r['y']).reshape(SEQ_PER_CORE, T, D) for r in res.results], axis=0)
    return out.reshape(NCORES * SEQ_PER_CORE, T, D).astype(np.float32)
```
